# Optimizing a Trainium2 kernel written in Bass

```python
import jax, jax.numpy as jnp
from jax import lax
import numpy as np

D_MODEL = 1024
BATCH = 16
SEQ = 2048
DEPTH = 1

CTX_LEN = 256
GRID_W = 64
D_MIX = 1024
HEAD_DIM = 64
N_Q_HEADS = 8
N_KV_HEADS = 2
Q_PER_KV = N_Q_HEADS // N_KV_HEADS
D_ATTN = N_Q_HEADS * HEAD_DIM
D_KV = N_KV_HEADS * HEAD_DIM
N_GM_GROUPS = 8
GM_GROUP_DIM = 64
D_GM = N_GM_GROUPS * GM_GROUP_DIM
CHUNK = 128
Q_BLOCK = 128
D_IN = D_ATTN + 2 * D_KV + 2 * D_GM
ROPE_THETA = 10000.0
N_EXPERTS = 16
CAPACITY_FACTOR = 2
EXPERT_FF = 2048
EPS = 1e-6

kernel_name = "hybrid_gqa_gmlp_ecmoe_dit_layer"


def rmsnorm(x, g):
    xf = x.astype(jnp.float32)
    y = xf * lax.rsqrt(jnp.mean(xf * xf, axis=-1, keepdims=True) + EPS)
    return (y * g.astype(jnp.float32)).astype(x.dtype)


def adaln(cvec, w_mod, b_mod):
    m = jax.nn.silu(cvec) @ w_mod + b_mod
    return jnp.split(m, 6, axis=-1)


def axial_rope(row, col):
    n_freq = HEAD_DIM // 4
    inv = ROPE_THETA ** (-jnp.arange(n_freq, dtype=jnp.float32) / n_freq)
    ang = jnp.concatenate([row[:, None].astype(jnp.float32) * inv,
                           col[:, None].astype(jnp.float32) * inv], axis=-1)
    return jnp.cos(ang), jnp.sin(ang)


def apply_rope(x, cos, sin):
    shape = (cos.shape[0],) + (1,) * (x.ndim - 3) + (cos.shape[-1],)
    cos = cos.reshape(shape).astype(x.dtype)
    sin = sin.reshape(shape).astype(x.dtype)
    x1, x2 = jnp.split(x, 2, axis=-1)
    return jnp.concatenate([x1 * cos - x2 * sin, x1 * sin + x2 * cos], axis=-1)


def modulate(x, g, shift, scale):
    return rmsnorm(x, g) * (1 + scale) + shift


def split_in_proj(w_in):
    return jnp.split(w_in, [D_ATTN, D_ATTN + 2 * D_KV], axis=1)


def queries(h, w_q, q_gain):
    b, n = h.shape[:2]
    return rmsnorm((h @ w_q).reshape(b, n, N_KV_HEADS, Q_PER_KV, HEAD_DIM), q_gain)


def keys_values(h, w_kv, k_gain):
    b, n = h.shape[:2]
    k, v = jnp.split(h @ w_kv, 2, axis=-1)
    k = rmsnorm(k.reshape(b, n, N_KV_HEADS, HEAD_DIM), k_gain)
    return k, v.reshape(b, n, N_KV_HEADS, HEAD_DIM)


def gqa_attend(q, k, v):
    b, nq = q.shape[:2]
    scale = HEAD_DIM ** -0.5

    def block(qb):
        s = jnp.einsum('bqkgd,bskd->bkgqs', qb, k).astype(jnp.float32) * scale
        p = jax.nn.softmax(s, axis=-1).astype(v.dtype)
        return jnp.einsum('bkgqs,bskd->bqkgd', p, v)

    nb = nq // Q_BLOCK
    qb = jnp.moveaxis(q.reshape(b, nb, Q_BLOCK, N_KV_HEADS, Q_PER_KV, HEAD_DIM), 1, 0)
    o = lax.map(block, qb)
    return jnp.moveaxis(o, 0, 1).reshape(b, nq, D_ATTN)


def spatial_gating(h, w_gm, v_gain, w_s, b_s):
    b, n = h.shape[:2]
    u, vv = jnp.split(jax.nn.gelu(h @ w_gm, approximate=False), 2, axis=-1)
    vv = rmsnorm(vv, v_gain).reshape(b, n // CHUNK, CHUNK, N_GM_GROUPS, GM_GROUP_DIM)
    mixed = jnp.einsum('gij,bcjgd->bcigd', w_s, vv) + b_s.T[:, :, None]
    return u * mixed.reshape(b, n, D_GM)


def expert_choice_ffn(h, w_router, w1, w3, w2):
    n, d = h.shape[1], h.shape[2]
    cap = CAPACITY_FACTOR * n // N_EXPERTS
    aff = jax.nn.softmax((h @ w_router).astype(jnp.float32), axis=-1)
    gate, idx = lax.top_k(jnp.swapaxes(aff, 1, 2), cap)
    xe = jax.vmap(lambda hb, ib: hb[ib])(h, idx)
    hid = jax.nn.silu(jnp.einsum('becd,edf->becf', xe, w1)) * jnp.einsum('becd,edf->becf', xe, w3)
    ye = jnp.einsum('becf,efd->becd', hid, w2) * gate[..., None].astype(h.dtype)

    def scatter(yb, ib):
        return jnp.zeros((n, d), h.dtype).at[ib.reshape(-1)].add(yb.reshape(-1, d))

    return jax.vmap(scatter)(ye, idx)


def hybrid_layer(xl, xc, c, c_ctx, cos, sin, w_mod, b_mod, g_mix, g_ffn, w_in, q_gain, k_gain,
                 v_gain, w_s, b_s, w_out, w_router, w1, w3, w2, update_ctx):
    sh1, sc1, gt1, sh2, sc2, gt2 = [m[:, None, :] for m in adaln(c, w_mod, b_mod)]
    csh1, csc1, cgt1, csh2, csc2, cgt2 = adaln(c_ctx, w_mod, b_mod)
    w_q, w_kv, w_gm = split_in_proj(w_in)

    hl = modulate(xl, g_mix, sh1, sc1)
    hc = modulate(xc, g_mix, csh1, csc1)
    kc, vc = keys_values(hc, w_kv, k_gain)
    ql = apply_rope(queries(hl, w_q, q_gain), cos, sin)
    kl, vl = keys_values(hl, w_kv, k_gain)
    kl = apply_rope(kl, cos, sin)
    k_all = jnp.concatenate([kl, kc], axis=1)
    v_all = jnp.concatenate([vl, vc], axis=1)
    mix_l = jnp.concatenate([gqa_attend(ql, k_all, v_all),
                             spatial_gating(hl, w_gm, v_gain, w_s, b_s)], axis=-1)
    xl_new = xl + gt1 * (mix_l @ w_out)

    xl_new = xl_new + gt2 * expert_choice_ffn(modulate(xl_new, g_ffn, sh2, sc2), w_router, w1, w3, w2)

    if update_ctx:
        qc = queries(hc, w_q, q_gain)
        mix_c = jnp.concatenate([gqa_attend(qc, kc, vc),
                                 spatial_gating(hc, w_gm, v_gain, w_s, b_s)], axis=-1)
        xc = xc + cgt1 * (mix_c @ w_out)
        xc = xc + cgt2 * expert_choice_ffn(modulate(xc, g_ffn, csh2, csc2), w_router, w1, w3, w2)
    return xl_new, xc


def setup_inputs(seed: int = 0) -> dict:
    key = jax.random.key(seed)
    ks = jax.random.split(key, 21)
    f32 = jnp.float32
    L = DEPTH

    def nrm(k, shape, s):
        return jax.random.normal(k, shape, f32) * s

    return {
        "x": nrm(ks[0], (BATCH, SEQ, D_MODEL), 1.0),
        "c": nrm(ks[1], (BATCH, D_MODEL), 1.0),
        "ctx": nrm(ks[2], (BATCH, CTX_LEN, D_MODEL), 1.0),
        "c_ctx": nrm(ks[3], (D_MODEL,), 1.0),
        "w_mod": nrm(ks[4], (L, D_MODEL, 6 * D_MODEL), D_MODEL ** -0.5),
        "b_mod": nrm(ks[5], (L, 6 * D_MODEL), 0.02),
        "g_mix": 1.0 + nrm(ks[6], (L, D_MODEL), 0.02),
        "g_ffn": 1.0 + nrm(ks[7], (L, D_MODEL), 0.02),
        "w_in": nrm(ks[8], (L, D_MODEL, D_IN), D_MODEL ** -0.5),
        "q_gain": 1.0 + nrm(ks[9], (L, HEAD_DIM), 0.02),
        "k_gain": 1.0 + nrm(ks[10], (L, HEAD_DIM), 0.02),
        "v_gain": 1.0 + nrm(ks[11], (L, D_GM), 0.02),
        "w_s": nrm(ks[12], (L, N_GM_GROUPS, CHUNK, CHUNK), CHUNK ** -0.5),
        "b_s": 1.0 + nrm(ks[13], (L, N_GM_GROUPS, CHUNK), 0.02),
        "w_out": nrm(ks[14], (L, D_MIX, D_MODEL), D_MIX ** -0.5),
        "w_router": nrm(ks[15], (L, D_MODEL, N_EXPERTS), D_MODEL ** -0.5),
        "w1": nrm(ks[16], (L, N_EXPERTS, D_MODEL, EXPERT_FF), D_MODEL ** -0.5),
        "w3": nrm(ks[17], (L, N_EXPERTS, D_MODEL, EXPERT_FF), D_MODEL ** -0.5),
        "w2": nrm(ks[18], (L, N_EXPERTS, EXPERT_FF, D_MODEL), EXPERT_FF ** -0.5),
        "g_final": 1.0 + nrm(ks[19], (D_MODEL,), 0.02),
    }


def reference(x, c, ctx, c_ctx, w_mod, b_mod, g_mix, g_ffn, w_in, q_gain, k_gain, v_gain,
              w_s, b_s, w_out, w_router, w1, w3, w2, g_final):
    n_lat = x.shape[1]
    rows = n_lat // GRID_W
    row = jnp.repeat(jnp.arange(rows), GRID_W)
    col = jnp.tile(jnp.arange(GRID_W), rows)
    cos, sin = axial_rope(row, col)
    xl, xc = x, ctx
    for layer in range(DEPTH):
        xl, xc = hybrid_layer(xl, xc, c, c_ctx, cos, sin, w_mod[layer], b_mod[layer], g_mix[layer],
                              g_ffn[layer], w_in[layer], q_gain[layer], k_gain[layer], v_gain[layer],
                              w_s[layer], b_s[layer], w_out[layer], w_router[layer], w1[layer],
                              w3[layer], w2[layer], update_ctx=layer + 1 < DEPTH)
    return rmsnorm(xl, g_final)
```

```python
import numpy as np
import concourse.bass as bass
import concourse.mybir as mybir
from concourse.bass_utils import run_bass_kernel_spmd
from contextlib import ExitStack

F32 = mybir.dt.float32
BF16 = mybir.dt.bfloat16
U8 = mybir.dt.uint8
ALU = mybir.AluOpType
AF = mybir.ActivationFunctionType
AX = mybir.AxisListType

COMPUTE = ("pe", "act", "dve", "pool")
POOL_TO_DVE = True
CARVE_LOG = []
EPS = 1e-6
NCORES = 8
T = 2048
NT = 16
D = 1024
NE = 16
CAP = 256


class DSem:
    def __init__(self, name):
        self.name = name
        self.sem = None
        self.count = 0


class Ins:
    __slots__ = ("eng", "fn", "waits", "flag", "idx", "dsem")

    def __init__(self, eng, fn):
        self.eng = eng
        self.fn = fn
        self.waits = []
        self.flag = False
        self.idx = None
        self.dsem = None


class Prog:
    def __init__(self, nc):
        self.nc = nc
        self.q = {e: [] for e in COMPUTE + ("sync",)}
        self.last_w = {}
        self.readers = {}
        self.waited_c = {e: {b: -1 for b in COMPUTE} for e in self.q}
        self.waited_d = {e: {} for e in self.q}
        self.dsems = []

    def dsem(self, name):
        d = DSem(name)
        self.dsems.append(d)
        return d

    def _add_wait(self, ins, dep):
        q = ins.eng
        if dep[0] == "c":
            _, b, idx = dep
            if b == "pe" and q == "pe":
                return
            while idx >= 0 and (self.q[b][idx].fn is None or self.q[b][idx].dsem is not None):
                idx -= 1
            if idx < 0 or self.waited_c[q][b] >= idx:
                return
            self.waited_c[q][b] = idx
            self.q[b][idx].flag = True
            ins.waits.append(("c", b, idx))
        else:
            _, d, cnt = dep
            if self.waited_d[q].get(id(d), 0) >= cnt:
                return
            self.waited_d[q][id(d)] = cnt
            ins.waits.append(dep)

    def _deps(self, ins, reads, writes, token):
        ex = [r for r in reads if r.startswith("ps:")]
        if ex:
            reads = [r for r in reads if not r.startswith("ps:")]
            writes = list(writes) + ex
        for r in reads:
            w = self.last_w.get(r)
            if w is not None:
                self._add_wait(ins, w)
        for w_ in writes:
            w = self.last_w.get(w_)
            if w is not None:
                self._add_wait(ins, w)
            for rd in self.readers.get(w_, {}).values():
                self._add_wait(ins, rd)
        key = token[1] if token[0] == "c" else id(token[1])
        for r in reads:
            self.readers.setdefault(r, {})[key] = token
        for w_ in writes:
            self.last_w[w_] = token
            self.readers[w_] = {}

    def op(self, eng, fn, reads=(), writes=()):
        if eng == "pool!":
            eng = "pool"
        elif eng == "pool" and POOL_TO_DVE:
            eng = "dve"
        ins = Ins(eng, fn)
        ins.idx = len(self.q[eng])
        self._deps(ins, reads, writes, ("c", eng, ins.idx))
        self.q[eng].append(ins)
        return ins

    def dma(self, queue, fn, dsem, reads=(), writes=()):
        ins = Ins(queue, fn)
        ins.idx = len(self.q[queue])
        dsem.count += 16
        ins.dsem = dsem
        self._deps(ins, reads, writes, ("d", dsem, dsem.count))
        self.q[queue].append(ins)
        return ins

    def barrier_all(self):
        for q in self.q:
            ins = Ins(q, None)
            ins.idx = len(self.q[q])
            for b in COMPUTE:
                j = len(self.q[b]) - 1
                if j >= 0 and b != q:
                    self._add_wait(ins, ("c", b, j))
            for d in self.dsems:
                if d.count > 0:
                    self._add_wait(ins, ("d", d, d.count))
            self.q[q].append(ins)
        self.last_w = {}
        self.readers = {}

    def final_wait(self, queue, dsems):
        ins = Ins(queue, None)
        ins.idx = len(self.q[queue])
        for d in dsems:
            if d.count > 0:
                self._add_wait(ins, ("d", d, d.count))
        self.q[queue].append(ins)

    def emit(self):
        nc = self.nc
        with ExitStack() as st:
            csem = {e: st.enter_context(nc.semaphore("cs_" + e)) for e in COMPUTE}
            for d in self.dsems:
                if d.count > 0:
                    d.sem = st.enter_context(nc.semaphore("ds_" + d.name))
            mile = {}
            for e in COMPUTE:
                c = 0
                arr = []
                for ins in self.q[e]:
                    if ins.flag:
                        c += 1
                    arr.append(c)
                mile[e] = arr
            block = st.enter_context(nc.Block())

            def run(qname, eng):
                for ins in self.q[qname]:
                    for dep in ins.waits:
                        if dep[0] == "c":
                            eng.wait_ge(csem[dep[1]], mile[dep[1]][dep[2]])
                        else:
                            eng.wait_ge(dep[1].sem, dep[2])
                    if ins.fn is None:
                        continue
                    r = ins.fn(eng)
                    if ins.dsem is not None:
                        r.then_inc(ins.dsem.sem, 16)
                    elif ins.flag:
                        r.then_inc(csem[qname], 1)

            @block.sync
            def _(sync):
                run("sync", sync)

            @block.scalar
            def _(scalar):
                run("act", scalar)

            @block.vector
            def _(vector):
                run("dve", vector)

            @block.gpsimd
            def _(gpsimd):
                run("pool", gpsimd)

            @block.tensor
            def _(tensor):
                run("pe", tensor)


def _consts():
    c = {}
    c["identf"] = np.eye(128, dtype=np.float32)
    k = np.arange(128)
    c["tri"] = (k[:, None] < k[None, :]).astype(np.float32)
    c["iota256"] = np.tile(np.arange(256, dtype=np.float32)[None, :], (128, 1))
    c["iotap"] = np.stack([k, k + 128], axis=1).astype(np.float32)
    sel48 = np.zeros((48, 32, 128), np.float32)
    for b in range(2):
        for e in range(16):
            sel48[32 * b + e, 16 * b + e, :] = 1.0
    c["sel48"] = sel48.reshape(48, 32 * 128)
    sel3 = np.zeros((3, 3, 128), np.float32)
    for j in range(3):
        sel3[j, j, :] = 1.0
    c["sel3"] = sel3.reshape(3, 384)
    nf = 16
    inv = (10000.0 ** (-np.arange(nf, dtype=np.float32) / nf)).astype(np.float32)
    tok = np.arange(T)
    row = (tok // 64).astype(np.float32)
    col = (tok % 64).astype(np.float32)
    ang = np.concatenate([row[:, None] * inv[None, :], col[:, None] * inv[None, :]], axis=-1).astype(np.float32)
    c["cs"] = np.concatenate([np.cos(ang), np.sin(ang)], axis=-1).astype(np.float32)
    tokc = np.zeros((128, NT, 48, 2), np.float32)
    for t in range(NT):
        for col in range(48):
            bb = col // 32
            tokc[:, t, col, 0] = bb * 8 + t // 2
            tokc[:, t, col, 1] = (t % 2) * 128 + np.arange(128)
    c["tokc"] = tokc.reshape(128, NT * 48 * 2)
    return c


def build_nc(stage=9):
    nc = bass.Bass("TRN2", target_bir_lowering=False)
    P = Prog(nc)

    def din(name, shape):
        return nc.dram_tensor(name, list(shape), F32, kind="ExternalInput").ap()

    x = din("x", [2, T, D])
    ctx = din("ctx", [2, 256, D])
    c3 = din("c3", [3, D])
    w_mod = din("w_mod", [D, 6 * D])
    bmod3 = din("bmod3", [3, 6 * D])
    rows3 = din("rows3", [3, 3200])
    g2 = din("g2", [16, 128])
    w_in = din("w_in", [D, 1792])
    w_s = din("w_s", [8, 128, 128])
    b_s = din("b_s", [8, 128])
    w_out = din("w_out", [D, D])
    w_router = din("w_router", [D, NE])
    w1 = din("w1", [NE, D, 2048])
    w3 = din("w3", [NE, D, 2048])
    w2 = din("w2", [NE, 2048, D])
    k_identf = din("identf", [128, 128])
    k_tri = din("tri", [128, 128])
    k_iota256 = din("iota256", [128, 256])
    k_iotap = din("iotap", [128, 2])
    k_sel48 = din("sel48", [48, 4096])
    k_sel3 = din("sel3", [3, 384])
    k_cs = din("cs", [T, 64])
    y = nc.dram_tensor("y", [2, T, D], F32, kind="ExternalOutput").ap()
    x1s = nc.dram_tensor("x1s", [2, T, D], F32, kind="Internal").ap()
    yes = nc.dram_tensor("yes", [2, NE, 2, 128, D], BF16, kind="Internal").ap()
    h2s = nc.dram_tensor("h2s", [2 * T, D], BF16, kind="Internal").ap()
    k_tokc = din("tokc", [128, NT * 48 * 2])

    def sb(name, shape, dt):
        return nc.alloc_sbuf_tensor(name, list(shape), dt)

    identb = sb("identb", [128, 128], BF16)
    identf = sb("identf_sb", [128, 128], F32)
    trib = sb("trib", [128, 128], BF16)
    onesb = sb("onesb", [128, 128], BF16)
    iota256 = sb("iota256_sb", [128, 256], F32)
    iotap = sb("iotap_sb", [128, 2], F32)
    sel48 = sb("sel48_sb", [48, 32, 128], BF16)
    sel3 = sb("sel3_sb", [3, 3, 128], F32)
    cs = sb("cs_sb", [128, NT, 64], F32)
    gain640 = sb("gain640", [128, 640], F32)
    vgainbc = sb("vgainbc", [128, 512], F32)
    gfinbc = sb("gfinbc", [128, D], F32)
    wsT = sb("wsT", [128, 8, 128], BF16)
    bsT = sb("bsT", [128, 8], F32)
    wr = sb("wr", [128, 8, NE], BF16)
    scT = sb("scT", [128, 8, 3], BF16)
    mT = sb("mT", [128, 48, 3], F32)
    gT = sb("gT", [128, 16], F32)
    s1T = sb("s1T", [128, 8, 3], F32)
    msb = sb("msb", [3, 4, D], F32)
    posm = sb("posm", [128, NT, 48], F32)
    ghl = sb("ghl", [128, NT, 48, 4], BF16)
    idxi = sb("idxi", [128, 8], mybir.dt.int32)
    posmT = sb("posmT", [48, T], BF16)
    st = sb("st", [128, 64], F32)
    epsb = sb("epsb", [128, 1], F32)

    RBYTES = 150 * 1024
    R = sb("R", [128, RBYTES], U8)

    class Carver:
        def __init__(self):
            self.off = 0
            CARVE_LOG.append([])

        def get(self, shape, dt, parts=128):
            es = 2 if dt == BF16 else 4
            n = int(np.prod(shape[1:]))
            nb = n * es
            off = (self.off + 63) // 64 * 64
            assert off + nb <= RBYTES, (off, nb)
            v = R[0:parts, off:off + nb].bitcast(dt)
            CARVE_LOG[-1].append((off, tuple(shape), "bf16" if dt == BF16 else "f32"))
            self.off = off + nb
            if len(shape) == 3:
                v = v.rearrange("p (a b) -> p a b", a=shape[1])
            elif len(shape) == 4:
                v = v.rearrange("p (a b c) -> p a b c", a=shape[1], b=shape[2])
            return v

    pp = [nc.alloc_psum_tensor("pp%d" % i, [128, 1024], F32) for i in range(4)]

    def bank(i):
        return pp[i // 2][:, (i % 2) * 512:(i % 2) * 512 + 512]

    def BP(j):
        return ["ps:%d" % (2 * j), "ps:%d" % (2 * j + 1)]

    def bankb(i):
        return bank(i).bitcast(BF16)

    dcache = {}

    def dget(key):
        if key not in dcache:
            dcache[key] = P.dsem("d%d" % len(dcache))
        return dcache[key]

    def load(queue, dst, src, key, reads=()):
        P.dma(queue, lambda e: e.dma_start(out=dst, in_=src), dget(key), reads=reads, writes=[key])

    d_outs = [P.dsem("out0"), P.dsem("out1")]

    cv = Carver()
    c3_sb = cv.get([3, D], F32, parts=3)
    sc3 = cv.get([3, D], F32, parts=3)
    rows3_sb = cv.get([3, 3200], F32, parts=3)
    bmod_sb = cv.get([3, 6 * D], F32, parts=3)
    wm = [cv.get([128, 8, 512], BF16) for _ in range(3)]
    wstmp = cv.get([128, 8, 128], F32)
    g2_sb = cv.get([16, 128], F32, parts=16)
    bs_sb = cv.get([8, 128], F32, parts=8)
    mtmp = [cv.get([3, 512], F32, parts=3) for _ in range(2)]
    trif = cv.get([128, 128], F32)
    sel48f = cv.get([48, 4096], F32, parts=48)

    load("sync", identf[:], k_identf, "identf")
    load("pool", identb[:], k_identf, "identb")
    load("pool", trib[:], k_tri, "trib")
    load("sync", iota256[:], k_iota256, "iota256")
    load("sync", iotap[:], k_iotap, "iotap")
    load("pool", sel48[:].rearrange("p a b -> p (a b)"), k_sel48, "sel48")
    load("sync", sel3[:].rearrange("p a b -> p (a b)"), k_sel3, "sel3")
    load("sync", cs[:], k_cs.rearrange("(i p) c -> p i c", p=128), "cs")
    load("sync", c3_sb, c3, "c3")
    load("sync", rows3_sb, rows3, "rows3")
    load("sync", bmod_sb, bmod3, "bmod")
    load("sync", g2_sb, g2, "g2")
    load("sync", bs_sb, b_s, "bs")
    load("sync", wstmp, w_s.rearrange("g i j -> i g j"), "wstmp")
    load("pool", wr[:], w_router.rearrange("(k p) e -> p k e", p=128), "wr")
    P.op("pool!", lambda e: e.memset(onesb[:], 1.0), writes=["onesb"])
    P.op("pool!", lambda e: e.memset(epsb[:], EPS), writes=["epsb"])

    P.op("act", lambda e: e.activation(out=sc3, in_=c3_sb, func=AF.Silu), reads=["c3"], writes=["sc3"])
    for k in range(8):
        P.op("pe", lambda e, k=k: e.transpose(out=bank(0)[:, k * 3:k * 3 + 3], in_=sc3[:, k * 128:(k + 1) * 128],
                                              identity=identf[0:3, 0:3]), reads=["sc3", "identf"], writes=["ps:0"])
    P.op("dve", lambda e: e.tensor_copy(out=scT[:].rearrange("p a b -> p (a b)"), in_=bank(0)[:, 0:24]),
         reads=["ps:0"], writes=["scT"])

    psT = bank(3)
    for n in range(12):
        seg, hh = n // 2, n % 2
        wmb = wm[n % 3]
        load("pool", wmb, w_mod[:, n * 512:(n + 1) * 512].rearrange("(k p) n -> p k n", p=128), "wm%d" % (n % 3))
        pm = bank(1 + n % 2)
        pk = "ps:%d" % (1 + n % 2)
        for k in range(8):
            P.op("pe", lambda e, k=k, wmb=wmb, pm=pm: e.matmul(pm[0:3, :], lhsT=scT[:, k, :], rhs=wmb[:, k, :],
                                                               start=(k == 0), stop=(k == 7)),
                 reads=["scT", "wm%d" % (n % 3)], writes=[pk])
        mt = mtmp[n % 2]
        mk = "mtmp%d" % (n % 2)
        P.op("dve", lambda e, pm=pm, mt=mt, n=n: e.tensor_tensor(out=mt, in0=pm[0:3, :],
                                                                in1=bmod_sb[:, n * 512:(n + 1) * 512], op=ALU.add),
             reads=[pk, "bmod"], writes=[mk])
        for j in range(4):
            cidx = (n * 4 + j) * 3
            P.op("pe", lambda e, mt=mt, j=j, cidx=cidx: e.transpose(out=psT[:, cidx:cidx + 3],
                                                                   in_=mt[:, j * 128:(j + 1) * 128],
                                                                   identity=identf[0:3, 0:3]),
                 reads=[mk, "identf"], writes=["ps:3"])
        if seg == 2:
            P.op("act", lambda e, mt=mt, hh=hh: e.copy(out=msb[:, 0, hh * 512:(hh + 1) * 512], in_=mt),
                 reads=[mk], writes=["msb"])
        elif seg == 3:
            P.op("act", lambda e, mt=mt, hh=hh: e.copy(out=msb[:, 2, hh * 512:(hh + 1) * 512], in_=mt),
                 reads=[mk], writes=["msb"])
        elif seg == 5:
            P.op("act", lambda e, mt=mt, hh=hh: e.copy(out=msb[:, 3, hh * 512:(hh + 1) * 512], in_=mt),
                 reads=[mk], writes=["msb"])
        elif seg == 4:
            P.op("dve", lambda e, mt=mt, hh=hh: e.scalar_tensor_tensor(
                out=msb[:, 1, hh * 512:(hh + 1) * 512], in0=mt, scalar=1.0,
                in1=rows3_sb[:, hh * 512:(hh + 1) * 512], op0=ALU.add, op1=ALU.mult),
                 reads=[mk, "rows3"], writes=["msb"])
    P.op("dve", lambda e: e.tensor_copy(out=mT[:].rearrange("p a b -> p (a b)"), in_=psT[:, 0:144]),
         reads=["ps:3"], writes=["mT"])
    P.op("pe", lambda e: e.transpose(out=bank(0)[:, 32:48], in_=g2_sb, identity=identf[0:16, 0:16]),
         reads=["g2", "identf"], writes=["ps:0"])
    P.op("dve", lambda e: e.tensor_copy(out=gT[:], in_=bank(0)[:, 32:48]), reads=["ps:0"], writes=["gT"])
    P.op("dve", lambda e: e.scalar_tensor_tensor(out=s1T[:], in0=mT[:, 8:16, :], scalar=1.0,
                                                 in1=gT[:, 0:8].unsqueeze(2).to_broadcast([128, 8, 3]),
                                                 op0=ALU.add, op1=ALU.mult),
         reads=["mT", "gT"], writes=["s1T"])
    for (dst, c0, n_) in ((gfinbc[:, 0:512], 1024, 512), (gfinbc[:, 512:1024], 1536, 512),
                          (gain640[:, 0:512], 2048, 512), (gain640[:, 512:640], 2560, 128),
                          (vgainbc[:, 0:512], 2688, 512)):
        P.op("pe", lambda e, c0=c0, n_=n_: e.matmul(bank(4)[:, 0:n_], lhsT=sel3[:, 0, :], rhs=rows3_sb[:, c0:c0 + n_],
                                                    start=True, stop=True), reads=["sel3", "rows3"], writes=["ps:4"])
        P.op("act", lambda e, dst=dst, n_=n_: e.copy(out=dst, in_=bank(4)[:, 0:n_]), reads=["ps:4"], writes=["bcst"])
    for g in range(8):
        P.op("pe", lambda e, g=g: e.transpose(out=bank(5 + g // 4)[:, (g % 4) * 128:(g % 4) * 128 + 128],
                                              in_=wstmp[:, g, :], identity=identf[:]),
             reads=["wstmp", "identf"], writes=["ps:%d" % (5 + g // 4)])
    for h_ in range(2):
        P.op("act", lambda e, h_=h_: e.copy(out=wsT[:, h_ * 4:(h_ + 1) * 4, :].rearrange("p a b -> p (a b)"),
                                            in_=bank(5 + h_)), reads=["ps:%d" % (5 + h_)], writes=["wsT"])
    P.op("pe", lambda e: e.transpose(out=bank(0)[:, 64:72], in_=bs_sb, identity=identf[0:8, 0:8]),
         reads=["bs", "identf", "gT"], writes=["ps:0"])
    P.op("dve", lambda e: e.tensor_copy(out=bsT[:], in_=bank(0)[:, 64:72]), reads=["ps:0"], writes=["bsT"])
    P.barrier_all()
    if stage == 0:
        P.dma("sync", lambda e: e.dma_start(out=y[0, 0:128, :], in_=gfinbc[:]), d_outs[0], reads=["bcst"])
        P.dma("sync", lambda e: e.dma_start(out=y[0, 128:256, 0:640], in_=gain640[:]), d_outs[0], reads=["bcst"])
        P.dma("sync", lambda e: e.dma_start(out=y[0, 256:384, 0:144], in_=mT[:].rearrange("p a b -> p (a b)")), d_outs[0])
        P.dma("sync", lambda e: e.dma_start(out=y[0, 384:387, :], in_=msb[:, 1, :]), d_outs[0])
        P.dma("sync", lambda e: e.dma_start(out=y[0, 512:640, 0:24], in_=s1T[:].rearrange("p a b -> p (a b)")), d_outs[0])
        P.final_wait("sync", d_outs)
        P.emit()
        return nc

    def rstd_chain(col, scale):
        P.op("act", lambda e: e.activation(out=st[:, col + 1:col + 2], in_=st[:, col:col + 1], func=AF.Sqrt,
                                           scale=scale, bias=epsb[:, 0:1]),
             reads=["st%d" % col, "epsb"], writes=["st%d" % (col + 1)])
        P.op("dve", lambda e: e.reciprocal(out=st[:, col + 1:col + 2], in_=st[:, col + 1:col + 2]),
             reads=["st%d" % (col + 1)], writes=["st%d" % (col + 1)])

    def epoch_ac(b):
        cv = Carver()
        wio = cv.get([128, 8, 1792], BF16)
        qa = cv.get([128, 4, T], BF16)
        gmT = cv.get([128, 4, T], BF16)
        kT = cv.get([128, 2, 2304], BF16)
        vaug = cv.get([128, 18, 2, 192], BF16)
        xt = [cv.get([128, D], F32) for _ in range(2)]
        sqj = cv.get([128, D], BF16)
        xn = cv.get([128, D], BF16)
        hTt = cv.get([128, 8, 128], BF16)
        qk = cv.get([128, 10, 64], F32)
        qk2 = cv.get([128, 10, 64], F32)
        qkr = cv.get([128, 10, 64], BF16)
        kd = cv.get([128, 4, 64], BF16)
        rp = cv.get([128, 4, 320], F32)
        u_sb = cv.get([128, 512], F32)
        vv_sb = cv.get([128, 512], F32)
        vvn = cv.get([128, 512], BF16)
        gtmp = cv.get([128, 512], F32)
        gmb = cv.get([128, 512], BF16)
        PT = [cv.get([128, 1024], BF16) for _ in range(4)]
        rd = cv.get([128, 512], F32)
        gt1bc = cv.get([128, D], F32)
        tmpo = cv.get([128, D], F32)
        s10 = st[:, 16:26]
        r10 = st[:, 32:42]

        for kk in range(4):
            load("pool", wio[:, 2 * kk:2 * kk + 2, :],
                 w_in[kk * 256:(kk + 1) * 256, :].rearrange("(k p) n -> p k n", p=128), "wio")
        for hh in range(2):
            P.op("pe", lambda e, hh=hh: e.matmul(bank(0), lhsT=sel3[:, b, :], rhs=msb[:, 0, hh * 512:(hh + 1) * 512],
                                                 start=True, stop=True), reads=["sel3", "msb"], writes=["ps:0"])
            P.op("act", lambda e, hh=hh: e.copy(out=gt1bc[:, hh * 512:(hh + 1) * 512], in_=bank(0)),
                 reads=["ps:0"], writes=["gt1bc"])
        P.op("pool!", lambda e: e.memset(vaug.rearrange("p a b c -> p (a b c)"), 1.0), writes=["vaug"])

        g3 = gain640[:].rearrange("p (a b) -> p a b", a=10)
        qkf = qk.rearrange("p a b -> p (a b)")
        qkrf = qkr.rearrange("p a b -> p (a b)")
        kdf = kd.rearrange("p a b -> p (a b)")
        kdv = kd.rearrange("p (a b) c -> p a b c", a=2)
        rpv = [rp[:, i, :].rearrange("p (a b) -> p a b", a=10) for i in range(4)]
        pA = bankb(0)
        pB = bankb(5)
        pC = bankb(7)

        def X1(t):
            lat = t < 16
            xb = xt[t % 2]
            xk = "xt%d" % (t % 2)
            src = x[b, t * 128:(t + 1) * 128, :] if lat else ctx[b, (t - 16) * 128:(t - 15) * 128, :]
            load("sync", xb, src, xk)
            P.op("act", lambda e: e.activation(out=sqj, in_=xb, func=AF.Square, accum_out=st[:, 0:1]),
                 reads=[xk], writes=["sqj", "st0"])
            rstd_chain(0, 1.0 / D)
            P.op("act", lambda e: e.activation(out=xn, in_=xb, func=AF.Copy, scale=st[:, 1:2]),
                 reads=[xk, "st1"], writes=["xn"])
            for k in range(8):
                P.op("pe", lambda e, k=k: e.transpose(out=pA[:, k * 128:(k + 1) * 128], in_=xn[:, k * 128:(k + 1) * 128],
                                                      identity=identb[:]), reads=["xn", "identb"], writes=["ps:0"])

        def X2(t):
            lat = t < 16
            j = b if lat else 2
            for k in range(8):
                eng_ = "act" if k % 2 == 0 else "dve"
                if eng_ == "act":
                    P.op("act", lambda e, k=k: e.activation(out=hTt[:, k, :], in_=pA[:, k * 128:(k + 1) * 128],
                                                            func=AF.Identity, scale=s1T[:, k, j:j + 1],
                                                            bias=mT[:, k, j:j + 1]),
                         reads=["ps:0", "s1T", "mT"], writes=["hTt%d" % k])
                else:
                    P.op("dve", lambda e, k=k: e.tensor_scalar(out=hTt[:, k, :], in0=pA[:, k * 128:(k + 1) * 128],
                                                              scalar1=s1T[:, k, j:j + 1], scalar2=mT[:, k, j:j + 1],
                                                              op0=ALU.mult, op1=ALU.add),
                         reads=["ps:0", "s1T", "mT"], writes=["hTt%d" % k])
            slices = [(1, 0, 512), (2, 512, 256), (3, 768, 512), (4, 1280, 512)] if lat else [(2, 512, 256)]
            for (bi, c0, n_) in slices:
                for k in range(8):
                    P.op("pe", lambda e, bi=bi, c0=c0, n_=n_, k=k: e.matmul(bank(bi)[:, 0:n_], lhsT=hTt[:, k, :],
                                                                           rhs=wio[:, k, c0:c0 + n_],
                                                                           start=(k == 0), stop=(k == 7)),
                         reads=["hTt%d" % k, "wio"], writes=["ps:%d" % bi])

        def Ya(t):
            lat = t < 16
            if lat:
                P.op("dve", lambda e: e.tensor_copy(out=qkf[:, 0:512], in_=bank(1)), reads=["ps:1"], writes=["qk"])
            P.op("dve", lambda e: e.tensor_copy(out=qkf[:, 512:640], in_=bank(2)[:, 0:128]), reads=["ps:2"], writes=["qk"])
            P.op("act", lambda e: e.copy(out=vaug[:, t, :, 64:128],
                                         in_=bank(2)[:, 128:256].rearrange("p (a b) -> p a b", a=2)),
                 reads=["ps:2"], writes=["vaug"])
            if lat:
                P.op("act", lambda e: e.activation(out=u_sb, in_=bank(3), func=AF.Gelu), reads=["ps:3"], writes=["u_sb"])
                P.op("act", lambda e: e.activation(out=vv_sb, in_=bank(4), func=AF.Gelu), reads=["ps:4"], writes=["vv_sb"])

        def Yb(t):
            lat = t < 16
            h0 = 0 if lat else 8
            nh = 10 - h0
            if lat:
                P.op("act", lambda e: e.activation(out=gtmp, in_=vv_sb, func=AF.Square, accum_out=st[:, 4:5]),
                     reads=["vv_sb"], writes=["gtmp", "st4"])
                P.op("act", lambda e: e.activation(out=st[:, 5:6], in_=st[:, 4:5], func=AF.Sqrt, scale=1.0 / 512,
                                                   bias=epsb[:, 0:1]), reads=["st4", "epsb"], writes=["st5"])
            P.op("dve", lambda e: e.tensor_tensor(out=qk2[:, h0:10, :], in0=qk[:, h0:10, :], in1=qk[:, h0:10, :],
                                                  op=ALU.mult), reads=["qk"], writes=["qk2"])
            P.op("dve", lambda e: e.tensor_reduce(out=s10[:, h0:10], in_=qk2[:, h0:10, :], axis=AX.X, op=ALU.add),
                 reads=["qk2"], writes=["s10"])
            P.op("act", lambda e: e.activation(out=r10[:, h0:10], in_=s10[:, h0:10], func=AF.Sqrt,
                                               scale=1.0 / 64, bias=epsb[:, 0:1]),
                 reads=["s10", "epsb"], writes=["r10"])
            if lat:
                P.op("dve", lambda e: e.reciprocal(out=st[:, 5:6], in_=st[:, 5:6]), reads=["st5"], writes=["st5"])
                P.op("dve", lambda e: e.scalar_tensor_tensor(out=vvn, in0=vv_sb, scalar=st[:, 5:6], in1=vgainbc[:],
                                                             op0=ALU.mult, op1=ALU.mult),
                     reads=["vv_sb", "st5", "bcst"], writes=["vvn"])
                for g in range(8):
                    P.op("pe", lambda e, g=g: e.matmul(bank(6)[:, g * 64:(g + 1) * 64], lhsT=wsT[:, g, :],
                                                       rhs=vvn[:, g * 64:(g + 1) * 64], start=True, stop=True),
                         reads=["wsT", "vvn"], writes=["ps:6"])
            P.op("dve", lambda e: e.reciprocal(out=r10[:, h0:10], in_=r10[:, h0:10]), reads=["r10"], writes=["r10"])
            P.op("dve", lambda e: e.tensor_tensor(out=qk2[:, h0:10, :], in0=qk[:, h0:10, :],
                                                  in1=r10[:, h0:10].unsqueeze(2).to_broadcast([128, nh, 64]),
                                                  op=ALU.mult), reads=["qk", "r10"], writes=["qk2"])
            P.op("dve", lambda e: e.tensor_tensor(out=qk2[:, h0:10, :], in0=qk2[:, h0:10, :], in1=g3[:, h0:10, :],
                                                  op=ALU.mult), reads=["qk2", "bcst"], writes=["qk2"])
            if lat:
                cosb = cs[:, t, 0:32].unsqueeze(1).to_broadcast([128, 10, 32])
                sinb = cs[:, t, 32:64].unsqueeze(1).to_broadcast([128, 10, 32])
                x1v = qk2[:, :, 0:32]
                x2v = qk2[:, :, 32:64]
                P.op("dve", lambda e: e.tensor_tensor(out=rpv[0], in0=x1v, in1=cosb, op=ALU.mult),
                     reads=["qk2", "cs"], writes=["rp0"])
                P.op("dve", lambda e: e.tensor_tensor(out=rpv[1], in0=x2v, in1=sinb, op=ALU.mult),
                     reads=["qk2", "cs"], writes=["rp1"])
                P.op("pool", lambda e: e.tensor_tensor(out=rpv[2], in0=x1v, in1=sinb, op=ALU.mult),
                     reads=["qk2", "cs"], writes=["rp2"])
                P.op("pool", lambda e: e.tensor_tensor(out=rpv[3], in0=x2v, in1=cosb, op=ALU.mult),
                     reads=["qk2", "cs"], writes=["rp3"])
                P.op("dve", lambda e: e.tensor_tensor(out=qkr[:, :, 0:32], in0=rpv[0], in1=rpv[1], op=ALU.subtract),
                     reads=["rp0", "rp1"], writes=["qkr"])
                P.op("pool", lambda e: e.tensor_tensor(out=qkr[:, :, 32:64], in0=rpv[2], in1=rpv[3], op=ALU.add),
                     reads=["rp2", "rp3"], writes=["qkr"])
            else:
                P.op("dve", lambda e: e.tensor_copy(out=qkr[:, 8:10, :], in_=qk2[:, 8:10, :]), reads=["qk2"], writes=["qkr"])
            P.op("pool", lambda e: e.tensor_copy(out=kdv, in_=qkr[:, 8:10, :].unsqueeze(2).to_broadcast([128, 2, 2, 64])),
                 reads=["qkr"], writes=["kd"])
            if lat:
                for c in range(4):
                    P.op("pe", lambda e, c=c: e.transpose(out=pB[:, c * 128:(c + 1) * 128], in_=qkrf[:, c * 128:(c + 1) * 128],
                                                          identity=identb[:]), reads=["qkr", "identb"], writes=["ps:5"])
            for kv in range(2):
                P.op("pe", lambda e, kv=kv: e.transpose(out=pB[:, 512 + kv * 128:512 + (kv + 1) * 128],
                                                        in_=kdf[:, kv * 128:(kv + 1) * 128], identity=identb[:]),
                     reads=["kd", "identb"], writes=["ps:5"])
            if lat:
                P.op("act", lambda e: e.copy(out=qa[:, :, t * 128:(t + 1) * 128],
                                             in_=pB[:, 0:512].rearrange("p (a b) -> p a b", a=4)),
                     reads=["ps:5"], writes=["qa"])
            P.op("act", lambda e: e.copy(out=kT[:, :, t * 128:(t + 1) * 128],
                                         in_=pB[:, 512:768].rearrange("p (a b) -> p a b", a=2)),
                 reads=["ps:5"], writes=["kT"])
            if not lat:
                return
            P.op("dve", lambda e: e.tensor_tensor(out=gtmp.rearrange("p (a b) -> p a b", a=8),
                                                  in0=bank(6).rearrange("p (a b) -> p a b", a=8),
                                                  in1=bsT[:].unsqueeze(2).to_broadcast([128, 8, 64]), op=ALU.add),
                 reads=["ps:6", "bsT"], writes=["gtmp"])
            P.op("pool", lambda e: e.tensor_tensor(out=gmb, in0=gtmp, in1=u_sb, op=ALU.mult),
                 reads=["gtmp", "u_sb"], writes=["gmb"])
            for c in range(4):
                P.op("pe", lambda e, c=c: e.transpose(out=pC[:, c * 128:(c + 1) * 128], in_=gmb[:, c * 128:(c + 1) * 128],
                                                      identity=identb[:]), reads=["gmb", "identb"], writes=["ps:7"])
            P.op("act", lambda e: e.copy(out=gmT[:, :, t * 128:(t + 1) * 128],
                                         in_=pC[:, 0:512].rearrange("p (a b) -> p a b", a=4)),
                 reads=["ps:7"], writes=["gmT"])

        NTA = 18
        X1(0)
        X2(0)
        X1(1)
        for t in range(NTA):
            Ya(t)
            if t + 1 < NTA:
                X2(t + 1)
            if t + 2 < NTA:
                X1(t + 2)
            Yb(t)

        P.barrier_all()
        steps = [(c, qg, sc_) for c in range(4) for qg in range(4) for sc_ in range(18)]
        Osb = cv.get([128, 1024], F32)

        def qk_step(i):
            c, qg, sc_ = steps[i]
            kv = c // 2
            sj = (0, 1, 3)[i % 3]
            S = pp[sj]
            for half in range(2):
                r0 = half * 64
                P.op("pe", lambda e, half=half, r0=r0: e.matmul(
                    S[:, half * 512:(half + 1) * 512], lhsT=kT[r0:r0 + 64, kv, sc_ * 128:(sc_ + 1) * 128],
                    rhs=qa[r0:r0 + 64, c, qg * 512:(qg + 1) * 512], start=True, stop=True),
                     reads=["kT", "qa%d_%d_%d" % (c, half, qg), "qa"], writes=BP(sj))
            P.op("act", lambda e: e.activation(out=PT[i % 4], in_=S[:], func=AF.Exp, scale=0.125),
                 reads=BP(sj), writes=["PT%d" % (i % 4)])

        def pv_step(i):
            c, qg, sc_ = steps[i]
            kv = c // 2
            for half in range(2):
                off = 64 if half == 0 else 0
                P.op("pe", lambda e, half=half, off=off: e.matmul(
                    bank(4 + half), lhsT=vaug[:, sc_, kv, off:off + 128],
                    rhs=PT[i % 4][:, half * 512:(half + 1) * 512], start=(sc_ == 0), stop=(sc_ == 17)),
                     reads=["vaug", "PT%d" % (i % 4)], writes=["ps:%d" % (4 + half)])
            if sc_ == 17:
                P.op("dve", lambda e: e.tensor_copy(out=Osb, in_=pp[2][:]), reads=BP(2), writes=["Osb"])
                for half in range(2):
                    nr = half * 64
                    dr = 64 - nr
                    P.op("dve", lambda e, half=half, nr=nr, dr=dr: e.reciprocal(
                        out=rd[nr:nr + 64, :], in_=Osb[dr:dr + 64, half * 512:(half + 1) * 512]),
                         reads=["Osb"], writes=["rd%d" % half])
                    P.op("dve", lambda e, half=half, nr=nr: e.tensor_tensor(
                        out=qa[nr:nr + 64, c, qg * 512:(qg + 1) * 512], in0=Osb[nr:nr + 64, half * 512:(half + 1) * 512],
                        in1=rd[nr:nr + 64, :], op=ALU.mult),
                         reads=["Osb", "rd%d" % half], writes=["qa%d_%d_%d" % (c, half, qg)])

        if stage >= 2:
            n = len(steps)
            qk_step(0)
            qk_step(1)
            for i in range(n):
                if i + 2 < n:
                    qk_step(i + 2)
                pv_step(i)

        P.barrier_all()
        for kk in range(4):
            load("pool", wio[:, 2 * kk:2 * kk + 2, 0:1024],
                 w_out[kk * 256:(kk + 1) * 256, :].rearrange("(k p) n -> p k n", p=128), "wio")
        tmpo2 = [tmpo, Osb]
        for t in range(16):
            xb = xt[t % 2]
            xk = "xt%d" % (t % 2)
            load("sync", xb, x[b, t * 128:(t + 1) * 128, :], xk)
            pj = 2 + t % 2
            pO = pp[pj]
            tb = tmpo2[t % 2]
            tk = "tmpo%d" % (t % 2)
            for hh in range(2):
                for k in range(8):
                    src = qa[:, k, t * 128:(t + 1) * 128] if k < 4 else gmT[:, k - 4, t * 128:(t + 1) * 128]
                    rk = ["qa%d_%d_%d" % (k, hf, t // 4) for hf in range(2)] + ["qa"] if k < 4 else ["gmT"]
                    P.op("pe", lambda e, hh=hh, k=k, src=src, pO=pO: e.matmul(pO[:, hh * 512:(hh + 1) * 512], lhsT=src,
                                                                             rhs=wio[:, k, hh * 512:(hh + 1) * 512],
                                                                             start=(k == 0), stop=(k == 7)),
                         reads=rk + ["wio"], writes=BP(pj))
            P.op("dve", lambda e, pO=pO, tb=tb: e.tensor_tensor(out=tb, in0=pO[:], in1=gt1bc, op=ALU.mult),
                 reads=BP(pj) + ["gt1bc"], writes=[tk])
            P.op("pool", lambda e, xb=xb, tb=tb: e.tensor_tensor(out=xb, in0=xb, in1=tb, op=ALU.add),
                 reads=[tk, xk], writes=[xk])
            dst = x1s[b, t * 128:(t + 1) * 128, :] if stage >= 3 else y[b, t * 128:(t + 1) * 128, :]
            P.dma("sync", lambda e, xb=xb, dst=dst: e.dma_start(out=dst, in_=xb), d_outs[t % 2],
                  reads=[xk], writes=["x1s%d_%d" % (b, t)])

    for b in range(2):
        epoch_ac(b)
        P.barrier_all()


    def epoch_d():
        cv = Carver()
        h2 = cv.get([128, 2 * NT, D], BF16)
        x1t = [cv.get([128, D], F32) for _ in range(2)]
        sqj = cv.get([128, D], BF16)
        tmp = cv.get([128, D], F32)
        s2bc = cv.get([128, D], F32)
        sh2bc = cv.get([128, D], F32)
        h2Tt = cv.get([128, D], BF16)
        ex = cv.get([128, 16], F32)
        aff2 = cv.get([128, NT, 48], F32)
        affT = cv.get([48, T], F32, parts=48)
        work = cv.get([48, T], F32, parts=48)
        mx8 = cv.get([48, 8], F32, parts=48)
        gate2 = cv.get([128, NT, 48], F32)
        mask2 = cv.get([128, NT, 48], BF16)
        mask2f = cv.get([128, NT, 48], F32)
        totsb = cv.get([128, NT, 48], F32)
        base = cv.get([128, NT, 48], F32)
        pos = cv.get([128, NT, 48], F32)
        ghf = cv.get([128, NT, 48], F32)
        gh = cv.get([128, NT, 48], BF16)
        fl = lambda v: v.rearrange("p a b -> p (a b)")

        P.op("pool!", lambda e: e.memset(fl(aff2), 0.0), writes=["aff2"])
        d_h2s = [P.dsem("h2s0"), P.dsem("h2s1")]
        tokf = cv.get([128, NT * 48, 2], F32)
        load("sync", tokf.rearrange("p a b -> p (a b)"), k_tokc, "tokf")
        P.op("dve", lambda e: e.tensor_copy(out=ghl[:].rearrange("p a b c -> p (a b) c")[:, :, 2:4], in_=tokf),
             reads=["tokf"], writes=["ghl23"])
        for b in range(2):
            for (dst, ri, key) in ((s2bc, 1, "s2bc"), (sh2bc, 2, "sh2bc")):
                for hh in range(2):
                    P.op("pe", lambda e, ri=ri, hh=hh, b=b: e.matmul(bank(0), lhsT=sel3[:, b, :],
                                                               rhs=msb[:, ri, hh * 512:(hh + 1) * 512],
                                                               start=True, stop=True), reads=[], writes=["ps:0"])
                    P.op("act", lambda e, dst=dst, hh=hh: e.copy(out=dst[:, hh * 512:(hh + 1) * 512], in_=bank(0)),
                         reads=["ps:0"], writes=[key])
            def DX(t, b=b):
                xb = x1t[t % 2]
                xk = "x1t%d" % (t % 2)
                load("sync", xb, x1s[b, t * 128:(t + 1) * 128, :], xk)
                P.op("act", lambda e: e.activation(out=sqj, in_=xb, func=AF.Square, accum_out=st[:, 0:1]),
                     reads=[xk], writes=["sqj", "st0"])
                rstd_chain(0, 1.0 / D)
                P.op("dve", lambda e: e.scalar_tensor_tensor(out=tmp, in0=xb, scalar=st[:, 1:2], in1=s2bc,
                                                             op0=ALU.mult, op1=ALU.mult),
                     reads=[xk, "st1", "s2bc"], writes=["tmp"])
                h2t = h2[:, b * NT + t, :]
                P.op("pool", lambda e: e.tensor_tensor(out=h2t, in0=tmp, in1=sh2bc, op=ALU.add),
                     reads=["tmp", "sh2bc"], writes=["h2t"])
                P.dma("sync", lambda e: e.dma_start(out=h2s[b * T + t * 128:b * T + (t + 1) * 128, :], in_=h2t),
                      d_h2s[t % 2], reads=["h2t"])
                pA = bankb(1)
                for k in range(8):
                    P.op("pe", lambda e, k=k: e.transpose(out=pA[:, k * 128:(k + 1) * 128],
                                                          in_=h2t[:, k * 128:(k + 1) * 128], identity=identb[:]),
                         reads=["h2t"], writes=["ps:1"])
                P.op("act", lambda e: e.copy(out=h2Tt, in_=pA), reads=["ps:1"], writes=["h2Tt"])
                lg = bank(2)[:, t * 16:(t + 1) * 16]
                for k in range(8):
                    P.op("pe", lambda e, k=k: e.matmul(lg, lhsT=h2Tt[:, k * 128:(k + 1) * 128], rhs=wr[:, k, :],
                                                       start=(k == 0), stop=(k == 7)),
                         reads=["h2Tt"], writes=["ps:2"])

            def DY(t, b=b):
                lg = bank(2)[:, t * 16:(t + 1) * 16]
                P.op("dve", lambda e: e.tensor_reduce(out=st[:, 8:9], in_=lg, axis=AX.X, op=ALU.max),
                     reads=["ps:2"], writes=["st8"])
                P.op("dve", lambda e: e.tensor_scalar(out=st[:, 9:10], in0=st[:, 8:9], scalar1=-1.0, scalar2=None,
                                                      op0=ALU.mult), reads=["st8"], writes=["st9"])
                P.op("act", lambda e: e.activation(out=ex, in_=lg, func=AF.Exp, bias=st[:, 9:10],
                                                   accum_out=st[:, 10:11]),
                     reads=["ps:2", "st9"], writes=["ex", "st10"])
                P.op("dve", lambda e: e.reciprocal(out=st[:, 11:12], in_=st[:, 10:11]), reads=["st10"], writes=["st11"])
                P.op("dve", lambda e: e.tensor_scalar(out=aff2[:, t, 32 * b:32 * b + 16], in0=ex,
                                                      scalar1=st[:, 11:12], scalar2=None, op0=ALU.mult),
                     reads=["ex", "st11"], writes=["aff2"])

            DX(0)
            for t in range(NT):
                if t + 1 < NT:
                    DX(t + 1)
                DY(t)
        for t in range(NT):
            P.op("pe", lambda e, t=t: e.transpose(out=pp[2 + t // 8][0:48, (t % 8) * 128:(t % 8) * 128 + 128],
                                                  in_=aff2[:, t, :], identity=identf[:]),
                 reads=["aff2"], writes=BP(2 + t // 8))
        for hh in range(2):
            P.op("act", lambda e, hh=hh: e.copy(out=affT[:, hh * 1024:(hh + 1) * 1024], in_=pp[2 + hh][0:48, :]),
                 reads=BP(2 + hh), writes=["affT"])
            P.op("dve", lambda e, hh=hh: e.tensor_copy(out=work[:, hh * 1024:(hh + 1) * 1024], in_=pp[2 + hh][0:48, :]),
                 reads=BP(2 + hh), writes=["work"])
        for r in range(CAP // 8):
            P.op("dve", lambda e: e.max(out=mx8, in_=work), reads=["work"], writes=["mx8"])
            P.op("dve", lambda e: e.match_replace(out=work, in_to_replace=mx8, in_values=work, imm_value=0.0),
                 reads=["work", "mx8"], writes=["work"])
        P.op("dve", lambda e: e.tensor_tensor(out=work, in0=affT, in1=work, op=ALU.subtract),
             reads=["affT", "work"], writes=["work"])
        for t in range(NT):
            P.op("pe", lambda e, t=t: e.transpose(out=pp[t // 8][:, (t % 8) * 64:(t % 8) * 64 + 48],
                                                  in_=work[:, t * 128:(t + 1) * 128], identity=identf[0:48, 0:48]),
                 reads=["work"], writes=BP(t // 8))
        for hh in range(2):
            P.op("act", lambda e, hh=hh: e.copy(out=gate2[:, hh * 8:(hh + 1) * 8, :],
                                                in_=pp[hh][:, 0:512].rearrange("p (a b) -> p a b", a=8)[:, :, 0:48]),
                 reads=BP(hh), writes=["gate2"])
        P.op("dve", lambda e: e.tensor_scalar(out=fl(mask2), in0=fl(gate2), scalar1=0.0, scalar2=None, op0=ALU.is_gt),
             reads=["gate2"], writes=["mask2"])
        P.op("dve", lambda e: e.tensor_scalar(out=fl(mask2f), in0=fl(gate2), scalar1=0.0, scalar2=None, op0=ALU.is_gt),
             reads=["gate2"], writes=["mask2f"])
        for hh in range(2):
            P.op("pe", lambda e, hh=hh: e.matmul(bank(4 + hh)[:, 0:384], lhsT=trib[:], rhs=fl(mask2)[:, hh * 384:(hh + 1) * 384],
                                                 start=True, stop=True), reads=["mask2"], writes=["ps:%d" % (4 + hh)])
            P.op("pe", lambda e, hh=hh: e.matmul(bank(6 + hh)[:, 0:384], lhsT=onesb[:], rhs=fl(mask2)[:, hh * 384:(hh + 1) * 384],
                                                 start=True, stop=True), reads=["mask2"], writes=["ps:%d" % (6 + hh)])
            P.op("act", lambda e, hh=hh: e.copy(out=fl(totsb)[:, hh * 384:(hh + 1) * 384], in_=bank(6 + hh)[:, 0:384]),
                 reads=["ps:%d" % (6 + hh)], writes=["totsb"])
        P.op("dve", lambda e: e.memset(base[:, 0, :], 0.0), writes=["base"])
        for t in range(1, NT):
            P.op("dve", lambda e, t=t: e.tensor_tensor(out=base[:, t, :], in0=base[:, t - 1, :], in1=totsb[:, t - 1, :],
                                                      op=ALU.add), reads=["base", "totsb"], writes=["base"])
        for hh in range(2):
            P.op("dve", lambda e, hh=hh: e.tensor_tensor(out=fl(pos)[:, hh * 384:(hh + 1) * 384], in0=bank(4 + hh)[:, 0:384],
                                                        in1=fl(base)[:, hh * 384:(hh + 1) * 384], op=ALU.add),
                 reads=["ps:%d" % (4 + hh), "base"], writes=["pos"])
        P.op("dve", lambda e: e.scalar_tensor_tensor(out=fl(pos), in0=fl(pos), scalar=1.0, in1=fl(mask2f),
                                                     op0=ALU.add, op1=ALU.mult), reads=["pos", "mask2f"], writes=["pos"])
        P.op("dve", lambda e: e.tensor_scalar(out=fl(posm[:]), in0=fl(pos), scalar1=-1.0, scalar2=None, op0=ALU.add),
             reads=["pos"], writes=["posm"])
        for t in range(NT):
            P.op("pe", lambda e, t=t: e.transpose(out=pp[2 + t // 8][0:48, (t % 8) * 128:(t % 8) * 128 + 128],
                                                  in_=posm[:, t, :], identity=identf[:]),
                 reads=["posm"], writes=BP(2 + t // 8))
        for hh in range(2):
            P.op("act", lambda e, hh=hh: e.copy(out=posmT[:, hh * 1024:(hh + 1) * 1024], in_=pp[2 + hh][0:48, :]),
                 reads=BP(2 + hh), writes=["posmT"])
        P.op("dve", lambda e: e.tensor_copy(out=fl(gh), in_=fl(gate2)), reads=["gate2"], writes=["gh"])
        P.op("dve", lambda e: e.tensor_copy(out=fl(ghf), in_=fl(gh)), reads=["gh"], writes=["ghf"])
        P.op("dve", lambda e: e.tensor_tensor(out=ghl[:, :, :, 1], in0=gate2, in1=ghf, op=ALU.subtract),
             reads=["gate2", "ghf"], writes=["ghl1"])
        P.op("dve", lambda e: e.tensor_copy(out=ghl[:, :, :, 0], in_=gh), reads=["gh"], writes=["ghl0"])

    def epoch_e():
        cv = Carver()
        ring = [cv.get([128, 8, 512], BF16) for _ in range(8)]
        S = [cv.get([128, NT, 256], BF16) for _ in range(2)]
        xeT = cv.get([128, 8, 512], BF16)
        hidT = cv.get([128, 16, 512], BF16)
        sil = [cv.get([128, 512], F32) for _ in range(2)]
        ye_sb = cv.get([128, 4, D], BF16)
        pgs = cv.get([128, 2, 16], F32)
        idxf = cv.get([128, 2, 4], F32)
        gsb = cv.get([128, 2, 4], F32)
        xetok = [cv.get([128, 4, D], BF16) for _ in range(2)]
        d_yes = [P.dsem("yes0"), P.dsem("yes1")]
        d_g = [P.dsem("gath0"), P.dsem("gath1")]
        NRING = 8
        uspec = []
        for e2 in range(NE):
            for g in range(4):
                uspec.append(("a", w1[e2][:, g * 512:(g + 1) * 512].rearrange("(k p) f -> p k f", p=128)))
                uspec.append(("a", w3[e2][:, g * 512:(g + 1) * 512].rearrange("(k p) f -> p k f", p=128)))
            for dq in range(4):
                uspec.append(("b", [w2[e2][hf * 1024:(hf + 1) * 1024, dq * 256:(dq + 1) * 256]
                                    .rearrange("(c p) d -> p c d", p=128) for hf in range(2)]))
        issued = [0]

        def uview(u):
            s_ = u % NRING
            if uspec[u][0] == "a":
                return ring[s_], "ring%d" % s_
            return ring[s_].rearrange("p a b -> p (a b)").rearrange("p (c d) -> p c d", c=16), "ring%d" % s_

        def ensure_issued(upto):
            upto = min(upto, len(uspec) - 1)
            while issued[0] <= upto:
                u = issued[0]
                v, key = uview(u)
                if uspec[u][0] == "a":
                    load("pool", v, uspec[u][1], key)
                else:
                    for hf in range(2):
                        load("pool", v[:, hf * 8:(hf + 1) * 8, :], uspec[u][1][hf], key)
                issued[0] += 1

        def prep(e_):
            par = e_ % 2
            for b in range(2):
                col = 32 * b + e_
                for t in range(NT):
                    P.op("dve", lambda e, b=b, t=t, col=col: e.tensor_scalar(out=S[b][:, t, :], in0=iota256[:],
                                                                           scalar1=posm[:, t, col:col + 1], scalar2=None,
                                                                           op0=ALU.is_equal),
                         reads=[], writes=["S%d" % b])
            pg = bank(5)[:, 256:272]
            for b in range(2):
                col = 32 * b + e_
                for half in range(2):
                    gi = b * 2 + half
                    for t in range(NT):
                        P.op("pe", lambda e, b=b, t=t, half=half, gi=gi, col=col: e.matmul(
                            pg[:, gi * 4:gi * 4 + 4], lhsT=S[b][:, t, half * 128:(half + 1) * 128],
                            rhs=ghl[:, t, col, :], start=(t == 0), stop=(t == NT - 1)),
                             reads=["S%d" % b], writes=["ps:5"])
            P.op("dve", lambda e: e.tensor_copy(out=pgs[:, par, :], in_=pg), reads=["ps:5"], writes=["pgs%d" % par])
            pv4 = pgs[:, par, :].rearrange("p (a b) -> p a b", a=4)
            P.op("dve", lambda e: e.tensor_tensor(out=gsb[:, par, :], in0=pv4[:, :, 0], in1=pv4[:, :, 1], op=ALU.add),
                 reads=["pgs%d" % par], writes=["gs%d" % par])
            P.op("dve", lambda e: e.scalar_tensor_tensor(out=idxf[:, par, :], in0=pv4[:, :, 2], scalar=256.0,
                                                         in1=pv4[:, :, 3], op0=ALU.mult, op1=ALU.add),
                 reads=["pgs%d" % par], writes=["idxf%d" % par])
            P.op("dve", lambda e: e.tensor_copy(out=idxi[:, par * 4:par * 4 + 4], in_=idxf[:, par, :]),
                 reads=["idxf%d" % par], writes=["idxi%d" % par])
            for gi in range(4):
                P.dma("pool", lambda e, gi=gi: e.indirect_dma_start(
                    out=xetok[par][:, gi, :], out_offset=None, in_=h2s[:, :],
                    in_offset=bass.IndirectOffsetOnAxis(ap=idxi[:, par * 4 + gi:par * 4 + gi + 1], axis=0)),
                      d_g[par], reads=["idxi%d" % par], writes=["xetok%d" % par])

        import os
        ne_dbg = int(os.environ.get("K_NE", NE))
        prep(0)
        for e_ in range(ne_dbg):
            par = e_ % 2
            gs = gsb[:, par, :]
            for gi in range(4):
                pT = bankb(6 + gi % 2)
                for k in range(8):
                    P.op("pe", lambda e, gi=gi, k=k, pT=pT, par=par: e.transpose(out=pT[:, k * 128:(k + 1) * 128],
                                                                        in_=xetok[par][:, gi, k * 128:(k + 1) * 128],
                                                                        identity=identb[:]),
                         reads=["xetok%d" % par], writes=["ps:%d" % (6 + gi % 2)])
                P.op("act", lambda e, gi=gi, pT=pT: e.copy(out=xeT[:, :, gi * 128:(gi + 1) * 128],
                                                           in_=pT.rearrange("p (a b) -> p a b", a=8)),
                     reads=["ps:%d" % (6 + gi % 2)], writes=["xeT"])
            for g in range(4):
                ua = 12 * e_ + 2 * g
                ensure_issued(ua + 1)
                wa, ka = uview(ua)
                wb, kb = uview(ua + 1)
                for mm in range(4):
                    m = g * 4 + mm
                    ph1 = bank(m % 2)
                    ph3 = bank(2 + m % 2)
                    for k in range(8):
                        P.op("pe", lambda e, k=k, mm=mm, wa=wa, ph1=ph1: e.matmul(
                            ph1, lhsT=wa[:, k, mm * 128:(mm + 1) * 128], rhs=xeT[:, k, :], start=(k == 0), stop=(k == 7)),
                             reads=[ka, "xeT"], writes=["ps:%d" % (m % 2)])
                    for k in range(8):
                        P.op("pe", lambda e, k=k, mm=mm, wb=wb, ph3=ph3: e.matmul(
                            ph3, lhsT=wb[:, k, mm * 128:(mm + 1) * 128], rhs=xeT[:, k, :], start=(k == 0), stop=(k == 7)),
                             reads=[kb, "xeT"], writes=["ps:%d" % (2 + m % 2)])
                    P.op("act", lambda e, m=m, ph1=ph1: e.activation(out=sil[m % 2], in_=ph1, func=AF.Silu),
                         reads=["ps:%d" % (m % 2)], writes=["sil%d" % (m % 2)])
                    P.op("dve", lambda e, m=m, ph3=ph3: e.tensor_tensor(out=hidT[:, m, :], in0=sil[m % 2], in1=ph3,
                                                                        op=ALU.mult),
                         reads=["sil%d" % (m % 2), "ps:%d" % (2 + m % 2)], writes=["hidT"])
                ensure_issued(min(ua + 1 + NRING, 12 * e_ + 11))
            ensure_issued(12 * e_ + 11)
            if e_ + 1 < ne_dbg:
                prep(e_ + 1)
            for dq in range(4):
                uw = 12 * e_ + 8 + dq
                wv, kw = uview(uw)
                for sc in range(4):
                    pye = bank(4 + sc % 2)[:, 0:256]
                    for m in range(16):
                        P.op("pe", lambda e, m=m, sc=sc, wv=wv, pye=pye: e.matmul(
                            pye, lhsT=hidT[:, m, sc * 128:(sc + 1) * 128], rhs=wv[:, m, :], start=(m == 0), stop=(m == 15)),
                             reads=[kw, "hidT"], writes=["ps:%d" % (4 + sc % 2)])
                    P.op("dve", lambda e, sc=sc, dq=dq, pye=pye, gs=gs: e.tensor_scalar(
                        out=ye_sb[:, sc, dq * 256:(dq + 1) * 256], in0=pye, scalar1=gs[:, sc:sc + 1], scalar2=None,
                        op0=ALU.mult), reads=["ps:%d" % (4 + sc % 2), "gs%d" % par], writes=["ye_sb"])
                ensure_issued(uw + NRING)
            for b in range(2):
                P.dma("sync", lambda e, b=b, e_=e_: e.dma_start(out=yes[b, e_].rearrange("h p d -> p h d"),
                                                               in_=ye_sb[:, 2 * b:2 * b + 2, :]),
                      d_yes[b], reads=["ye_sb"], writes=["yes%d" % b])

    def epoch_f(b):
        cv = Carver()
        yall = cv.get([128, 32, D], BF16)
        ST = [cv.get([128, 2, 256], BF16) for _ in range(2)]
        x1t = [cv.get([128, D], F32) for _ in range(2)]
        outt = [cv.get([128, D], F32) for _ in range(2)]
        gt2bc = cv.get([128, D], F32)
        tmp = cv.get([128, D], F32)
        sqj = cv.get([128, D], BF16)
        for q_ in range(4):
            load("sync", yall[:, q_ * 8:(q_ + 1) * 8, :], yes[b, q_ * 4:(q_ + 1) * 4].rearrange("e h p d -> p (e h) d"),
                 "yall%d" % q_)
        for hh in range(2):
            P.op("pe", lambda e, hh=hh: e.matmul(bank(6), lhsT=sel3[:, b, :], rhs=msb[:, 3, hh * 512:(hh + 1) * 512],
                                                 start=True, stop=True), reads=[], writes=["ps:6"])
            P.op("act", lambda e, hh=hh: e.copy(out=gt2bc[:, hh * 512:(hh + 1) * 512], in_=bank(6)),
                 reads=["ps:6"], writes=["gt2bc"])
        def bc_step(tg, e_):
            pbc = bank(4 + e_ % 2)[:, 0:256]
            P.op("pe", lambda e: e.matmul(pbc, lhsT=sel48[:, 16 * b + e_, :],
                                          rhs=posmT[:, tg * 256:(tg + 1) * 256], start=True, stop=True),
                 reads=[], writes=["ps:%d" % (4 + e_ % 2)])

        bc_step(0, 0)
        for tg in range(8):
            for e_ in range(NE):
                pbc = bank(4 + e_ % 2)[:, 0:256]
                bk = "ps:%d" % (4 + e_ % 2)
                stv = ST[e_ % 2]
                sk = "ST%d" % (e_ % 2)
                for half in range(2):
                    P.op("dve", lambda e, half=half, pbc=pbc, stv=stv: e.tensor_scalar(
                        out=stv[:, half, :], in0=pbc, scalar1=iotap[:, half:half + 1], scalar2=None, op0=ALU.is_equal),
                         reads=[bk], writes=[sk])
                if e_ + 1 < NE:
                    bc_step(tg, e_ + 1)
                elif tg + 1 < 8:
                    bc_step(tg + 1, 0)
                for tl in range(2):
                    for dh in range(2):
                        for half in range(2):
                            P.op("pe", lambda e, e_=e_, tl=tl, dh=dh, half=half, stv=stv: e.matmul(
                                pp[tl][:, dh * 512:(dh + 1) * 512], lhsT=stv[:, half, tl * 128:(tl + 1) * 128],
                                rhs=yall[:, e_ * 2 + half, dh * 512:(dh + 1) * 512],
                                start=(e_ == 0 and half == 0), stop=(e_ == NE - 1 and half == 1)),
                                 reads=[sk, "yall%d" % (e_ // 4)], writes=BP(tl))
            for tl in range(2):
                t = tg * 2 + tl
                xb = x1t[t % 2]
                xk = "x1t%d" % (t % 2)
                ob = outt[t % 2]
                okk = "outt%d" % (t % 2)
                load("sync", xb, x1s[b, t * 128:(t + 1) * 128, :], xk)
                P.op("dve", lambda e, tl=tl: e.tensor_tensor(out=tmp, in0=pp[tl][:], in1=gt2bc, op=ALU.mult),
                     reads=BP(tl) + ["gt2bc"], writes=["tmp"])
                P.op("pool", lambda e, xb=xb: e.tensor_tensor(out=xb, in0=xb, in1=tmp, op=ALU.add),
                     reads=["tmp", xk], writes=[xk])
                P.op("act", lambda e, xb=xb: e.activation(out=sqj, in_=xb, func=AF.Square, accum_out=st[:, 0:1]),
                     reads=[xk], writes=["sqj", "st0"])
                rstd_chain(0, 1.0 / D)
                P.op("dve", lambda e, xb=xb, ob=ob: e.scalar_tensor_tensor(out=ob, in0=xb, scalar=st[:, 1:2], in1=gfinbc[:],
                                                                          op0=ALU.mult, op1=ALU.mult),
                     reads=[xk, "st1"], writes=[okk])
                P.dma("sync", lambda e, ob=ob, t=t: e.dma_start(out=y[b, t * 128:(t + 1) * 128, :], in_=ob),
                      d_outs[t % 2], reads=[okk])

    if stage >= 3:
        epoch_d()
        P.barrier_all()
        if stage == 25:
            P.final_wait("sync", d_outs)
            P.emit()
            return nc
        epoch_e()
        P.barrier_all()
        if stage == 26:
            P.final_wait("sync", d_outs)
            P.emit()
            return nc
        for b in range(2):
            epoch_f(b)
            P.barrier_all()

    P.final_wait("sync", d_outs)
    P.emit()
    return nc


def build_moe(nc, P, env):
    raise NotImplementedError


_STAGE = 3


def kernel(x, c, ctx, c_ctx, w_mod, b_mod, g_mix, g_ffn, w_in, q_gain, k_gain, v_gain,
           w_s, b_s, w_out, w_router, w1, w3, w2, g_final):
    f = lambda a: np.ascontiguousarray(np.asarray(a, dtype=np.float32))
    x, c, ctx, c_ctx = f(x), f(c), f(ctx), f(c_ctx)
    consts = _consts()
    rows = np.concatenate([f(g_ffn)[0], f(g_final), np.tile(f(q_gain)[0], 8), np.tile(f(k_gain)[0], 2),
                           f(v_gain)[0]])
    assert rows.shape[0] == 3200
    shared = {
        "w_mod": f(w_mod)[0], "bmod3": np.tile(f(b_mod)[0][None, :], (3, 1)),
        "rows3": np.tile(rows[None, :], (3, 1)),
        "g2": np.concatenate([f(g_mix)[0].reshape(8, 128), f(g_ffn)[0].reshape(8, 128)], axis=0),
        "w_in": f(w_in)[0], "w_s": f(w_s)[0], "b_s": f(b_s)[0], "w_out": f(w_out)[0],
        "w_router": f(w_router)[0], "w1": f(w1)[0], "w3": f(w3)[0], "w2": f(w2)[0],
    }
    shared.update(consts)
    in_maps = []
    for i in range(NCORES):
        m = dict(shared)
        m["x"] = x[2 * i:2 * i + 2]
        m["ctx"] = ctx[2 * i:2 * i + 2]
        m["c3"] = np.concatenate([c[2 * i:2 * i + 2], c_ctx[None, :]], axis=0)
        in_maps.append(m)
    nc = build_nc(_STAGE)
    res = run_bass_kernel_spmd(nc, in_maps, core_ids=list(range(NCORES)))
    return np.concatenate([r["y"] for r in res.results], axis=0)
```

```python
import numpy as np
import concourse.bass as bass
import concourse.mybir as mybir
from concourse.bass_utils import run_bass_kernel_spmd
from contextlib import ExitStack

F32 = mybir.dt.float32
BF16 = mybir.dt.bfloat16
U8 = mybir.dt.uint8
ALU = mybir.AluOpType
AF = mybir.ActivationFunctionType
AX = mybir.AxisListType

COMPUTE = ("pe", "act", "dve", "pool")
POOL_TO_DVE = True
CARVE_LOG = []
EPS = 1e-6
NCORES = 8
T = 2048
NT = 16
D = 1024
NE = 16
CAP = 256


class DSem:
    def __init__(self, name):
        self.name = name
        self.sem = None
        self.count = 0


class Ins:
    __slots__ = ("eng", "fn", "waits", "flag", "idx", "dsem")

    def __init__(self, eng, fn):
        self.eng = eng
        self.fn = fn
        self.waits = []
        self.flag = False
        self.idx = None
        self.dsem = None


class Prog:
    def __init__(self, nc):
        self.nc = nc
        self.q = {e: [] for e in COMPUTE + ("sync",)}
        self.last_w = {}
        self.readers = {}
        self.waited_c = {e: {b: -1 for b in COMPUTE} for e in self.q}
        self.waited_d = {e: {} for e in self.q}
        self.dsems = []

    def dsem(self, name):
        d = DSem(name)
        self.dsems.append(d)
        return d

    def _add_wait(self, ins, dep):
        q = ins.eng
        if dep[0] == "c":
            _, b, idx = dep
            if b == "pe" and q == "pe":
                return
            while idx >= 0 and (self.q[b][idx].fn is None or self.q[b][idx].dsem is not None):
                idx -= 1
            if idx < 0 or self.waited_c[q][b] >= idx:
                return
            self.waited_c[q][b] = idx
            self.q[b][idx].flag = True
            ins.waits.append(("c", b, idx))
        else:
            _, d, cnt = dep
            if self.waited_d[q].get(id(d), 0) >= cnt:
                return
            self.waited_d[q][id(d)] = cnt
            ins.waits.append(dep)

    def _deps(self, ins, reads, writes, token):
        ex = [r for r in reads if r.startswith("ps:")]
        if ex:
            reads = [r for r in reads if not r.startswith("ps:")]
            writes = list(writes) + ex
        for r in reads:
            w = self.last_w.get(r)
            if w is not None:
                self._add_wait(ins, w)
        for w_ in writes:
            w = self.last_w.get(w_)
            if w is not None:
                self._add_wait(ins, w)
            for rd in self.readers.get(w_, {}).values():
                self._add_wait(ins, rd)
        key = token[1] if token[0] == "c" else id(token[1])
        for r in reads:
            self.readers.setdefault(r, {})[key] = token
        for w_ in writes:
            self.last_w[w_] = token
            self.readers[w_] = {}

    def op(self, eng, fn, reads=(), writes=()):
        if eng == "pool!":
            eng = "pool"
        elif eng == "pool" and POOL_TO_DVE:
            eng = "dve"
        ins = Ins(eng, fn)
        ins.idx = len(self.q[eng])
        self._deps(ins, reads, writes, ("c", eng, ins.idx))
        self.q[eng].append(ins)
        return ins

    def dma(self, queue, fn, dsem, reads=(), writes=()):
        ins = Ins(queue, fn)
        ins.idx = len(self.q[queue])
        dsem.count += 16
        ins.dsem = dsem
        self._deps(ins, reads, writes, ("d", dsem, dsem.count))
        self.q[queue].append(ins)
        return ins

    def barrier_all(self):
        for q in self.q:
            ins = Ins(q, None)
            ins.idx = len(self.q[q])
            for b in COMPUTE:
                j = len(self.q[b]) - 1
                if j >= 0 and b != q:
                    self._add_wait(ins, ("c", b, j))
            for d in self.dsems:
                if d.count > 0:
                    self._add_wait(ins, ("d", d, d.count))
            self.q[q].append(ins)
        self.last_w = {}
        self.readers = {}

    def final_wait(self, queue, dsems):
        ins = Ins(queue, None)
        ins.idx = len(self.q[queue])
        for d in dsems:
            if d.count > 0:
                self._add_wait(ins, ("d", d, d.count))
        self.q[queue].append(ins)

    def emit(self):
        nc = self.nc
        with ExitStack() as st:
            csem = {e: st.enter_context(nc.semaphore("cs_" + e)) for e in COMPUTE}
            for d in self.dsems:
                if d.count > 0:
                    d.sem = st.enter_context(nc.semaphore("ds_" + d.name))
            mile = {}
            for e in COMPUTE:
                c = 0
                arr = []
                for ins in self.q[e]:
                    if ins.flag:
                        c += 1
                    arr.append(c)
                mile[e] = arr
            block = st.enter_context(nc.Block())

            def run(qname, eng):
                for ins in self.q[qname]:
                    for dep in ins.waits:
                        if dep[0] == "c":
                            eng.wait_ge(csem[dep[1]], mile[dep[1]][dep[2]])
                        else:
                            eng.wait_ge(dep[1].sem, dep[2])
                    if ins.fn is None:
                        continue
                    r = ins.fn(eng)
                    if ins.dsem is not None:
                        r.then_inc(ins.dsem.sem, 16)
                    elif ins.flag:
                        r.then_inc(csem[qname], 1)

            @block.sync
            def _(sync):
                run("sync", sync)

            @block.scalar
            def _(scalar):
                run("act", scalar)

            @block.vector
            def _(vector):
                run("dve", vector)

            @block.gpsimd
            def _(gpsimd):
                run("pool", gpsimd)

            @block.tensor
            def _(tensor):
                run("pe", tensor)


def _consts():
    c = {}
    c["identf"] = np.eye(128, dtype=np.float32)
    k = np.arange(128)
    c["tri"] = (k[:, None] < k[None, :]).astype(np.float32)
    c["iota256"] = np.tile(np.arange(256, dtype=np.float32)[None, :], (128, 1))
    c["iotap"] = np.stack([k, k + 128], axis=1).astype(np.float32)
    sel48 = np.zeros((48, 32, 128), np.float32)
    for b in range(2):
        for e in range(16):
            sel48[32 * b + e, 16 * b + e, :] = 1.0
    c["sel48"] = sel48.reshape(48, 32 * 128)
    sel3 = np.zeros((3, 3, 128), np.float32)
    for j in range(3):
        sel3[j, j, :] = 1.0
    c["sel3"] = sel3.reshape(3, 384)
    nf = 16
    inv = (10000.0 ** (-np.arange(nf, dtype=np.float32) / nf)).astype(np.float32)
    tok = np.arange(T)
    row = (tok // 64).astype(np.float32)
    col = (tok % 64).astype(np.float32)
    ang = np.concatenate([row[:, None] * inv[None, :], col[:, None] * inv[None, :]], axis=-1).astype(np.float32)
    c["cs"] = np.concatenate([np.cos(ang), np.sin(ang)], axis=-1).astype(np.float32)
    tokc = np.zeros((128, NT, 48, 2), np.float32)
    for t in range(NT):
        for col in range(48):
            bb = col // 32
            tokc[:, t, col, 0] = bb * 8 + t // 2
            tokc[:, t, col, 1] = (t % 2) * 128 + np.arange(128)
    c["tokc"] = tokc.reshape(128, NT * 48 * 2)
    return c


def build_nc(stage=9):
    nc = bass.Bass("TRN2", target_bir_lowering=False)
    P = Prog(nc)

    def din(name, shape):
        return nc.dram_tensor(name, list(shape), F32, kind="ExternalInput").ap()

    x = din("x", [2, T, D])
    ctx = din("ctx", [2, 256, D])
    c3 = din("c3", [3, D])
    w_mod = din("w_mod", [D, 6 * D])
    bmod3 = din("bmod3", [3, 6 * D])
    rows3 = din("rows3", [3, 3200])
    g2 = din("g2", [16, 128])
    w_in = din("w_in", [D, 1792])
    w_s = din("w_s", [8, 128, 128])
    b_s = din("b_s", [8, 128])
    w_out = din("w_out", [D, D])
    w_router = din("w_router", [D, NE])
    w1 = din("w1", [NE, D, 2048])
    w3 = din("w3", [NE, D, 2048])
    w2 = din("w2", [NE, 2048, D])
    k_identf = din("identf", [128, 128])
    k_tri = din("tri", [128, 128])
    k_iota256 = din("iota256", [128, 256])
    k_iotap = din("iotap", [128, 2])
    k_sel48 = din("sel48", [48, 4096])
    k_sel3 = din("sel3", [3, 384])
    k_cs = din("cs", [T, 64])
    y = nc.dram_tensor("y", [2, T, D], F32, kind="ExternalOutput").ap()
    x1s = nc.dram_tensor("x1s", [2, T, D], F32, kind="Internal").ap()
    yes = nc.dram_tensor("yes", [2, NE, 2, 128, D], BF16, kind="Internal").ap()
    h2s = nc.dram_tensor("h2s", [2 * T, D], BF16, kind="Internal").ap()
    k_tokc = din("tokc", [128, NT * 48 * 2])

    def sb(name, shape, dt):
        return nc.alloc_sbuf_tensor(name, list(shape), dt)

    identb = sb("identb", [128, 128], BF16)
    identf = sb("identf_sb", [128, 128], F32)
    trib = sb("trib", [128, 128], BF16)
    onesb = sb("onesb", [128, 128], BF16)
    iota256 = sb("iota256_sb", [128, 256], F32)
    iotap = sb("iotap_sb", [128, 2], F32)
    sel48 = sb("sel48_sb", [48, 32, 128], BF16)
    sel3 = sb("sel3_sb", [3, 3, 128], F32)
    cs = sb("cs_sb", [128, NT, 64], F32)
    gain640 = sb("gain640", [128, 640], F32)
    vgainbc = sb("vgainbc", [128, 512], F32)
    gfinbc = sb("gfinbc", [128, D], F32)
    wsT = sb("wsT", [128, 8, 128], BF16)
    bsT = sb("bsT", [128, 8], F32)
    wr = sb("wr", [128, 8, NE], BF16)
    scT = sb("scT", [128, 8, 3], BF16)
    mT = sb("mT", [128, 48, 3], F32)
    gT = sb("gT", [128, 16], F32)
    s1T = sb("s1T", [128, 8, 3], F32)
    msb = sb("msb", [3, 4, D], F32)
    posm = sb("posm", [128, NT, 48], F32)
    ghl = sb("ghl", [128, NT, 48, 4], BF16)
    idxi = sb("idxi", [128, 8], mybir.dt.int32)
    posmT = sb("posmT", [48, T], BF16)
    st = sb("st", [128, 64], F32)
    epsb = sb("epsb", [128, 1], F32)

    RBYTES = 150 * 1024
    R = sb("R", [128, RBYTES], U8)

    class Carver:
        def __init__(self):
            self.off = 0
            CARVE_LOG.append([])

        def get(self, shape, dt, parts=128):
            es = 2 if dt == BF16 else 4
            n = int(np.prod(shape[1:]))
            nb = n * es
            off = (self.off + 63) // 64 * 64
            assert off + nb <= RBYTES, (off, nb)
            v = R[0:parts, off:off + nb].bitcast(dt)
            CARVE_LOG[-1].append((off, tuple(shape), "bf16" if dt == BF16 else "f32"))
            self.off = off + nb
            if len(shape) == 3:
                v = v.rearrange("p (a b) -> p a b", a=shape[1])
            elif len(shape) == 4:
                v = v.rearrange("p (a b c) -> p a b c", a=shape[1], b=shape[2])
            return v

    pp = [nc.alloc_psum_tensor("pp%d" % i, [128, 1024], F32) for i in range(4)]

    def bank(i):
        return pp[i // 2][:, (i % 2) * 512:(i % 2) * 512 + 512]

    def BP(j):
        return ["ps:%d" % (2 * j), "ps:%d" % (2 * j + 1)]

    def bankb(i):
        return bank(i).bitcast(BF16)

    dcache = {}

    def dget(key):
        if key not in dcache:
            dcache[key] = P.dsem("d%d" % len(dcache))
        return dcache[key]

    def load(queue, dst, src, key, reads=()):
        P.dma(queue, lambda e: e.dma_start(out=dst, in_=src), dget(key), reads=reads, writes=[key])

    d_outs = [P.dsem("out0"), P.dsem("out1")]

    cv = Carver()
    c3_sb = cv.get([3, D], F32, parts=3)
    sc3 = cv.get([3, D], F32, parts=3)
    rows3_sb = cv.get([3, 3200], F32, parts=3)
    bmod_sb = cv.get([3, 6 * D], F32, parts=3)
    wm = [cv.get([128, 8, 512], BF16) for _ in range(3)]
    wstmp = cv.get([128, 8, 128], F32)
    g2_sb = cv.get([16, 128], F32, parts=16)
    bs_sb = cv.get([8, 128], F32, parts=8)
    mtmp = [cv.get([3, 512], F32, parts=3) for _ in range(2)]
    trif = cv.get([128, 128], F32)
    sel48f = cv.get([48, 4096], F32, parts=48)

    load("sync", identf[:], k_identf, "identf")
    load("pool", identb[:], k_identf, "identb")
    load("pool", trib[:], k_tri, "trib")
    load("sync", iota256[:], k_iota256, "iota256")
    load("sync", iotap[:], k_iotap, "iotap")
    load("pool", sel48[:].rearrange("p a b -> p (a b)"), k_sel48, "sel48")
    load("sync", sel3[:].rearrange("p a b -> p (a b)"), k_sel3, "sel3")
    load("sync", cs[:], k_cs.rearrange("(i p) c -> p i c", p=128), "cs")
    load("sync", c3_sb, c3, "c3")
    load("sync", rows3_sb, rows3, "rows3")
    load("sync", bmod_sb, bmod3, "bmod")
    load("sync", g2_sb, g2, "g2")
    load("sync", bs_sb, b_s, "bs")
    load("sync", wstmp, w_s.rearrange("g i j -> i g j"), "wstmp")
    load("pool", wr[:], w_router.rearrange("(k p) e -> p k e", p=128), "wr")
    P.op("pool!", lambda e: e.memset(onesb[:], 1.0), writes=["onesb"])
    P.op("pool!", lambda e: e.memset(epsb[:], EPS), writes=["epsb"])

    P.op("act", lambda e: e.activation(out=sc3, in_=c3_sb, func=AF.Silu), reads=["c3"], writes=["sc3"])
    for k in range(8):
        P.op("pe", lambda e, k=k: e.transpose(out=bank(0)[:, k * 3:k * 3 + 3], in_=sc3[:, k * 128:(k + 1) * 128],
                                              identity=identf[0:3, 0:3]), reads=["sc3", "identf"], writes=["ps:0"])
    P.op("dve", lambda e: e.tensor_copy(out=scT[:].rearrange("p a b -> p (a b)"), in_=bank(0)[:, 0:24]),
         reads=["ps:0"], writes=["scT"])

    psT = bank(3)
    for n in range(12):
        seg, hh = n // 2, n % 2
        wmb = wm[n % 3]
        load("pool", wmb, w_mod[:, n * 512:(n + 1) * 512].rearrange("(k p) n -> p k n", p=128), "wm%d" % (n % 3))
        pm = bank(1 + n % 2)
        pk = "ps:%d" % (1 + n % 2)
        for k in range(8):
            P.op("pe", lambda e, k=k, wmb=wmb, pm=pm: e.matmul(pm[0:3, :], lhsT=scT[:, k, :], rhs=wmb[:, k, :],
                                                               start=(k == 0), stop=(k == 7)),
                 reads=["scT", "wm%d" % (n % 3)], writes=[pk])
        mt = mtmp[n % 2]
        mk = "mtmp%d" % (n % 2)
        P.op("dve", lambda e, pm=pm, mt=mt, n=n: e.tensor_tensor(out=mt, in0=pm[0:3, :],
                                                                in1=bmod_sb[:, n * 512:(n + 1) * 512], op=ALU.add),
             reads=[pk, "bmod"], writes=[mk])
        for j in range(4):
            cidx = (n * 4 + j) * 3
            P.op("pe", lambda e, mt=mt, j=j, cidx=cidx: e.transpose(out=psT[:, cidx:cidx + 3],
                                                                   in_=mt[:, j * 128:(j + 1) * 128],
                                                                   identity=identf[0:3, 0:3]),
                 reads=[mk, "identf"], writes=["ps:3"])
        if seg == 2:
            P.op("act", lambda e, mt=mt, hh=hh: e.copy(out=msb[:, 0, hh * 512:(hh + 1) * 512], in_=mt),
                 reads=[mk], writes=["msb"])
        elif seg == 3:
            P.op("act", lambda e, mt=mt, hh=hh: e.copy(out=msb[:, 2, hh * 512:(hh + 1) * 512], in_=mt),
                 reads=[mk], writes=["msb"])
        elif seg == 5:
            P.op("act", lambda e, mt=mt, hh=hh: e.copy(out=msb[:, 3, hh * 512:(hh + 1) * 512], in_=mt),
                 reads=[mk], writes=["msb"])
        elif seg == 4:
            P.op("dve", lambda e, mt=mt, hh=hh: e.scalar_tensor_tensor(
                out=msb[:, 1, hh * 512:(hh + 1) * 512], in0=mt, scalar=1.0,
                in1=rows3_sb[:, hh * 512:(hh + 1) * 512], op0=ALU.add, op1=ALU.mult),
                 reads=[mk, "rows3"], writes=["msb"])
    P.op("dve", lambda e: e.tensor_copy(out=mT[:].rearrange("p a b -> p (a b)"), in_=psT[:, 0:144]),
         reads=["ps:3"], writes=["mT"])
    P.op("pe", lambda e: e.transpose(out=bank(0)[:, 32:48], in_=g2_sb, identity=identf[0:16, 0:16]),
         reads=["g2", "identf"], writes=["ps:0"])
    P.op("dve", lambda e: e.tensor_copy(out=gT[:], in_=bank(0)[:, 32:48]), reads=["ps:0"], writes=["gT"])
    P.op("dve", lambda e: e.scalar_tensor_tensor(out=s1T[:], in0=mT[:, 8:16, :], scalar=1.0,
                                                 in1=gT[:, 0:8].unsqueeze(2).to_broadcast([128, 8, 3]),
                                                 op0=ALU.add, op1=ALU.mult),
         reads=["mT", "gT"], writes=["s1T"])
    for (dst, c0, n_) in ((gfinbc[:, 0:512], 1024, 512), (gfinbc[:, 512:1024], 1536, 512),
                          (gain640[:, 0:512], 2048, 512), (gain640[:, 512:640], 2560, 128),
                          (vgainbc[:, 0:512], 2688, 512)):
        P.op("pe", lambda e, c0=c0, n_=n_: e.matmul(bank(4)[:, 0:n_], lhsT=sel3[:, 0, :], rhs=rows3_sb[:, c0:c0 + n_],
                                                    start=True, stop=True), reads=["sel3", "rows3"], writes=["ps:4"])
        P.op("act", lambda e, dst=dst, n_=n_: e.copy(out=dst, in_=bank(4)[:, 0:n_]), reads=["ps:4"], writes=["bcst"])
    for g in range(8):
        P.op("pe", lambda e, g=g: e.transpose(out=bank(5 + g // 4)[:, (g % 4) * 128:(g % 4) * 128 + 128],
                                              in_=wstmp[:, g, :], identity=identf[:]),
             reads=["wstmp", "identf"], writes=["ps:%d" % (5 + g // 4)])
    for h_ in range(2):
        P.op("act", lambda e, h_=h_: e.copy(out=wsT[:, h_ * 4:(h_ + 1) * 4, :].rearrange("p a b -> p (a b)"),
                                            in_=bank(5 + h_)), reads=["ps:%d" % (5 + h_)], writes=["wsT"])
    P.op("pe", lambda e: e.transpose(out=bank(0)[:, 64:72], in_=bs_sb, identity=identf[0:8, 0:8]),
         reads=["bs", "identf", "gT"], writes=["ps:0"])
    P.op("dve", lambda e: e.tensor_copy(out=bsT[:], in_=bank(0)[:, 64:72]), reads=["ps:0"], writes=["bsT"])
    P.barrier_all()
    if stage == 0:
        P.dma("sync", lambda e: e.dma_start(out=y[0, 0:128, :], in_=gfinbc[:]), d_outs[0], reads=["bcst"])
        P.dma("sync", lambda e: e.dma_start(out=y[0, 128:256, 0:640], in_=gain640[:]), d_outs[0], reads=["bcst"])
        P.dma("sync", lambda e: e.dma_start(out=y[0, 256:384, 0:144], in_=mT[:].rearrange("p a b -> p (a b)")), d_outs[0])
        P.dma("sync", lambda e: e.dma_start(out=y[0, 384:387, :], in_=msb[:, 1, :]), d_outs[0])
        P.dma("sync", lambda e: e.dma_start(out=y[0, 512:640, 0:24], in_=s1T[:].rearrange("p a b -> p (a b)")), d_outs[0])
        P.final_wait("sync", d_outs)
        P.emit()
        return nc

    def rstd_chain(col, scale):
        P.op("act", lambda e: e.activation(out=st[:, col + 1:col + 2], in_=st[:, col:col + 1], func=AF.Sqrt,
                                           scale=scale, bias=epsb[:, 0:1]),
             reads=["st%d" % col, "epsb"], writes=["st%d" % (col + 1)])
        P.op("dve", lambda e: e.reciprocal(out=st[:, col + 1:col + 2], in_=st[:, col + 1:col + 2]),
             reads=["st%d" % (col + 1)], writes=["st%d" % (col + 1)])

    def epoch_ac(b):
        cv = Carver()
        wio = cv.get([128, 8, 1792], BF16)
        qa = cv.get([128, 4, T], BF16)
        gmT = cv.get([128, 4, T], BF16)
        kT = cv.get([128, 2, 2304], BF16)
        vaug = cv.get([128, 18, 2, 192], BF16)
        xt = [cv.get([128, D], F32) for _ in range(2)]
        sqj = cv.get([128, D], BF16)
        xn = cv.get([128, D], BF16)
        hTt = cv.get([128, 8, 128], BF16)
        qk = cv.get([128, 10, 64], F32)
        qk2 = cv.get([128, 10, 64], F32)
        qkr = cv.get([128, 10, 64], BF16)
        kd = cv.get([128, 4, 64], BF16)
        rp = cv.get([128, 4, 320], F32)
        u_sb = cv.get([128, 512], F32)
        vv_sb = cv.get([128, 512], F32)
        vvn = cv.get([128, 512], BF16)
        gtmp = cv.get([128, 512], F32)
        gmb = cv.get([128, 512], BF16)
        PT = [cv.get([128, 1024], BF16) for _ in range(4)]
        rd = cv.get([128, 512], F32)
        gt1bc = cv.get([128, D], F32)
        tmpo = cv.get([128, D], F32)
        s10 = st[:, 16:26]
        r10 = st[:, 32:42]

        for kk in range(4):
            load("pool", wio[:, 2 * kk:2 * kk + 2, :],
                 w_in[kk * 256:(kk + 1) * 256, :].rearrange("(k p) n -> p k n", p=128), "wio")
        for hh in range(2):
            P.op("pe", lambda e, hh=hh: e.matmul(bank(0), lhsT=sel3[:, b, :], rhs=msb[:, 0, hh * 512:(hh + 1) * 512],
                                                 start=True, stop=True), reads=["sel3", "msb"], writes=["ps:0"])
            P.op("act", lambda e, hh=hh: e.copy(out=gt1bc[:, hh * 512:(hh + 1) * 512], in_=bank(0)),
                 reads=["ps:0"], writes=["gt1bc"])
        P.op("pool!", lambda e: e.memset(vaug.rearrange("p a b c -> p (a b c)"), 1.0), writes=["vaug"])

        g3 = gain640[:].rearrange("p (a b) -> p a b", a=10)
        qkf = qk.rearrange("p a b -> p (a b)")
        qkrf = qkr.rearrange("p a b -> p (a b)")
        kdf = kd.rearrange("p a b -> p (a b)")
        kdv = kd.rearrange("p (a b) c -> p a b c", a=2)
        rpv = [rp[:, i, :].rearrange("p (a b) -> p a b", a=10) for i in range(4)]
        pA = bankb(0)
        pB = bankb(5)
        pC = bankb(7)

        def X1(t):
            lat = t < 16
            xb = xt[t % 2]
            xk = "xt%d" % (t % 2)
            src = x[b, t * 128:(t + 1) * 128, :] if lat else ctx[b, (t - 16) * 128:(t - 15) * 128, :]
            load("sync", xb, src, xk)
            P.op("act", lambda e: e.activation(out=sqj, in_=xb, func=AF.Square, accum_out=st[:, 0:1]),
                 reads=[xk], writes=["sqj", "st0"])
            rstd_chain(0, 1.0 / D)
            P.op("act", lambda e: e.activation(out=xn, in_=xb, func=AF.Copy, scale=st[:, 1:2]),
                 reads=[xk, "st1"], writes=["xn"])
            for k in range(8):
                P.op("pe", lambda e, k=k: e.transpose(out=pA[:, k * 128:(k + 1) * 128], in_=xn[:, k * 128:(k + 1) * 128],
                                                      identity=identb[:]), reads=["xn", "identb"], writes=["ps:0"])

        def X2(t):
            lat = t < 16
            j = b if lat else 2
            for k in range(8):
                eng_ = "act" if k % 2 == 0 else "dve"
                if eng_ == "act":
                    P.op("act", lambda e, k=k: e.activation(out=hTt[:, k, :], in_=pA[:, k * 128:(k + 1) * 128],
                                                            func=AF.Identity, scale=s1T[:, k, j:j + 1],
                                                            bias=mT[:, k, j:j + 1]),
                         reads=["ps:0", "s1T", "mT"], writes=["hTt%d" % k])
                else:
                    P.op("dve", lambda e, k=k: e.tensor_scalar(out=hTt[:, k, :], in0=pA[:, k * 128:(k + 1) * 128],
                                                              scalar1=s1T[:, k, j:j + 1], scalar2=mT[:, k, j:j + 1],
                                                              op0=ALU.mult, op1=ALU.add),
                         reads=["ps:0", "s1T", "mT"], writes=["hTt%d" % k])
            slices = [(1, 0, 512), (2, 512, 256), (3, 768, 512), (4, 1280, 512)] if lat else [(2, 512, 256)]
            for (bi, c0, n_) in slices:
                for k in range(8):
                    P.op("pe", lambda e, bi=bi, c0=c0, n_=n_, k=k: e.matmul(bank(bi)[:, 0:n_], lhsT=hTt[:, k, :],
                                                                           rhs=wio[:, k, c0:c0 + n_],
                                                                           start=(k == 0), stop=(k == 7)),
                         reads=["hTt%d" % k, "wio"], writes=["ps:%d" % bi])

        def Ya(t):
            lat = t < 16
            if lat:
                P.op("dve", lambda e: e.tensor_copy(out=qkf[:, 0:512], in_=bank(1)), reads=["ps:1"], writes=["qk"])
            P.op("dve", lambda e: e.tensor_copy(out=qkf[:, 512:640], in_=bank(2)[:, 0:128]), reads=["ps:2"], writes=["qk"])
            P.op("act", lambda e: e.copy(out=vaug[:, t, :, 64:128],
                                         in_=bank(2)[:, 128:256].rearrange("p (a b) -> p a b", a=2)),
                 reads=["ps:2"], writes=["vaug"])
            if lat:
                P.op("act", lambda e: e.activation(out=u_sb, in_=bank(3), func=AF.Gelu), reads=["ps:3"], writes=["u_sb"])
                P.op("act", lambda e: e.activation(out=vv_sb, in_=bank(4), func=AF.Gelu), reads=["ps:4"], writes=["vv_sb"])

        def Yb(t):
            lat = t < 16
            h0 = 0 if lat else 8
            nh = 10 - h0
            if lat:
                P.op("act", lambda e: e.activation(out=gtmp, in_=vv_sb, func=AF.Square, accum_out=st[:, 4:5]),
                     reads=["vv_sb"], writes=["gtmp", "st4"])
                P.op("act", lambda e: e.activation(out=st[:, 5:6], in_=st[:, 4:5], func=AF.Sqrt, scale=1.0 / 512,
                                                   bias=epsb[:, 0:1]), reads=["st4", "epsb"], writes=["st5"])
            P.op("dve", lambda e: e.tensor_tensor(out=qk2[:, h0:10, :], in0=qk[:, h0:10, :], in1=qk[:, h0:10, :],
                                                  op=ALU.mult), reads=["qk"], writes=["qk2"])
            P.op("dve", lambda e: e.tensor_reduce(out=s10[:, h0:10], in_=qk2[:, h0:10, :], axis=AX.X, op=ALU.add),
                 reads=["qk2"], writes=["s10"])
            P.op("act", lambda e: e.activation(out=r10[:, h0:10], in_=s10[:, h0:10], func=AF.Sqrt,
                                               scale=1.0 / 64, bias=epsb[:, 0:1]),
                 reads=["s10", "epsb"], writes=["r10"])
            if lat:
                P.op("dve", lambda e: e.reciprocal(out=st[:, 5:6], in_=st[:, 5:6]), reads=["st5"], writes=["st5"])
                P.op("dve", lambda e: e.scalar_tensor_tensor(out=vvn, in0=vv_sb, scalar=st[:, 5:6], in1=vgainbc[:],
                                                             op0=ALU.mult, op1=ALU.mult),
                     reads=["vv_sb", "st5", "bcst"], writes=["vvn"])
                for g in range(8):
                    P.op("pe", lambda e, g=g: e.matmul(bank(6)[:, g * 64:(g + 1) * 64], lhsT=wsT[:, g, :],
                                                       rhs=vvn[:, g * 64:(g + 1) * 64], start=True, stop=True),
                         reads=["wsT", "vvn"], writes=["ps:6"])
            P.op("dve", lambda e: e.reciprocal(out=r10[:, h0:10], in_=r10[:, h0:10]), reads=["r10"], writes=["r10"])
            P.op("dve", lambda e: e.tensor_tensor(out=qk2[:, h0:10, :], in0=qk[:, h0:10, :],
                                                  in1=r10[:, h0:10].unsqueeze(2).to_broadcast([128, nh, 64]),
                                                  op=ALU.mult), reads=["qk", "r10"], writes=["qk2"])
            P.op("dve", lambda e: e.tensor_tensor(out=qk2[:, h0:10, :], in0=qk2[:, h0:10, :], in1=g3[:, h0:10, :],
                                                  op=ALU.mult), reads=["qk2", "bcst"], writes=["qk2"])
            if lat:
                cosb = cs[:, t, 0:32].unsqueeze(1).to_broadcast([128, 10, 32])
                sinb = cs[:, t, 32:64].unsqueeze(1).to_broadcast([128, 10, 32])
                x1v = qk2[:, :, 0:32]
                x2v = qk2[:, :, 32:64]
                P.op("dve", lambda e: e.tensor_tensor(out=rpv[0], in0=x1v, in1=cosb, op=ALU.mult),
                     reads=["qk2", "cs"], writes=["rp0"])
                P.op("dve", lambda e: e.tensor_tensor(out=rpv[1], in0=x2v, in1=sinb, op=ALU.mult),
                     reads=["qk2", "cs"], writes=["rp1"])
                P.op("pool", lambda e: e.tensor_tensor(out=rpv[2], in0=x1v, in1=sinb, op=ALU.mult),
                     reads=["qk2", "cs"], writes=["rp2"])
                P.op("pool", lambda e: e.tensor_tensor(out=rpv[3], in0=x2v, in1=cosb, op=ALU.mult),
                     reads=["qk2", "cs"], writes=["rp3"])
                P.op("dve", lambda e: e.tensor_tensor(out=qkr[:, :, 0:32], in0=rpv[0], in1=rpv[1], op=ALU.subtract),
                     reads=["rp0", "rp1"], writes=["qkr"])
                P.op("pool", lambda e: e.tensor_tensor(out=qkr[:, :, 32:64], in0=rpv[2], in1=rpv[3], op=ALU.add),
                     reads=["rp2", "rp3"], writes=["qkr"])
            else:
                P.op("dve", lambda e: e.tensor_copy(out=qkr[:, 8:10, :], in_=qk2[:, 8:10, :]), reads=["qk2"], writes=["qkr"])
            P.op("pool", lambda e: e.tensor_copy(out=kdv, in_=qkr[:, 8:10, :].unsqueeze(2).to_broadcast([128, 2, 2, 64])),
                 reads=["qkr"], writes=["kd"])
            if lat:
                for c in range(4):
                    P.op("pe", lambda e, c=c: e.transpose(out=pB[:, c * 128:(c + 1) * 128], in_=qkrf[:, c * 128:(c + 1) * 128],
                                                          identity=identb[:]), reads=["qkr", "identb"], writes=["ps:5"])
            for kv in range(2):
                P.op("pe", lambda e, kv=kv: e.transpose(out=pB[:, 512 + kv * 128:512 + (kv + 1) * 128],
                                                        in_=kdf[:, kv * 128:(kv + 1) * 128], identity=identb[:]),
                     reads=["kd", "identb"], writes=["ps:5"])
            if lat:
                P.op("act", lambda e: e.copy(out=qa[:, :, t * 128:(t + 1) * 128],
                                             in_=pB[:, 0:512].rearrange("p (a b) -> p a b", a=4)),
                     reads=["ps:5"], writes=["qa"])
            P.op("act", lambda e: e.copy(out=kT[:, :, t * 128:(t + 1) * 128],
                                         in_=pB[:, 512:768].rearrange("p (a b) -> p a b", a=2)),
                 reads=["ps:5"], writes=["kT"])
            if not lat:
                return
            P.op("dve", lambda e: e.tensor_tensor(out=gtmp.rearrange("p (a b) -> p a b", a=8),
                                                  in0=bank(6).rearrange("p (a b) -> p a b", a=8),
                                                  in1=bsT[:].unsqueeze(2).to_broadcast([128, 8, 64]), op=ALU.add),
                 reads=["ps:6", "bsT"], writes=["gtmp"])
            P.op("pool", lambda e: e.tensor_tensor(out=gmb, in0=gtmp, in1=u_sb, op=ALU.mult),
                 reads=["gtmp", "u_sb"], writes=["gmb"])
            for c in range(4):
                P.op("pe", lambda e, c=c: e.transpose(out=pC[:, c * 128:(c + 1) * 128], in_=gmb[:, c * 128:(c + 1) * 128],
                                                      identity=identb[:]), reads=["gmb", "identb"], writes=["ps:7"])
            P.op("act", lambda e: e.copy(out=gmT[:, :, t * 128:(t + 1) * 128],
                                         in_=pC[:, 0:512].rearrange("p (a b) -> p a b", a=4)),
                 reads=["ps:7"], writes=["gmT"])

        NTA = 18
        X1(0)
        X2(0)
        X1(1)
        for t in range(NTA):
            Ya(t)
            if t + 1 < NTA:
                X2(t + 1)
            if t + 2 < NTA:
                X1(t + 2)
            Yb(t)

        P.barrier_all()
        steps = [(c, qg, sc_) for c in range(4) for qg in range(4) for sc_ in range(18)]
        Osb = cv.get([128, 1024], F32)

        def qk_step(i):
            c, qg, sc_ = steps[i]
            kv = c // 2
            sj = (0, 1, 3)[i % 3]
            S = pp[sj]
            for half in range(2):
                r0 = half * 64
                P.op("pe", lambda e, half=half, r0=r0: e.matmul(
                    S[:, half * 512:(half + 1) * 512], lhsT=kT[r0:r0 + 64, kv, sc_ * 128:(sc_ + 1) * 128],
                    rhs=qa[r0:r0 + 64, c, qg * 512:(qg + 1) * 512], start=True, stop=True),
                     reads=["kT", "qa%d_%d_%d" % (c, half, qg), "qa"], writes=BP(sj))
            P.op("act", lambda e: e.activation(out=PT[i % 4], in_=S[:], func=AF.Exp, scale=0.125),
                 reads=BP(sj), writes=["PT%d" % (i % 4)])

        def pv_step(i):
            c, qg, sc_ = steps[i]
            kv = c // 2
            for half in range(2):
                off = 64 if half == 0 else 0
                P.op("pe", lambda e, half=half, off=off: e.matmul(
                    bank(4 + half), lhsT=vaug[:, sc_, kv, off:off + 128],
                    rhs=PT[i % 4][:, half * 512:(half + 1) * 512], start=(sc_ == 0), stop=(sc_ == 17)),
                     reads=["vaug", "PT%d" % (i % 4)], writes=["ps:%d" % (4 + half)])
            if sc_ == 17:
                P.op("dve", lambda e: e.tensor_copy(out=Osb, in_=pp[2][:]), reads=BP(2), writes=["Osb"])
                for half in range(2):
                    nr = half * 64
                    dr = 64 - nr
                    P.op("dve", lambda e, half=half, nr=nr, dr=dr: e.reciprocal(
                        out=rd[nr:nr + 64, :], in_=Osb[dr:dr + 64, half * 512:(half + 1) * 512]),
                         reads=["Osb"], writes=["rd%d" % half])
                    P.op("dve", lambda e, half=half, nr=nr: e.tensor_tensor(
                        out=qa[nr:nr + 64, c, qg * 512:(qg + 1) * 512], in0=Osb[nr:nr + 64, half * 512:(half + 1) * 512],
                        in1=rd[nr:nr + 64, :], op=ALU.mult),
                         reads=["Osb", "rd%d" % half], writes=["qa%d_%d_%d" % (c, half, qg)])

        if stage >= 2:
            n = len(steps)
            qk_step(0)
            qk_step(1)
            for i in range(n):
                if i + 2 < n:
                    qk_step(i + 2)
                pv_step(i)

        P.barrier_all()
        for kk in range(4):
            load("pool", wio[:, 2 * kk:2 * kk + 2, 0:1024],
                 w_out[kk * 256:(kk + 1) * 256, :].rearrange("(k p) n -> p k n", p=128), "wio")
        tmpo2 = [tmpo, Osb]
        for t in range(16):
            xb = xt[t % 2]
            xk = "xt%d" % (t % 2)
            load("sync", xb, x[b, t * 128:(t + 1) * 128, :], xk)
            pj = 2 + t % 2
            pO = pp[pj]
            tb = tmpo2[t % 2]
            tk = "tmpo%d" % (t % 2)
            for hh in range(2):
                for k in range(8):
                    src = qa[:, k, t * 128:(t + 1) * 128] if k < 4 else gmT[:, k - 4, t * 128:(t + 1) * 128]
                    rk = ["qa%d_%d_%d" % (k, hf, t // 4) for hf in range(2)] + ["qa"] if k < 4 else ["gmT"]
                    P.op("pe", lambda e, hh=hh, k=k, src=src, pO=pO: e.matmul(pO[:, hh * 512:(hh + 1) * 512], lhsT=src,
                                                                             rhs=wio[:, k, hh * 512:(hh + 1) * 512],
                                                                             start=(k == 0), stop=(k == 7)),
                         reads=rk + ["wio"], writes=BP(pj))
            P.op("dve", lambda e, pO=pO, tb=tb: e.tensor_tensor(out=tb, in0=pO[:], in1=gt1bc, op=ALU.mult),
                 reads=BP(pj) + ["gt1bc"], writes=[tk])
            P.op("pool", lambda e, xb=xb, tb=tb: e.tensor_tensor(out=xb, in0=xb, in1=tb, op=ALU.add),
                 reads=[tk, xk], writes=[xk])
            dst = x1s[b, t * 128:(t + 1) * 128, :] if stage >= 3 else y[b, t * 128:(t + 1) * 128, :]
            P.dma("sync", lambda e, xb=xb, dst=dst: e.dma_start(out=dst, in_=xb), d_outs[t % 2],
                  reads=[xk], writes=["x1s%d_%d" % (b, t)])

    for b in range(2):
        epoch_ac(b)
        P.barrier_all()


    def epoch_d():
        cv = Carver()
        h2 = cv.get([128, 2 * NT, D], BF16)
        x1t = [cv.get([128, D], F32) for _ in range(2)]
        sqj = cv.get([128, D], BF16)
        tmp = cv.get([128, D], F32)
        s2bc = cv.get([128, D], F32)
        sh2bc = cv.get([128, D], F32)
        h2Tt = cv.get([128, D], BF16)
        ex = cv.get([128, 16], F32)
        aff2 = cv.get([128, NT, 48], F32)
        affT = cv.get([48, T], F32, parts=48)
        work = cv.get([48, T], F32, parts=48)
        mx8 = cv.get([48, 8], F32, parts=48)
        gate2 = cv.get([128, NT, 48], F32)
        mask2 = cv.get([128, NT, 48], BF16)
        mask2f = cv.get([128, NT, 48], F32)
        totsb = cv.get([128, NT, 48], F32)
        base = cv.get([128, NT, 48], F32)
        pos = cv.get([128, NT, 48], F32)
        ghf = cv.get([128, NT, 48], F32)
        gh = cv.get([128, NT, 48], BF16)
        fl = lambda v: v.rearrange("p a b -> p (a b)")

        P.op("pool!", lambda e: e.memset(fl(aff2), 0.0), writes=["aff2"])
        d_h2s = [P.dsem("h2s0"), P.dsem("h2s1")]
        tokf = cv.get([128, NT * 48, 2], F32)
        load("sync", tokf.rearrange("p a b -> p (a b)"), k_tokc, "tokf")
        P.op("dve", lambda e: e.tensor_copy(out=ghl[:].rearrange("p a b c -> p (a b) c")[:, :, 2:4], in_=tokf),
             reads=["tokf"], writes=["ghl23"])
        for b in range(2):
            for (dst, ri, key) in ((s2bc, 1, "s2bc"), (sh2bc, 2, "sh2bc")):
                for hh in range(2):
                    P.op("pe", lambda e, ri=ri, hh=hh, b=b: e.matmul(bank(0), lhsT=sel3[:, b, :],
                                                               rhs=msb[:, ri, hh * 512:(hh + 1) * 512],
                                                               start=True, stop=True), reads=[], writes=["ps:0"])
                    P.op("act", lambda e, dst=dst, hh=hh: e.copy(out=dst[:, hh * 512:(hh + 1) * 512], in_=bank(0)),
                         reads=["ps:0"], writes=[key])
            def DX(t, b=b):
                xb = x1t[t % 2]
                xk = "x1t%d" % (t % 2)
                load("sync", xb, x1s[b, t * 128:(t + 1) * 128, :], xk)
                P.op("act", lambda e: e.activation(out=sqj, in_=xb, func=AF.Square, accum_out=st[:, 0:1]),
                     reads=[xk], writes=["sqj", "st0"])
                rstd_chain(0, 1.0 / D)
                P.op("dve", lambda e: e.scalar_tensor_tensor(out=tmp, in0=xb, scalar=st[:, 1:2], in1=s2bc,
                                                             op0=ALU.mult, op1=ALU.mult),
                     reads=[xk, "st1", "s2bc"], writes=["tmp"])
                h2t = h2[:, b * NT + t, :]
                P.op("pool", lambda e: e.tensor_tensor(out=h2t, in0=tmp, in1=sh2bc, op=ALU.add),
                     reads=["tmp", "sh2bc"], writes=["h2t"])
                P.dma("sync", lambda e: e.dma_start(out=h2s[b * T + t * 128:b * T + (t + 1) * 128, :], in_=h2t),
                      d_h2s[t % 2], reads=["h2t"])
                pA = bankb(1)
                for k in range(8):
                    P.op("pe", lambda e, k=k: e.transpose(out=pA[:, k * 128:(k + 1) * 128],
                                                          in_=h2t[:, k * 128:(k + 1) * 128], identity=identb[:]),
                         reads=["h2t"], writes=["ps:1"])
                P.op("act", lambda e: e.copy(out=h2Tt, in_=pA), reads=["ps:1"], writes=["h2Tt"])
                lg = bank(2)[:, t * 16:(t + 1) * 16]
                for k in range(8):
                    P.op("pe", lambda e, k=k: e.matmul(lg, lhsT=h2Tt[:, k * 128:(k + 1) * 128], rhs=wr[:, k, :],
                                                       start=(k == 0), stop=(k == 7)),
                         reads=["h2Tt"], writes=["ps:2"])

            def DY(t, b=b):
                lg = bank(2)[:, t * 16:(t + 1) * 16]
                P.op("dve", lambda e: e.tensor_reduce(out=st[:, 8:9], in_=lg, axis=AX.X, op=ALU.max),
                     reads=["ps:2"], writes=["st8"])
                P.op("dve", lambda e: e.tensor_scalar(out=st[:, 9:10], in0=st[:, 8:9], scalar1=-1.0, scalar2=None,
                                                      op0=ALU.mult), reads=["st8"], writes=["st9"])
                P.op("act", lambda e: e.activation(out=ex, in_=lg, func=AF.Exp, bias=st[:, 9:10],
                                                   accum_out=st[:, 10:11]),
                     reads=["ps:2", "st9"], writes=["ex", "st10"])
                P.op("dve", lambda e: e.reciprocal(out=st[:, 11:12], in_=st[:, 10:11]), reads=["st10"], writes=["st11"])
                P.op("dve", lambda e: e.tensor_scalar(out=aff2[:, t, 32 * b:32 * b + 16], in0=ex,
                                                      scalar1=st[:, 11:12], scalar2=None, op0=ALU.mult),
                     reads=["ex", "st11"], writes=["aff2"])

            DX(0)
            for t in range(NT):
                if t + 1 < NT:
                    DX(t + 1)
                DY(t)
        for t in range(NT):
            P.op("pe", lambda e, t=t: e.transpose(out=pp[2 + t // 8][0:48, (t % 8) * 128:(t % 8) * 128 + 128],
                                                  in_=aff2[:, t, :], identity=identf[:]),
                 reads=["aff2"], writes=BP(2 + t // 8))
        for hh in range(2):
            P.op("act", lambda e, hh=hh: e.copy(out=affT[:, hh * 1024:(hh + 1) * 1024], in_=pp[2 + hh][0:48, :]),
                 reads=BP(2 + hh), writes=["affT"])
            P.op("dve", lambda e, hh=hh: e.tensor_copy(out=work[:, hh * 1024:(hh + 1) * 1024], in_=pp[2 + hh][0:48, :]),
                 reads=BP(2 + hh), writes=["work"])
        for r in range(CAP // 8):
            P.op("dve", lambda e: e.max(out=mx8, in_=work), reads=["work"], writes=["mx8"])
            P.op("dve", lambda e: e.match_replace(out=work, in_to_replace=mx8, in_values=work, imm_value=0.0),
                 reads=["work", "mx8"], writes=["work"])
        P.op("dve", lambda e: e.tensor_tensor(out=work, in0=affT, in1=work, op=ALU.subtract),
             reads=["affT", "work"], writes=["work"])
        for t in range(NT):
            P.op("pe", lambda e, t=t: e.transpose(out=pp[t // 8][:, (t % 8) * 64:(t % 8) * 64 + 48],
                                                  in_=work[:, t * 128:(t + 1) * 128], identity=identf[0:48, 0:48]),
                 reads=["work"], writes=BP(t // 8))
        for hh in range(2):
            P.op("act", lambda e, hh=hh: e.copy(out=gate2[:, hh * 8:(hh + 1) * 8, :],
                                                in_=pp[hh][:, 0:512].rearrange("p (a b) -> p a b", a=8)[:, :, 0:48]),
                 reads=BP(hh), writes=["gate2"])
        P.op("dve", lambda e: e.tensor_scalar(out=fl(mask2), in0=fl(gate2), scalar1=0.0, scalar2=None, op0=ALU.is_gt),
             reads=["gate2"], writes=["mask2"])
        P.op("dve", lambda e: e.tensor_scalar(out=fl(mask2f), in0=fl(gate2), scalar1=0.0, scalar2=None, op0=ALU.is_gt),
             reads=["gate2"], writes=["mask2f"])
        for hh in range(2):
            P.op("pe", lambda e, hh=hh: e.matmul(bank(4 + hh)[:, 0:384], lhsT=trib[:], rhs=fl(mask2)[:, hh * 384:(hh + 1) * 384],
                                                 start=True, stop=True), reads=["mask2"], writes=["ps:%d" % (4 + hh)])
            P.op("pe", lambda e, hh=hh: e.matmul(bank(6 + hh)[:, 0:384], lhsT=onesb[:], rhs=fl(mask2)[:, hh * 384:(hh + 1) * 384],
                                                 start=True, stop=True), reads=["mask2"], writes=["ps:%d" % (6 + hh)])
            P.op("act", lambda e, hh=hh: e.copy(out=fl(totsb)[:, hh * 384:(hh + 1) * 384], in_=bank(6 + hh)[:, 0:384]),
                 reads=["ps:%d" % (6 + hh)], writes=["totsb"])
        P.op("dve", lambda e: e.memset(base[:, 0, :], 0.0), writes=["base"])
        for t in range(1, NT):
            P.op("dve", lambda e, t=t: e.tensor_tensor(out=base[:, t, :], in0=base[:, t - 1, :], in1=totsb[:, t - 1, :],
                                                      op=ALU.add), reads=["base", "totsb"], writes=["base"])
        for hh in range(2):
            P.op("dve", lambda e, hh=hh: e.tensor_tensor(out=fl(pos)[:, hh * 384:(hh + 1) * 384], in0=bank(4 + hh)[:, 0:384],
                                                        in1=fl(base)[:, hh * 384:(hh + 1) * 384], op=ALU.add),
                 reads=["ps:%d" % (4 + hh), "base"], writes=["pos"])
        P.op("dve", lambda e: e.scalar_tensor_tensor(out=fl(pos), in0=fl(pos), scalar=1.0, in1=fl(mask2f),
                                                     op0=ALU.add, op1=ALU.mult), reads=["pos", "mask2f"], writes=["pos"])
        P.op("dve", lambda e: e.tensor_scalar(out=fl(posm[:]), in0=fl(pos), scalar1=-1.0, scalar2=None, op0=ALU.add),
             reads=["pos"], writes=["posm"])
        for t in range(NT):
            P.op("pe", lambda e, t=t: e.transpose(out=pp[2 + t // 8][0:48, (t % 8) * 128:(t % 8) * 128 + 128],
                                                  in_=posm[:, t, :], identity=identf[:]),
                 reads=["posm"], writes=BP(2 + t // 8))
        for hh in range(2):
            P.op("act", lambda e, hh=hh: e.copy(out=posmT[:, hh * 1024:(hh + 1) * 1024], in_=pp[2 + hh][0:48, :]),
                 reads=BP(2 + hh), writes=["posmT"])
        P.op("dve", lambda e: e.tensor_copy(out=fl(gh), in_=fl(gate2)), reads=["gate2"], writes=["gh"])
        P.op("dve", lambda e: e.tensor_copy(out=fl(ghf), in_=fl(gh)), reads=["gh"], writes=["ghf"])
        P.op("dve", lambda e: e.tensor_tensor(out=ghl[:, :, :, 1], in0=gate2, in1=ghf, op=ALU.subtract),
             reads=["gate2", "ghf"], writes=["ghl1"])
        P.op("dve", lambda e: e.tensor_copy(out=ghl[:, :, :, 0], in_=gh), reads=["gh"], writes=["ghl0"])

    def epoch_e():
        cv = Carver()
        ring = [cv.get([128, 8, 512], BF16) for _ in range(8)]
        S = [cv.get([128, NT, 256], BF16) for _ in range(2)]
        xeT = cv.get([128, 8, 512], BF16)
        hidT = cv.get([128, 16, 512], BF16)
        sil = [cv.get([128, 512], F32) for _ in range(2)]
        ye_sb = cv.get([128, 4, D], F32)
        gt2bc = cv.get([128, 2, D], F32)
        pgs = cv.get([128, 2, 16], F32)
        idxf = cv.get([128, 2, 4], F32)
        gsb = cv.get([128, 2, 4], F32)
        xetok = [cv.get([128, 4, D], BF16) for _ in range(2)]
        d_sc = [P.dsem("scat0"), P.dsem("scat1")]
        d_g = [P.dsem("gath0"), P.dsem("gath1")]
        x1flat = x1s.rearrange("b t d -> (b t) d")
        for b in range(2):
            for hh in range(2):
                P.op("pe", lambda e, b=b, hh=hh: e.matmul(bank(6), lhsT=sel3[:, b, :], rhs=msb[:, 3, hh * 512:(hh + 1) * 512],
                                                         start=True, stop=True), reads=[], writes=["ps:6"])
                P.op("act", lambda e, b=b, hh=hh: e.copy(out=gt2bc[:, b, hh * 512:(hh + 1) * 512], in_=bank(6)),
                     reads=["ps:6"], writes=["gt2bc"])
        NRING = 8
        uspec = []
        for e2 in range(NE):
            for g in range(4):
                uspec.append(("a", w1[e2][:, g * 512:(g + 1) * 512].rearrange("(k p) f -> p k f", p=128)))
                uspec.append(("a", w3[e2][:, g * 512:(g + 1) * 512].rearrange("(k p) f -> p k f", p=128)))
            for dq in range(4):
                uspec.append(("b", [w2[e2][hf * 1024:(hf + 1) * 1024, dq * 256:(dq + 1) * 256]
                                    .rearrange("(c p) d -> p c d", p=128) for hf in range(2)]))
        issued = [0]

        def uview(u):
            s_ = u % NRING
            if uspec[u][0] == "a":
                return ring[s_], "ring%d" % s_
            return ring[s_].rearrange("p a b -> p (a b)").rearrange("p (c d) -> p c d", c=16), "ring%d" % s_

        def ensure_issued(upto):
            upto = min(upto, len(uspec) - 1)
            while issued[0] <= upto:
                u = issued[0]
                v, key = uview(u)
                if uspec[u][0] == "a":
                    load("pool", v, uspec[u][1], key)
                else:
                    for hf in range(2):
                        load("pool", v[:, hf * 8:(hf + 1) * 8, :], uspec[u][1][hf], key)
                issued[0] += 1

        def prep(e_):
            par = e_ % 2
            for b in range(2):
                col = 32 * b + e_
                for t in range(NT):
                    P.op("dve", lambda e, b=b, t=t, col=col: e.tensor_scalar(out=S[b][:, t, :], in0=iota256[:],
                                                                           scalar1=posm[:, t, col:col + 1], scalar2=None,
                                                                           op0=ALU.is_equal),
                         reads=[], writes=["S%d" % b])
            pg = bank(5)[:, 256:272]
            for b in range(2):
                col = 32 * b + e_
                for half in range(2):
                    gi = b * 2 + half
                    for t in range(NT):
                        P.op("pe", lambda e, b=b, t=t, half=half, gi=gi, col=col: e.matmul(
                            pg[:, gi * 4:gi * 4 + 4], lhsT=S[b][:, t, half * 128:(half + 1) * 128],
                            rhs=ghl[:, t, col, :], start=(t == 0), stop=(t == NT - 1)),
                             reads=["S%d" % b], writes=["ps:5"])
            P.op("dve", lambda e: e.tensor_copy(out=pgs[:, par, :], in_=pg), reads=["ps:5"], writes=["pgs%d" % par])
            pv4 = pgs[:, par, :].rearrange("p (a b) -> p a b", a=4)
            P.op("dve", lambda e: e.tensor_tensor(out=gsb[:, par, :], in0=pv4[:, :, 0], in1=pv4[:, :, 1], op=ALU.add),
                 reads=["pgs%d" % par], writes=["gs%d" % par])
            P.op("dve", lambda e: e.scalar_tensor_tensor(out=idxf[:, par, :], in0=pv4[:, :, 2], scalar=256.0,
                                                         in1=pv4[:, :, 3], op0=ALU.mult, op1=ALU.add),
                 reads=["pgs%d" % par], writes=["idxf%d" % par])
            P.op("dve", lambda e: e.tensor_copy(out=idxi[:, par * 4:par * 4 + 4], in_=idxf[:, par, :]),
                 reads=["idxf%d" % par], writes=["idxi%d" % par])
            for gi in range(4):
                P.dma("pool", lambda e, gi=gi: e.indirect_dma_start(
                    out=xetok[par][:, gi, :], out_offset=None, in_=h2s[:, :],
                    in_offset=bass.IndirectOffsetOnAxis(ap=idxi[:, par * 4 + gi:par * 4 + gi + 1], axis=0)),
                      d_g[par], reads=["idxi%d" % par], writes=["xetok%d" % par])

        import os
        ne_dbg = int(os.environ.get("K_NE", NE))
        prep(0)
        for e_ in range(ne_dbg):
            par = e_ % 2
            gs = gsb[:, par, :]
            for gi in range(4):
                pT = bankb(6 + gi % 2)
                for k in range(8):
                    P.op("pe", lambda e, gi=gi, k=k, pT=pT, par=par: e.transpose(out=pT[:, k * 128:(k + 1) * 128],
                                                                        in_=xetok[par][:, gi, k * 128:(k + 1) * 128],
                                                                        identity=identb[:]),
                         reads=["xetok%d" % par], writes=["ps:%d" % (6 + gi % 2)])
                P.op("act", lambda e, gi=gi, pT=pT: e.copy(out=xeT[:, :, gi * 128:(gi + 1) * 128],
                                                           in_=pT.rearrange("p (a b) -> p a b", a=8)),
                     reads=["ps:%d" % (6 + gi % 2)], writes=["xeT"])
            for g in range(4):
                ua = 12 * e_ + 2 * g
                ensure_issued(ua + 1)
                wa, ka = uview(ua)
                wb, kb = uview(ua + 1)
                for mm in range(4):
                    m = g * 4 + mm
                    ph1 = bank(m % 2)
                    ph3 = bank(2 + m % 2)
                    for k in range(8):
                        P.op("pe", lambda e, k=k, mm=mm, wa=wa, ph1=ph1: e.matmul(
                            ph1, lhsT=wa[:, k, mm * 128:(mm + 1) * 128], rhs=xeT[:, k, :], start=(k == 0), stop=(k == 7)),
                             reads=[ka, "xeT"], writes=["ps:%d" % (m % 2)])
                    for k in range(8):
                        P.op("pe", lambda e, k=k, mm=mm, wb=wb, ph3=ph3: e.matmul(
                            ph3, lhsT=wb[:, k, mm * 128:(mm + 1) * 128], rhs=xeT[:, k, :], start=(k == 0), stop=(k == 7)),
                             reads=[kb, "xeT"], writes=["ps:%d" % (2 + m % 2)])
                    P.op("act", lambda e, m=m, ph1=ph1: e.activation(out=sil[m % 2], in_=ph1, func=AF.Silu),
                         reads=["ps:%d" % (m % 2)], writes=["sil%d" % (m % 2)])
                    P.op("dve", lambda e, m=m, ph3=ph3: e.tensor_tensor(out=hidT[:, m, :], in0=sil[m % 2], in1=ph3,
                                                                        op=ALU.mult),
                         reads=["sil%d" % (m % 2), "ps:%d" % (2 + m % 2)], writes=["hidT"])
                ensure_issued(min(ua + 1 + NRING, 12 * e_ + 11))
            ensure_issued(12 * e_ + 11)
            if e_ + 1 < ne_dbg:
                prep(e_ + 1)
            for dq in range(4):
                uw = 12 * e_ + 8 + dq
                wv, kw = uview(uw)
                for sc in range(4):
                    pye = bank(4 + sc % 2)[:, 0:256]
                    for m in range(16):
                        P.op("pe", lambda e, m=m, sc=sc, wv=wv, pye=pye: e.matmul(
                            pye, lhsT=hidT[:, m, sc * 128:(sc + 1) * 128], rhs=wv[:, m, :], start=(m == 0), stop=(m == 15)),
                             reads=[kw, "hidT"], writes=["ps:%d" % (4 + sc % 2)])
                    P.op("dve", lambda e, sc=sc, dq=dq, pye=pye, gs=gs: e.scalar_tensor_tensor(
                        out=ye_sb[:, sc, dq * 256:(dq + 1) * 256], in0=pye, scalar=gs[:, sc:sc + 1],
                        in1=gt2bc[:, sc // 2, dq * 256:(dq + 1) * 256], op0=ALU.mult, op1=ALU.mult),
                         reads=["ps:%d" % (4 + sc % 2), "gs%d" % par, "gt2bc"], writes=["ye_sb"])
                ensure_issued(uw + NRING)
            for gi in range(4):
                P.dma("pool", lambda e, gi=gi, par=par: e.indirect_dma_start(
                    out=x1flat[:, :], out_offset=bass.IndirectOffsetOnAxis(ap=idxi[:, par * 4 + gi:par * 4 + gi + 1], axis=0),
                    in_=ye_sb[:, gi, :], in_offset=None, compute_op=ALU.add),
                      d_sc[gi // 2], reads=["ye_sb", "idxi%d" % par], writes=["x1acc%d" % (gi // 2)])

    def epoch_f(b):
        cv = Carver()
        x1t = [cv.get([128, D], F32) for _ in range(3)]
        outt = [cv.get([128, D], F32) for _ in range(2)]
        sqj = cv.get([128, D], BF16)
        for t in range(NT):
            xb = x1t[t % 3]
            xk = "x1t%d" % (t % 3)
            ob = outt[t % 2]
            okk = "outt%d" % (t % 2)
            c0 = 2 * (t % 2)
            load("sync", xb, x1s[b, t * 128:(t + 1) * 128, :], xk)
            P.op("act", lambda e, xb=xb, c0=c0: e.activation(out=sqj, in_=xb, func=AF.Square, accum_out=st[:, c0:c0 + 1]),
                 reads=[xk], writes=["sqj", "st%d" % c0])
            rstd_chain(c0, 1.0 / D)
            P.op("dve", lambda e, xb=xb, ob=ob, c0=c0: e.scalar_tensor_tensor(out=ob, in0=xb, scalar=st[:, c0 + 1:c0 + 2],
                                                                             in1=gfinbc[:], op0=ALU.mult, op1=ALU.mult),
                 reads=[xk, "st%d" % (c0 + 1)], writes=[okk])
            P.dma("sync", lambda e, ob=ob, t=t: e.dma_start(out=y[b, t * 128:(t + 1) * 128, :], in_=ob),
                  d_outs[t % 2], reads=[okk])

    if stage >= 3:
        epoch_d()
        P.barrier_all()
        if stage == 25:
            P.final_wait("sync", d_outs)
            P.emit()
            return nc
        epoch_e()
        P.barrier_all()
        if stage == 26:
            P.final_wait("sync", d_outs)
            P.emit()
            return nc
        for b in range(2):
            epoch_f(b)
            P.barrier_all()

    P.final_wait("sync", d_outs)
    P.emit()
    return nc


def build_moe(nc, P, env):
    raise NotImplementedError


_STAGE = 3


def kernel(x, c, ctx, c_ctx, w_mod, b_mod, g_mix, g_ffn, w_in, q_gain, k_gain, v_gain,
           w_s, b_s, w_out, w_router, w1, w3, w2, g_final):
    f = lambda a: np.ascontiguousarray(np.asarray(a, dtype=np.float32))
    x, c, ctx, c_ctx = f(x), f(c), f(ctx), f(c_ctx)
    consts = _consts()
    rows = np.concatenate([f(g_ffn)[0], f(g_final), np.tile(f(q_gain)[0], 8), np.tile(f(k_gain)[0], 2),
                           f(v_gain)[0]])
    assert rows.shape[0] == 3200
    shared = {
        "w_mod": f(w_mod)[0], "bmod3": np.tile(f(b_mod)[0][None, :], (3, 1)),
        "rows3": np.tile(rows[None, :], (3, 1)),
        "g2": np.concatenate([f(g_mix)[0].reshape(8, 128), f(g_ffn)[0].reshape(8, 128)], axis=0),
        "w_in": f(w_in)[0], "w_s": f(w_s)[0], "b_s": f(b_s)[0], "w_out": f(w_out)[0],
        "w_router": f(w_router)[0], "w1": f(w1)[0], "w3": f(w3)[0], "w2": f(w2)[0],
    }
    shared.update(consts)
    in_maps = []
    for i in range(NCORES):
        m = dict(shared)
        m["x"] = x[2 * i:2 * i + 2]
        m["ctx"] = ctx[2 * i:2 * i + 2]
        m["c3"] = np.concatenate([c[2 * i:2 * i + 2], c_ctx[None, :]], axis=0)
        in_maps.append(m)
    nc = build_nc(_STAGE)
    res = run_bass_kernel_spmd(nc, in_maps, core_ids=list(range(NCORES)))
    return np.concatenate([r["y"] for r in res.results], axis=0)
```

```python
import numpy as np
import concourse.bass as bass
import concourse.mybir as mybir
from concourse.bass_utils import run_bass_kernel_spmd
from contextlib import ExitStack

F32 = mybir.dt.float32
BF16 = mybir.dt.bfloat16
U8 = mybir.dt.uint8
ALU = mybir.AluOpType
AF = mybir.ActivationFunctionType
AX = mybir.AxisListType

COMPUTE = ("pe", "act", "dve", "pool")
POOL_TO_DVE = True
CARVE_LOG = []
EPS = 1e-6
NCORES = 8
T = 2048
NT = 16
D = 1024
NE = 16
CAP = 256


class DSem:
    def __init__(self, name):
        self.name = name
        self.sem = None
        self.count = 0


class Ins:
    __slots__ = ("eng", "fn", "waits", "flag", "idx", "dsem")

    def __init__(self, eng, fn):
        self.eng = eng
        self.fn = fn
        self.waits = []
        self.flag = False
        self.idx = None
        self.dsem = None


class Prog:
    def __init__(self, nc):
        self.nc = nc
        self.q = {e: [] for e in COMPUTE + ("sync",)}
        self.last_w = {}
        self.readers = {}
        self.waited_c = {e: {b: -1 for b in COMPUTE} for e in self.q}
        self.waited_d = {e: {} for e in self.q}
        self.dsems = []

    def dsem(self, name):
        d = DSem(name)
        self.dsems.append(d)
        return d

    def _add_wait(self, ins, dep):
        q = ins.eng
        if dep[0] == "c":
            _, b, idx = dep
            if b == "pe" and q == "pe":
                return
            while idx >= 0 and (self.q[b][idx].fn is None or self.q[b][idx].dsem is not None):
                idx -= 1
            if idx < 0 or self.waited_c[q][b] >= idx:
                return
            self.waited_c[q][b] = idx
            self.q[b][idx].flag = True
            ins.waits.append(("c", b, idx))
        else:
            _, d, cnt = dep
            if self.waited_d[q].get(id(d), 0) >= cnt:
                return
            self.waited_d[q][id(d)] = cnt
            ins.waits.append(dep)

    def _deps(self, ins, reads, writes, token):
        ex = [r for r in reads if r.startswith("ps:")]
        if ex:
            reads = [r for r in reads if not r.startswith("ps:")]
            writes = list(writes) + ex
        for r in reads:
            w = self.last_w.get(r)
            if w is not None:
                self._add_wait(ins, w)
        for w_ in writes:
            w = self.last_w.get(w_)
            if w is not None:
                self._add_wait(ins, w)
            for rd in self.readers.get(w_, {}).values():
                self._add_wait(ins, rd)
        key = token[1] if token[0] == "c" else id(token[1])
        for r in reads:
            self.readers.setdefault(r, {})[key] = token
        for w_ in writes:
            self.last_w[w_] = token
            self.readers[w_] = {}

    def op(self, eng, fn, reads=(), writes=()):
        if eng == "pool!":
            eng = "pool"
        elif eng == "pool" and POOL_TO_DVE:
            eng = "dve"
        ins = Ins(eng, fn)
        ins.idx = len(self.q[eng])
        self._deps(ins, reads, writes, ("c", eng, ins.idx))
        self.q[eng].append(ins)
        return ins

    def dma(self, queue, fn, dsem, reads=(), writes=()):
        ins = Ins(queue, fn)
        ins.idx = len(self.q[queue])
        dsem.count += 16
        ins.dsem = dsem
        self._deps(ins, reads, writes, ("d", dsem, dsem.count))
        self.q[queue].append(ins)
        return ins

    def barrier_all(self):
        for q in self.q:
            ins = Ins(q, None)
            ins.idx = len(self.q[q])
            for b in COMPUTE:
                j = len(self.q[b]) - 1
                if j >= 0 and b != q:
                    self._add_wait(ins, ("c", b, j))
            for d in self.dsems:
                if d.count > 0:
                    self._add_wait(ins, ("d", d, d.count))
            self.q[q].append(ins)
        self.last_w = {}
        self.readers = {}

    def final_wait(self, queue, dsems):
        ins = Ins(queue, None)
        ins.idx = len(self.q[queue])
        for d in dsems:
            if d.count > 0:
                self._add_wait(ins, ("d", d, d.count))
        self.q[queue].append(ins)

    def emit(self):
        nc = self.nc
        with ExitStack() as st:
            csem = {e: st.enter_context(nc.semaphore("cs_" + e)) for e in COMPUTE}
            for d in self.dsems:
                if d.count > 0:
                    d.sem = st.enter_context(nc.semaphore("ds_" + d.name))
            mile = {}
            for e in COMPUTE:
                c = 0
                arr = []
                for ins in self.q[e]:
                    if ins.flag:
                        c += 1
                    arr.append(c)
                mile[e] = arr
            block = st.enter_context(nc.Block())

            def run(qname, eng):
                for ins in self.q[qname]:
                    for dep in ins.waits:
                        if dep[0] == "c":
                            eng.wait_ge(csem[dep[1]], mile[dep[1]][dep[2]])
                        else:
                            eng.wait_ge(dep[1].sem, dep[2])
                    if ins.fn is None:
                        continue
                    r = ins.fn(eng)
                    if ins.dsem is not None:
                        r.then_inc(ins.dsem.sem, 16)
                    elif ins.flag:
                        r.then_inc(csem[qname], 1)

            @block.sync
            def _(sync):
                run("sync", sync)

            @block.scalar
            def _(scalar):
                run("act", scalar)

            @block.vector
            def _(vector):
                run("dve", vector)

            @block.gpsimd
            def _(gpsimd):
                run("pool", gpsimd)

            @block.tensor
            def _(tensor):
                run("pe", tensor)


def _consts():
    c = {}
    c["identf"] = np.eye(128, dtype=np.float32)
    k = np.arange(128)
    c["tri"] = (k[:, None] < k[None, :]).astype(np.float32)
    c["iota256"] = np.tile(np.arange(256, dtype=np.float32)[None, :], (128, 1))
    c["iotap"] = np.stack([k, k + 128], axis=1).astype(np.float32)
    sel48 = np.zeros((48, 32, 128), np.float32)
    for b in range(2):
        for e in range(16):
            sel48[32 * b + e, 16 * b + e, :] = 1.0
    c["sel48"] = sel48.reshape(48, 32 * 128)
    sel3 = np.zeros((3, 3, 128), np.float32)
    for j in range(3):
        sel3[j, j, :] = 1.0
    c["sel3"] = sel3.reshape(3, 384)
    nf = 16
    inv = (10000.0 ** (-np.arange(nf, dtype=np.float32) / nf)).astype(np.float32)
    tok = np.arange(T)
    row = (tok // 64).astype(np.float32)
    col = (tok % 64).astype(np.float32)
    ang = np.concatenate([row[:, None] * inv[None, :], col[:, None] * inv[None, :]], axis=-1).astype(np.float32)
    c["cs"] = np.concatenate([np.cos(ang), np.sin(ang)], axis=-1).astype(np.float32)
    tokc = np.zeros((128, NT, 48, 2), np.float32)
    for t in range(NT):
        for col in range(48):
            bb = col // 32
            tokc[:, t, col, 0] = bb * 8 + t // 2
            tokc[:, t, col, 1] = (t % 2) * 128 + np.arange(128)
    c["tokc"] = tokc.reshape(128, NT * 48 * 2)
    return c


def build_nc(stage=9):
    nc = bass.Bass("TRN2", target_bir_lowering=False)
    P = Prog(nc)

    def din(name, shape):
        return nc.dram_tensor(name, list(shape), F32, kind="ExternalInput").ap()

    x = din("x", [2, T, D])
    ctx = din("ctx", [2, 256, D])
    c3 = din("c3", [3, D])
    w_mod = din("w_mod", [D, 6 * D])
    bmod3 = din("bmod3", [3, 6 * D])
    rows3 = din("rows3", [3, 3200])
    g2 = din("g2", [16, 128])
    w_in = din("w_in", [D, 1792])
    w_s = din("w_s", [8, 128, 128])
    b_s = din("b_s", [8, 128])
    w_out = din("w_out", [D, D])
    w_router = din("w_router", [D, NE])
    w1 = din("w1", [NE, D, 2048])
    w3 = din("w3", [NE, D, 2048])
    w2 = din("w2", [NE, 2048, D])
    k_identf = din("identf", [128, 128])
    k_tri = din("tri", [128, 128])
    k_iota256 = din("iota256", [128, 256])
    k_iotap = din("iotap", [128, 2])
    k_sel48 = din("sel48", [48, 4096])
    k_sel3 = din("sel3", [3, 384])
    k_cs = din("cs", [T, 64])
    y = nc.dram_tensor("y", [2, T, D], F32, kind="ExternalOutput").ap()
    x1s = nc.dram_tensor("x1s", [2, T, D], F32, kind="Internal").ap()
    yes = nc.dram_tensor("yes", [2, NE, 2, 128, D], BF16, kind="Internal").ap()
    h2s = nc.dram_tensor("h2s", [2 * T, D], BF16, kind="Internal").ap()
    k_tokc = din("tokc", [128, NT * 48 * 2])

    def sb(name, shape, dt):
        return nc.alloc_sbuf_tensor(name, list(shape), dt)

    identb = sb("identb", [128, 128], BF16)
    identf = sb("identf_sb", [128, 128], F32)
    trib = sb("trib", [128, 128], BF16)
    onesb = sb("onesb", [128, 128], BF16)
    iota256 = sb("iota256_sb", [128, 256], F32)
    iotap = sb("iotap_sb", [128, 2], F32)
    sel48 = sb("sel48_sb", [48, 32, 128], BF16)
    sel3 = sb("sel3_sb", [3, 3, 128], F32)
    cs = sb("cs_sb", [128, NT, 64], F32)
    gain640 = sb("gain640", [128, 640], F32)
    vgainbc = sb("vgainbc", [128, 512], F32)
    gfinbc = sb("gfinbc", [128, D], F32)
    wsT = sb("wsT", [128, 8, 128], BF16)
    bsT = sb("bsT", [128, 8], F32)
    wr = sb("wr", [128, 8, NE], BF16)
    scT = sb("scT", [128, 8, 3], BF16)
    mT = sb("mT", [128, 48, 3], F32)
    gT = sb("gT", [128, 16], F32)
    s1T = sb("s1T", [128, 8, 3], F32)
    msb = sb("msb", [3, 4, D], F32)
    posm = sb("posm", [128, NT, 48], F32)
    ghl = sb("ghl", [128, NT, 48, 4], BF16)
    idxi = sb("idxi", [128, 8], mybir.dt.int32)
    posmT = sb("posmT", [48, T], BF16)
    st = sb("st", [128, 64], F32)
    epsb = sb("epsb", [128, 1], F32)

    RBYTES = 150 * 1024
    R = sb("R", [128, RBYTES], U8)

    class Carver:
        def __init__(self):
            self.off = 0
            CARVE_LOG.append([])

        def get(self, shape, dt, parts=128):
            es = 2 if dt == BF16 else 4
            n = int(np.prod(shape[1:]))
            nb = n * es
            off = (self.off + 63) // 64 * 64
            assert off + nb <= RBYTES, (off, nb)
            v = R[0:parts, off:off + nb].bitcast(dt)
            CARVE_LOG[-1].append((off, tuple(shape), "bf16" if dt == BF16 else "f32"))
            self.off = off + nb
            if len(shape) == 3:
                v = v.rearrange("p (a b) -> p a b", a=shape[1])
            elif len(shape) == 4:
                v = v.rearrange("p (a b c) -> p a b c", a=shape[1], b=shape[2])
            return v

    pp = [nc.alloc_psum_tensor("pp%d" % i, [128, 1024], F32) for i in range(4)]

    def bank(i):
        return pp[i // 2][:, (i % 2) * 512:(i % 2) * 512 + 512]

    def BP(j):
        return ["ps:%d" % (2 * j), "ps:%d" % (2 * j + 1)]

    def bankb(i):
        return bank(i).bitcast(BF16)

    dcache = {}

    def dget(key):
        if key not in dcache:
            dcache[key] = P.dsem("d%d" % len(dcache))
        return dcache[key]

    def load(queue, dst, src, key, reads=()):
        P.dma(queue, lambda e: e.dma_start(out=dst, in_=src), dget(key), reads=reads, writes=[key])

    d_outs = [P.dsem("out0"), P.dsem("out1")]

    cv = Carver()
    c3_sb = cv.get([3, D], F32, parts=3)
    sc3 = cv.get([3, D], F32, parts=3)
    rows3_sb = cv.get([3, 3200], F32, parts=3)
    bmod_sb = cv.get([3, 6 * D], F32, parts=3)
    wm = [cv.get([128, 8, 512], BF16) for _ in range(3)]
    wstmp = cv.get([128, 8, 128], F32)
    g2_sb = cv.get([16, 128], F32, parts=16)
    bs_sb = cv.get([8, 128], F32, parts=8)
    mtmp = [cv.get([3, 512], F32, parts=3) for _ in range(2)]
    trif = cv.get([128, 128], F32)
    sel48f = cv.get([48, 4096], F32, parts=48)

    load("sync", identf[:], k_identf, "identf")
    load("pool", identb[:], k_identf, "identb")
    load("pool", trib[:], k_tri, "trib")
    load("sync", iota256[:], k_iota256, "iota256")
    load("sync", iotap[:], k_iotap, "iotap")
    load("pool", sel48[:].rearrange("p a b -> p (a b)"), k_sel48, "sel48")
    load("sync", sel3[:].rearrange("p a b -> p (a b)"), k_sel3, "sel3")
    load("sync", cs[:], k_cs.rearrange("(i p) c -> p i c", p=128), "cs")
    load("sync", c3_sb, c3, "c3")
    load("sync", rows3_sb, rows3, "rows3")
    load("sync", bmod_sb, bmod3, "bmod")
    load("sync", g2_sb, g2, "g2")
    load("sync", bs_sb, b_s, "bs")
    load("sync", wstmp, w_s.rearrange("g i j -> i g j"), "wstmp")
    load("pool", wr[:], w_router.rearrange("(k p) e -> p k e", p=128), "wr")
    P.op("pool!", lambda e: e.memset(onesb[:], 1.0), writes=["onesb"])
    P.op("pool!", lambda e: e.memset(epsb[:], EPS), writes=["epsb"])

    P.op("act", lambda e: e.activation(out=sc3, in_=c3_sb, func=AF.Silu), reads=["c3"], writes=["sc3"])
    for k in range(8):
        P.op("pe", lambda e, k=k: e.transpose(out=bank(0)[:, k * 3:k * 3 + 3], in_=sc3[:, k * 128:(k + 1) * 128],
                                              identity=identf[0:3, 0:3]), reads=["sc3", "identf"], writes=["ps:0"])
    P.op("dve", lambda e: e.tensor_copy(out=scT[:].rearrange("p a b -> p (a b)"), in_=bank(0)[:, 0:24]),
         reads=["ps:0"], writes=["scT"])

    psT = bank(3)
    for n in range(12):
        seg, hh = n // 2, n % 2
        wmb = wm[n % 3]
        load("pool", wmb, w_mod[:, n * 512:(n + 1) * 512].rearrange("(k p) n -> p k n", p=128), "wm%d" % (n % 3))
        pm = bank(1 + n % 2)
        pk = "ps:%d" % (1 + n % 2)
        for k in range(8):
            P.op("pe", lambda e, k=k, wmb=wmb, pm=pm: e.matmul(pm[0:3, :], lhsT=scT[:, k, :], rhs=wmb[:, k, :],
                                                               start=(k == 0), stop=(k == 7)),
                 reads=["scT", "wm%d" % (n % 3)], writes=[pk])
        mt = mtmp[n % 2]
        mk = "mtmp%d" % (n % 2)
        P.op("dve", lambda e, pm=pm, mt=mt, n=n: e.tensor_tensor(out=mt, in0=pm[0:3, :],
                                                                in1=bmod_sb[:, n * 512:(n + 1) * 512], op=ALU.add),
             reads=[pk, "bmod"], writes=[mk])
        for j in range(4):
            cidx = (n * 4 + j) * 3
            P.op("pe", lambda e, mt=mt, j=j, cidx=cidx: e.transpose(out=psT[:, cidx:cidx + 3],
                                                                   in_=mt[:, j * 128:(j + 1) * 128],
                                                                   identity=identf[0:3, 0:3]),
                 reads=[mk, "identf"], writes=["ps:3"])
        if seg == 2:
            P.op("act", lambda e, mt=mt, hh=hh: e.copy(out=msb[:, 0, hh * 512:(hh + 1) * 512], in_=mt),
                 reads=[mk], writes=["msb"])
        elif seg == 3:
            P.op("act", lambda e, mt=mt, hh=hh: e.copy(out=msb[:, 2, hh * 512:(hh + 1) * 512], in_=mt),
                 reads=[mk], writes=["msb"])
        elif seg == 5:
            P.op("act", lambda e, mt=mt, hh=hh: e.copy(out=msb[:, 3, hh * 512:(hh + 1) * 512], in_=mt),
                 reads=[mk], writes=["msb"])
        elif seg == 4:
            P.op("dve", lambda e, mt=mt, hh=hh: e.scalar_tensor_tensor(
                out=msb[:, 1, hh * 512:(hh + 1) * 512], in0=mt, scalar=1.0,
                in1=rows3_sb[:, hh * 512:(hh + 1) * 512], op0=ALU.add, op1=ALU.mult),
                 reads=[mk, "rows3"], writes=["msb"])
    P.op("dve", lambda e: e.tensor_copy(out=mT[:].rearrange("p a b -> p (a b)"), in_=psT[:, 0:144]),
         reads=["ps:3"], writes=["mT"])
    P.op("pe", lambda e: e.transpose(out=bank(0)[:, 32:48], in_=g2_sb, identity=identf[0:16, 0:16]),
         reads=["g2", "identf"], writes=["ps:0"])
    P.op("dve", lambda e: e.tensor_copy(out=gT[:], in_=bank(0)[:, 32:48]), reads=["ps:0"], writes=["gT"])
    P.op("dve", lambda e: e.scalar_tensor_tensor(out=s1T[:], in0=mT[:, 8:16, :], scalar=1.0,
                                                 in1=gT[:, 0:8].unsqueeze(2).to_broadcast([128, 8, 3]),
                                                 op0=ALU.add, op1=ALU.mult),
         reads=["mT", "gT"], writes=["s1T"])
    for (dst, c0, n_) in ((gfinbc[:, 0:512], 1024, 512), (gfinbc[:, 512:1024], 1536, 512),
                          (gain640[:, 0:512], 2048, 512), (gain640[:, 512:640], 2560, 128),
                          (vgainbc[:, 0:512], 2688, 512)):
        P.op("pe", lambda e, c0=c0, n_=n_: e.matmul(bank(4)[:, 0:n_], lhsT=sel3[:, 0, :], rhs=rows3_sb[:, c0:c0 + n_],
                                                    start=True, stop=True), reads=["sel3", "rows3"], writes=["ps:4"])
        P.op("act", lambda e, dst=dst, n_=n_: e.copy(out=dst, in_=bank(4)[:, 0:n_]), reads=["ps:4"], writes=["bcst"])
    for g in range(8):
        P.op("pe", lambda e, g=g: e.transpose(out=bank(5 + g // 4)[:, (g % 4) * 128:(g % 4) * 128 + 128],
                                              in_=wstmp[:, g, :], identity=identf[:]),
             reads=["wstmp", "identf"], writes=["ps:%d" % (5 + g // 4)])
    for h_ in range(2):
        P.op("act", lambda e, h_=h_: e.copy(out=wsT[:, h_ * 4:(h_ + 1) * 4, :].rearrange("p a b -> p (a b)"),
                                            in_=bank(5 + h_)), reads=["ps:%d" % (5 + h_)], writes=["wsT"])
    P.op("pe", lambda e: e.transpose(out=bank(0)[:, 64:72], in_=bs_sb, identity=identf[0:8, 0:8]),
         reads=["bs", "identf", "gT"], writes=["ps:0"])
    P.op("dve", lambda e: e.tensor_copy(out=bsT[:], in_=bank(0)[:, 64:72]), reads=["ps:0"], writes=["bsT"])
    P.barrier_all()
    if stage == 0:
        P.dma("sync", lambda e: e.dma_start(out=y[0, 0:128, :], in_=gfinbc[:]), d_outs[0], reads=["bcst"])
        P.dma("sync", lambda e: e.dma_start(out=y[0, 128:256, 0:640], in_=gain640[:]), d_outs[0], reads=["bcst"])
        P.dma("sync", lambda e: e.dma_start(out=y[0, 256:384, 0:144], in_=mT[:].rearrange("p a b -> p (a b)")), d_outs[0])
        P.dma("sync", lambda e: e.dma_start(out=y[0, 384:387, :], in_=msb[:, 1, :]), d_outs[0])
        P.dma("sync", lambda e: e.dma_start(out=y[0, 512:640, 0:24], in_=s1T[:].rearrange("p a b -> p (a b)")), d_outs[0])
        P.final_wait("sync", d_outs)
        P.emit()
        return nc

    def rstd_chain(col, scale):
        P.op("act", lambda e: e.activation(out=st[:, col + 1:col + 2], in_=st[:, col:col + 1], func=AF.Sqrt,
                                           scale=scale, bias=epsb[:, 0:1]),
             reads=["st%d" % col, "epsb"], writes=["st%d" % (col + 1)])
        P.op("dve", lambda e: e.reciprocal(out=st[:, col + 1:col + 2], in_=st[:, col + 1:col + 2]),
             reads=["st%d" % (col + 1)], writes=["st%d" % (col + 1)])

    def epoch_ac(b):
        cv = Carver()
        wio = cv.get([128, 8, 1792], BF16)
        qa = cv.get([128, 4, T], BF16)
        gmT = cv.get([128, 4, T], BF16)
        kT = cv.get([128, 2, 2304], BF16)
        vaug = cv.get([128, 18, 2, 192], BF16)
        xt = [cv.get([128, D], F32) for _ in range(2)]
        sqj = cv.get([128, D], BF16)
        xn = cv.get([128, D], BF16)
        hTt = cv.get([128, 8, 128], BF16)
        qk = cv.get([128, 10, 64], F32)
        qk2 = cv.get([128, 10, 64], F32)
        qkr = cv.get([128, 10, 64], BF16)
        kd = cv.get([128, 4, 64], BF16)
        rp = cv.get([128, 4, 320], F32)
        u_sb = cv.get([128, 512], F32)
        vv_sb = cv.get([128, 512], F32)
        vvn = cv.get([128, 512], BF16)
        gtmp = cv.get([128, 512], F32)
        gmb = cv.get([128, 512], BF16)
        PT = [cv.get([128, 1024], BF16) for _ in range(4)]
        rd = cv.get([128, 512], F32)
        gt1bc = cv.get([128, D], F32)
        tmpo = cv.get([128, D], F32)
        s10 = st[:, 16:26]
        r10 = st[:, 32:42]

        for kk in range(4):
            load("pool", wio[:, 2 * kk:2 * kk + 2, :],
                 w_in[kk * 256:(kk + 1) * 256, :].rearrange("(k p) n -> p k n", p=128), "wio")
        for hh in range(2):
            P.op("pe", lambda e, hh=hh: e.matmul(bank(0), lhsT=sel3[:, b, :], rhs=msb[:, 0, hh * 512:(hh + 1) * 512],
                                                 start=True, stop=True), reads=["sel3", "msb"], writes=["ps:0"])
            P.op("act", lambda e, hh=hh: e.copy(out=gt1bc[:, hh * 512:(hh + 1) * 512], in_=bank(0)),
                 reads=["ps:0"], writes=["gt1bc"])
        P.op("pool!", lambda e: e.memset(vaug.rearrange("p a b c -> p (a b c)"), 1.0), writes=["vaug"])

        g3 = gain640[:].rearrange("p (a b) -> p a b", a=10)
        qkf = qk.rearrange("p a b -> p (a b)")
        qkrf = qkr.rearrange("p a b -> p (a b)")
        kdf = kd.rearrange("p a b -> p (a b)")
        kdv = kd.rearrange("p (a b) c -> p a b c", a=2)
        rpv = [rp[:, i, :].rearrange("p (a b) -> p a b", a=10) for i in range(4)]
        pA = bankb(0)
        pB = bankb(5)
        pC = bankb(7)

        def X1(t):
            lat = t < 16
            xb = xt[t % 2]
            xk = "xt%d" % (t % 2)
            src = x[b, t * 128:(t + 1) * 128, :] if lat else ctx[b, (t - 16) * 128:(t - 15) * 128, :]
            load("sync", xb, src, xk)
            P.op("act", lambda e: e.activation(out=sqj, in_=xb, func=AF.Square, accum_out=st[:, 0:1]),
                 reads=[xk], writes=["sqj", "st0"])
            rstd_chain(0, 1.0 / D)
            P.op("act", lambda e: e.activation(out=xn, in_=xb, func=AF.Copy, scale=st[:, 1:2]),
                 reads=[xk, "st1"], writes=["xn"])
            for k in range(8):
                P.op("pe", lambda e, k=k: e.transpose(out=pA[:, k * 128:(k + 1) * 128], in_=xn[:, k * 128:(k + 1) * 128],
                                                      identity=identb[:]), reads=["xn", "identb"], writes=["ps:0"])

        def X2(t):
            lat = t < 16
            j = b if lat else 2
            for k in range(8):
                eng_ = "dve"
                if eng_ == "act":
                    P.op("act", lambda e, k=k: e.activation(out=hTt[:, k, :], in_=pA[:, k * 128:(k + 1) * 128],
                                                            func=AF.Identity, scale=s1T[:, k, j:j + 1],
                                                            bias=mT[:, k, j:j + 1]),
                         reads=["ps:0", "s1T", "mT"], writes=["hTt%d" % k])
                else:
                    P.op("dve", lambda e, k=k: e.tensor_scalar(out=hTt[:, k, :], in0=pA[:, k * 128:(k + 1) * 128],
                                                              scalar1=s1T[:, k, j:j + 1], scalar2=mT[:, k, j:j + 1],
                                                              op0=ALU.mult, op1=ALU.add),
                         reads=["ps:0", "s1T", "mT"], writes=["hTt%d" % k])
            slices = [(1, 0, 512), (2, 512, 256), (3, 768, 512), (4, 1280, 512)] if lat else [(2, 512, 256)]
            for (bi, c0, n_) in slices:
                for k in range(8):
                    P.op("pe", lambda e, bi=bi, c0=c0, n_=n_, k=k: e.matmul(bank(bi)[:, 0:n_], lhsT=hTt[:, k, :],
                                                                           rhs=wio[:, k, c0:c0 + n_],
                                                                           start=(k == 0), stop=(k == 7)),
                         reads=["hTt%d" % k, "wio"], writes=["ps:%d" % bi])

        def Ya(t):
            lat = t < 16
            if lat:
                P.op("dve", lambda e: e.tensor_copy(out=qkf[:, 0:512], in_=bank(1)), reads=["ps:1"], writes=["qk"])
            P.op("dve", lambda e: e.tensor_copy(out=qkf[:, 512:640], in_=bank(2)[:, 0:128]), reads=["ps:2"], writes=["qk"])
            P.op("act", lambda e: e.copy(out=vaug[:, t, :, 64:128],
                                         in_=bank(2)[:, 128:256].rearrange("p (a b) -> p a b", a=2)),
                 reads=["ps:2"], writes=["vaug"])
            if lat:
                P.op("act", lambda e: e.activation(out=u_sb, in_=bank(3), func=AF.Gelu), reads=["ps:3"], writes=["u_sb"])
                P.op("act", lambda e: e.activation(out=vv_sb, in_=bank(4), func=AF.Gelu), reads=["ps:4"], writes=["vv_sb"])

        def Yb(t):
            lat = t < 16
            h0 = 0 if lat else 8
            nh = 10 - h0
            if lat:
                P.op("act", lambda e: e.activation(out=gtmp, in_=vv_sb, func=AF.Square, accum_out=st[:, 4:5]),
                     reads=["vv_sb"], writes=["gtmp", "st4"])
                P.op("act", lambda e: e.activation(out=st[:, 5:6], in_=st[:, 4:5], func=AF.Sqrt, scale=1.0 / 512,
                                                   bias=epsb[:, 0:1]), reads=["st4", "epsb"], writes=["st5"])
            P.op("dve", lambda e: e.tensor_tensor(out=qk2[:, h0:10, :], in0=qk[:, h0:10, :], in1=qk[:, h0:10, :],
                                                  op=ALU.mult), reads=["qk"], writes=["qk2"])
            P.op("dve", lambda e: e.tensor_reduce(out=s10[:, h0:10], in_=qk2[:, h0:10, :], axis=AX.X, op=ALU.add),
                 reads=["qk2"], writes=["s10"])
            P.op("act", lambda e: e.activation(out=r10[:, h0:10], in_=s10[:, h0:10], func=AF.Sqrt,
                                               scale=1.0 / 64, bias=epsb[:, 0:1]),
                 reads=["s10", "epsb"], writes=["r10"])
            if lat:
                P.op("dve", lambda e: e.reciprocal(out=st[:, 5:6], in_=st[:, 5:6]), reads=["st5"], writes=["st5"])
                P.op("dve", lambda e: e.scalar_tensor_tensor(out=vvn, in0=vv_sb, scalar=st[:, 5:6], in1=vgainbc[:],
                                                             op0=ALU.mult, op1=ALU.mult),
                     reads=["vv_sb", "st5", "bcst"], writes=["vvn"])
                for g in range(8):
                    P.op("pe", lambda e, g=g: e.matmul(bank(6)[:, g * 64:(g + 1) * 64], lhsT=wsT[:, g, :],
                                                       rhs=vvn[:, g * 64:(g + 1) * 64], start=True, stop=True),
                         reads=["wsT", "vvn"], writes=["ps:6"])
            P.op("dve", lambda e: e.reciprocal(out=r10[:, h0:10], in_=r10[:, h0:10]), reads=["r10"], writes=["r10"])
            P.op("dve", lambda e: e.tensor_tensor(out=qk2[:, h0:10, :], in0=qk[:, h0:10, :],
                                                  in1=r10[:, h0:10].unsqueeze(2).to_broadcast([128, nh, 64]),
                                                  op=ALU.mult), reads=["qk", "r10"], writes=["qk2"])
            P.op("dve", lambda e: e.tensor_tensor(out=qk2[:, h0:10, :], in0=qk2[:, h0:10, :], in1=g3[:, h0:10, :],
                                                  op=ALU.mult), reads=["qk2", "bcst"], writes=["qk2"])
            if lat:
                cosb = cs[:, t, 0:32].unsqueeze(1).to_broadcast([128, 10, 32])
                sinb = cs[:, t, 32:64].unsqueeze(1).to_broadcast([128, 10, 32])
                x1v = qk2[:, :, 0:32]
                x2v = qk2[:, :, 32:64]
                P.op("dve", lambda e: e.tensor_tensor(out=rpv[0], in0=x1v, in1=cosb, op=ALU.mult),
                     reads=["qk2", "cs"], writes=["rp0"])
                P.op("dve", lambda e: e.tensor_tensor(out=rpv[1], in0=x2v, in1=sinb, op=ALU.mult),
                     reads=["qk2", "cs"], writes=["rp1"])
                P.op("pool", lambda e: e.tensor_tensor(out=rpv[2], in0=x1v, in1=sinb, op=ALU.mult),
                     reads=["qk2", "cs"], writes=["rp2"])
                P.op("pool", lambda e: e.tensor_tensor(out=rpv[3], in0=x2v, in1=cosb, op=ALU.mult),
                     reads=["qk2", "cs"], writes=["rp3"])
                P.op("dve", lambda e: e.tensor_tensor(out=qkr[:, :, 0:32], in0=rpv[0], in1=rpv[1], op=ALU.subtract),
                     reads=["rp0", "rp1"], writes=["qkr"])
                P.op("pool", lambda e: e.tensor_tensor(out=qkr[:, :, 32:64], in0=rpv[2], in1=rpv[3], op=ALU.add),
                     reads=["rp2", "rp3"], writes=["qkr"])
            else:
                P.op("dve", lambda e: e.tensor_copy(out=qkr[:, 8:10, :], in_=qk2[:, 8:10, :]), reads=["qk2"], writes=["qkr"])
            P.op("pool", lambda e: e.tensor_copy(out=kdv, in_=qkr[:, 8:10, :].unsqueeze(2).to_broadcast([128, 2, 2, 64])),
                 reads=["qkr"], writes=["kd"])
            if lat:
                for c in range(4):
                    P.op("pe", lambda e, c=c: e.transpose(out=pB[:, c * 128:(c + 1) * 128], in_=qkrf[:, c * 128:(c + 1) * 128],
                                                          identity=identb[:]), reads=["qkr", "identb"], writes=["ps:5"])
            for kv in range(2):
                P.op("pe", lambda e, kv=kv: e.transpose(out=pB[:, 512 + kv * 128:512 + (kv + 1) * 128],
                                                        in_=kdf[:, kv * 128:(kv + 1) * 128], identity=identb[:]),
                     reads=["kd", "identb"], writes=["ps:5"])
            if lat:
                P.op("act", lambda e: e.copy(out=qa[:, :, t * 128:(t + 1) * 128],
                                             in_=pB[:, 0:512].rearrange("p (a b) -> p a b", a=4)),
                     reads=["ps:5"], writes=["qa"])
            P.op("act", lambda e: e.copy(out=kT[:, :, t * 128:(t + 1) * 128],
                                         in_=pB[:, 512:768].rearrange("p (a b) -> p a b", a=2)),
                 reads=["ps:5"], writes=["kT"])
            if not lat:
                return
            P.op("dve", lambda e: e.tensor_tensor(out=gtmp.rearrange("p (a b) -> p a b", a=8),
                                                  in0=bank(6).rearrange("p (a b) -> p a b", a=8),
                                                  in1=bsT[:].unsqueeze(2).to_broadcast([128, 8, 64]), op=ALU.add),
                 reads=["ps:6", "bsT"], writes=["gtmp"])
            P.op("pool", lambda e: e.tensor_tensor(out=gmb, in0=gtmp, in1=u_sb, op=ALU.mult),
                 reads=["gtmp", "u_sb"], writes=["gmb"])
            for c in range(4):
                P.op("pe", lambda e, c=c: e.transpose(out=pC[:, c * 128:(c + 1) * 128], in_=gmb[:, c * 128:(c + 1) * 128],
                                                      identity=identb[:]), reads=["gmb", "identb"], writes=["ps:7"])
            P.op("act", lambda e: e.copy(out=gmT[:, :, t * 128:(t + 1) * 128],
                                         in_=pC[:, 0:512].rearrange("p (a b) -> p a b", a=4)),
                 reads=["ps:7"], writes=["gmT"])

        NTA = 18
        X1(0)
        X2(0)
        X1(1)
        for t in range(NTA):
            Ya(t)
            if t + 1 < NTA:
                X2(t + 1)
            if t + 2 < NTA:
                X1(t + 2)
            Yb(t)

        P.barrier_all()
        steps = [(c, qg, sc_) for c in range(4) for qg in range(4) for sc_ in range(18)]
        Osb = cv.get([128, 1024], F32)

        def qk_step(i):
            c, qg, sc_ = steps[i]
            kv = c // 2
            sj = (0, 1, 3)[i % 3]
            S = pp[sj]
            for half in range(2):
                r0 = half * 64
                P.op("pe", lambda e, half=half, r0=r0: e.matmul(
                    S[:, half * 512:(half + 1) * 512], lhsT=kT[r0:r0 + 64, kv, sc_ * 128:(sc_ + 1) * 128],
                    rhs=qa[r0:r0 + 64, c, qg * 512:(qg + 1) * 512], start=True, stop=True),
                     reads=["kT", "qa%d_%d_%d" % (c, half, qg), "qa"], writes=BP(sj))
            P.op("act", lambda e: e.activation(out=PT[i % 4], in_=S[:], func=AF.Exp, scale=0.125),
                 reads=BP(sj), writes=["PT%d" % (i % 4)])

        def pv_step(i):
            c, qg, sc_ = steps[i]
            kv = c // 2
            for half in range(2):
                off = 64 if half == 0 else 0
                P.op("pe", lambda e, half=half, off=off: e.matmul(
                    bank(4 + half), lhsT=vaug[:, sc_, kv, off:off + 128],
                    rhs=PT[i % 4][:, half * 512:(half + 1) * 512], start=(sc_ == 0), stop=(sc_ == 17)),
                     reads=["vaug", "PT%d" % (i % 4)], writes=["ps:%d" % (4 + half)])
            if sc_ == 17:
                P.op("dve", lambda e: e.tensor_copy(out=Osb, in_=pp[2][:]), reads=BP(2), writes=["Osb"])
                for half in range(2):
                    nr = half * 64
                    dr = 64 - nr
                    P.op("dve", lambda e, half=half, nr=nr, dr=dr: e.reciprocal(
                        out=rd[nr:nr + 64, :], in_=Osb[dr:dr + 64, half * 512:(half + 1) * 512]),
                         reads=["Osb"], writes=["rd%d" % half])
                    P.op("dve", lambda e, half=half, nr=nr: e.tensor_tensor(
                        out=qa[nr:nr + 64, c, qg * 512:(qg + 1) * 512], in0=Osb[nr:nr + 64, half * 512:(half + 1) * 512],
                        in1=rd[nr:nr + 64, :], op=ALU.mult),
                         reads=["Osb", "rd%d" % half], writes=["qa%d_%d_%d" % (c, half, qg)])

        if stage >= 2:
            n = len(steps)
            qk_step(0)
            qk_step(1)
            for i in range(n):
                if i + 2 < n:
                    qk_step(i + 2)
                pv_step(i)

        P.barrier_all()
        for kk in range(4):
            load("pool", wio[:, 2 * kk:2 * kk + 2, 0:1024],
                 w_out[kk * 256:(kk + 1) * 256, :].rearrange("(k p) n -> p k n", p=128), "wio")
        tmpo2 = [tmpo, Osb]
        for t in range(16):
            xb = xt[t % 2]
            xk = "xt%d" % (t % 2)
            load("sync", xb, x[b, t * 128:(t + 1) * 128, :], xk)
            pj = 2 + t % 2
            pO = pp[pj]
            tb = tmpo2[t % 2]
            tk = "tmpo%d" % (t % 2)
            for hh in range(2):
                for k in range(8):
                    src = qa[:, k, t * 128:(t + 1) * 128] if k < 4 else gmT[:, k - 4, t * 128:(t + 1) * 128]
                    rk = ["qa%d_%d_%d" % (k, hf, t // 4) for hf in range(2)] + ["qa"] if k < 4 else ["gmT"]
                    P.op("pe", lambda e, hh=hh, k=k, src=src, pO=pO: e.matmul(pO[:, hh * 512:(hh + 1) * 512], lhsT=src,
                                                                             rhs=wio[:, k, hh * 512:(hh + 1) * 512],
                                                                             start=(k == 0), stop=(k == 7)),
                         reads=rk + ["wio"], writes=BP(pj))
            P.op("dve", lambda e, pO=pO, tb=tb: e.tensor_tensor(out=tb, in0=pO[:], in1=gt1bc, op=ALU.mult),
                 reads=BP(pj) + ["gt1bc"], writes=[tk])
            P.op("pool", lambda e, xb=xb, tb=tb: e.tensor_tensor(out=xb, in0=xb, in1=tb, op=ALU.add),
                 reads=[tk, xk], writes=[xk])
            dst = x1s[b, t * 128:(t + 1) * 128, :] if stage >= 3 else y[b, t * 128:(t + 1) * 128, :]
            P.dma("sync", lambda e, xb=xb, dst=dst: e.dma_start(out=dst, in_=xb), d_outs[t % 2],
                  reads=[xk], writes=["x1s%d_%d" % (b, t)])

    for b in range(2):
        epoch_ac(b)
        P.barrier_all()


    def epoch_d():
        cv = Carver()
        h2 = cv.get([128, 2 * NT, D], BF16)
        x1t = [cv.get([128, D], F32) for _ in range(2)]
        sqj = cv.get([128, D], BF16)
        tmp = cv.get([128, D], F32)
        s2bc = cv.get([128, D], F32)
        sh2bc = cv.get([128, D], F32)
        h2Tt = cv.get([128, D], BF16)
        ex = cv.get([128, 16], F32)
        aff2 = cv.get([128, NT, 48], F32)
        affT = cv.get([48, T], F32, parts=48)
        work = cv.get([48, T], F32, parts=48)
        mx8 = cv.get([48, 8], F32, parts=48)
        gate2 = cv.get([128, NT, 48], F32)
        mask2 = cv.get([128, NT, 48], BF16)
        mask2f = cv.get([128, NT, 48], F32)
        totsb = cv.get([128, NT, 48], F32)
        base = cv.get([128, NT, 48], F32)
        pos = cv.get([128, NT, 48], F32)
        ghf = cv.get([128, NT, 48], F32)
        gh = cv.get([128, NT, 48], BF16)
        fl = lambda v: v.rearrange("p a b -> p (a b)")

        P.op("pool!", lambda e: e.memset(fl(aff2), 0.0), writes=["aff2"])
        d_h2s = [P.dsem("h2s0"), P.dsem("h2s1")]
        tokf = cv.get([128, NT * 48, 2], F32)
        load("sync", tokf.rearrange("p a b -> p (a b)"), k_tokc, "tokf")
        P.op("dve", lambda e: e.tensor_copy(out=ghl[:].rearrange("p a b c -> p (a b) c")[:, :, 2:4], in_=tokf),
             reads=["tokf"], writes=["ghl23"])
        for b in range(2):
            for (dst, ri, key) in ((s2bc, 1, "s2bc"), (sh2bc, 2, "sh2bc")):
                for hh in range(2):
                    P.op("pe", lambda e, ri=ri, hh=hh, b=b: e.matmul(bank(0), lhsT=sel3[:, b, :],
                                                               rhs=msb[:, ri, hh * 512:(hh + 1) * 512],
                                                               start=True, stop=True), reads=[], writes=["ps:0"])
                    P.op("act", lambda e, dst=dst, hh=hh: e.copy(out=dst[:, hh * 512:(hh + 1) * 512], in_=bank(0)),
                         reads=["ps:0"], writes=[key])
            def DX(t, b=b):
                xb = x1t[t % 2]
                xk = "x1t%d" % (t % 2)
                load("sync", xb, x1s[b, t * 128:(t + 1) * 128, :], xk)
                P.op("act", lambda e: e.activation(out=sqj, in_=xb, func=AF.Square, accum_out=st[:, 0:1]),
                     reads=[xk], writes=["sqj", "st0"])
                rstd_chain(0, 1.0 / D)
                P.op("dve", lambda e: e.scalar_tensor_tensor(out=tmp, in0=xb, scalar=st[:, 1:2], in1=s2bc,
                                                             op0=ALU.mult, op1=ALU.mult),
                     reads=[xk, "st1", "s2bc"], writes=["tmp"])
                h2t = h2[:, b * NT + t, :]
                P.op("pool", lambda e: e.tensor_tensor(out=h2t, in0=tmp, in1=sh2bc, op=ALU.add),
                     reads=["tmp", "sh2bc"], writes=["h2t"])
                P.dma("sync", lambda e: e.dma_start(out=h2s[b * T + t * 128:b * T + (t + 1) * 128, :], in_=h2t),
                      d_h2s[t % 2], reads=["h2t"])
                pA = bankb(1)
                for k in range(8):
                    P.op("pe", lambda e, k=k: e.transpose(out=pA[:, k * 128:(k + 1) * 128],
                                                          in_=h2t[:, k * 128:(k + 1) * 128], identity=identb[:]),
                         reads=["h2t"], writes=["ps:1"])
                P.op("act", lambda e: e.copy(out=h2Tt, in_=pA), reads=["ps:1"], writes=["h2Tt"])
                lg = bank(2)[:, t * 16:(t + 1) * 16]
                for k in range(8):
                    P.op("pe", lambda e, k=k: e.matmul(lg, lhsT=h2Tt[:, k * 128:(k + 1) * 128], rhs=wr[:, k, :],
                                                       start=(k == 0), stop=(k == 7)),
                         reads=["h2Tt"], writes=["ps:2"])

            def DY(t, b=b):
                lg = bank(2)[:, t * 16:(t + 1) * 16]
                P.op("dve", lambda e: e.tensor_reduce(out=st[:, 8:9], in_=lg, axis=AX.X, op=ALU.max),
                     reads=["ps:2"], writes=["st8"])
                P.op("dve", lambda e: e.tensor_scalar(out=st[:, 9:10], in0=st[:, 8:9], scalar1=-1.0, scalar2=None,
                                                      op0=ALU.mult), reads=["st8"], writes=["st9"])
                P.op("act", lambda e: e.activation(out=ex, in_=lg, func=AF.Exp, bias=st[:, 9:10],
                                                   accum_out=st[:, 10:11]),
                     reads=["ps:2", "st9"], writes=["ex", "st10"])
                P.op("dve", lambda e: e.reciprocal(out=st[:, 11:12], in_=st[:, 10:11]), reads=["st10"], writes=["st11"])
                P.op("dve", lambda e: e.tensor_scalar(out=aff2[:, t, 32 * b:32 * b + 16], in0=ex,
                                                      scalar1=st[:, 11:12], scalar2=None, op0=ALU.mult),
                     reads=["ex", "st11"], writes=["aff2"])

            DX(0)
            for t in range(NT):
                if t + 1 < NT:
                    DX(t + 1)
                DY(t)
        for t in range(NT):
            P.op("pe", lambda e, t=t: e.transpose(out=pp[2 + t // 8][0:48, (t % 8) * 128:(t % 8) * 128 + 128],
                                                  in_=aff2[:, t, :], identity=identf[:]),
                 reads=["aff2"], writes=BP(2 + t // 8))
        for hh in range(2):
            P.op("act", lambda e, hh=hh: e.copy(out=affT[:, hh * 1024:(hh + 1) * 1024], in_=pp[2 + hh][0:48, :]),
                 reads=BP(2 + hh), writes=["affT"])
            P.op("dve", lambda e, hh=hh: e.tensor_copy(out=work[:, hh * 1024:(hh + 1) * 1024], in_=pp[2 + hh][0:48, :]),
                 reads=BP(2 + hh), writes=["work"])
        for r in range(CAP // 8):
            P.op("dve", lambda e: e.max(out=mx8, in_=work), reads=["work"], writes=["mx8"])
            P.op("dve", lambda e: e.match_replace(out=work, in_to_replace=mx8, in_values=work, imm_value=0.0),
                 reads=["work", "mx8"], writes=["work"])
        P.op("dve", lambda e: e.tensor_tensor(out=work, in0=affT, in1=work, op=ALU.subtract),
             reads=["affT", "work"], writes=["work"])
        for t in range(NT):
            P.op("pe", lambda e, t=t: e.transpose(out=pp[t // 8][:, (t % 8) * 64:(t % 8) * 64 + 48],
                                                  in_=work[:, t * 128:(t + 1) * 128], identity=identf[0:48, 0:48]),
                 reads=["work"], writes=BP(t // 8))
        for hh in range(2):
            P.op("act", lambda e, hh=hh: e.copy(out=gate2[:, hh * 8:(hh + 1) * 8, :],
                                                in_=pp[hh][:, 0:512].rearrange("p (a b) -> p a b", a=8)[:, :, 0:48]),
                 reads=BP(hh), writes=["gate2"])
        P.op("dve", lambda e: e.tensor_scalar(out=fl(mask2), in0=fl(gate2), scalar1=0.0, scalar2=None, op0=ALU.is_gt),
             reads=["gate2"], writes=["mask2"])
        P.op("dve", lambda e: e.tensor_scalar(out=fl(mask2f), in0=fl(gate2), scalar1=0.0, scalar2=None, op0=ALU.is_gt),
             reads=["gate2"], writes=["mask2f"])
        for hh in range(2):
            P.op("pe", lambda e, hh=hh: e.matmul(bank(4 + hh)[:, 0:384], lhsT=trib[:], rhs=fl(mask2)[:, hh * 384:(hh + 1) * 384],
                                                 start=True, stop=True), reads=["mask2"], writes=["ps:%d" % (4 + hh)])
            P.op("pe", lambda e, hh=hh: e.matmul(bank(6 + hh)[:, 0:384], lhsT=onesb[:], rhs=fl(mask2)[:, hh * 384:(hh + 1) * 384],
                                                 start=True, stop=True), reads=["mask2"], writes=["ps:%d" % (6 + hh)])
            P.op("act", lambda e, hh=hh: e.copy(out=fl(totsb)[:, hh * 384:(hh + 1) * 384], in_=bank(6 + hh)[:, 0:384]),
                 reads=["ps:%d" % (6 + hh)], writes=["totsb"])
        P.op("dve", lambda e: e.memset(base[:, 0, :], 0.0), writes=["base"])
        for t in range(1, NT):
            P.op("dve", lambda e, t=t: e.tensor_tensor(out=base[:, t, :], in0=base[:, t - 1, :], in1=totsb[:, t - 1, :],
                                                      op=ALU.add), reads=["base", "totsb"], writes=["base"])
        for hh in range(2):
            P.op("dve", lambda e, hh=hh: e.tensor_tensor(out=fl(pos)[:, hh * 384:(hh + 1) * 384], in0=bank(4 + hh)[:, 0:384],
                                                        in1=fl(base)[:, hh * 384:(hh + 1) * 384], op=ALU.add),
                 reads=["ps:%d" % (4 + hh), "base"], writes=["pos"])
        P.op("dve", lambda e: e.scalar_tensor_tensor(out=fl(pos), in0=fl(pos), scalar=1.0, in1=fl(mask2f),
                                                     op0=ALU.add, op1=ALU.mult), reads=["pos", "mask2f"], writes=["pos"])
        P.op("dve", lambda e: e.tensor_scalar(out=fl(posm[:]), in0=fl(pos), scalar1=-1.0, scalar2=None, op0=ALU.add),
             reads=["pos"], writes=["posm"])
        for t in range(NT):
            P.op("pe", lambda e, t=t: e.transpose(out=pp[2 + t // 8][0:48, (t % 8) * 128:(t % 8) * 128 + 128],
                                                  in_=posm[:, t, :], identity=identf[:]),
                 reads=["posm"], writes=BP(2 + t // 8))
        for hh in range(2):
            P.op("act", lambda e, hh=hh: e.copy(out=posmT[:, hh * 1024:(hh + 1) * 1024], in_=pp[2 + hh][0:48, :]),
                 reads=BP(2 + hh), writes=["posmT"])
        P.op("dve", lambda e: e.tensor_copy(out=fl(gh), in_=fl(gate2)), reads=["gate2"], writes=["gh"])
        P.op("dve", lambda e: e.tensor_copy(out=fl(ghf), in_=fl(gh)), reads=["gh"], writes=["ghf"])
        P.op("dve", lambda e: e.tensor_tensor(out=ghl[:, :, :, 1], in0=gate2, in1=ghf, op=ALU.subtract),
             reads=["gate2", "ghf"], writes=["ghl1"])
        P.op("dve", lambda e: e.tensor_copy(out=ghl[:, :, :, 0], in_=gh), reads=["gh"], writes=["ghl0"])

    def epoch_e():
        cv = Carver()
        ring = [cv.get([128, 8, 512], BF16) for _ in range(8)]
        S = [cv.get([128, NT, 256], BF16) for _ in range(2)]
        xeT = cv.get([128, 8, 512], BF16)
        hidT = cv.get([128, 16, 512], BF16)
        sil = [cv.get([128, 512], F32) for _ in range(2)]
        ye_sb = cv.get([128, 4, D], F32)
        gt2bc = cv.get([128, 2, D], F32)
        pgs = cv.get([128, 2, 16], F32)
        idxf = cv.get([128, 2, 4], F32)
        gsb = cv.get([128, 2, 4], F32)
        xetok = [cv.get([128, 4, D], BF16) for _ in range(2)]
        d_sc = [P.dsem("scat%d" % gi) for gi in range(4)]
        d_g = [[P.dsem("gath%d_%d" % (pp_, gi)) for gi in range(4)] for pp_ in range(2)]
        x1flat = x1s.rearrange("b t d -> (b t) d")
        for b in range(2):
            for hh in range(2):
                P.op("pe", lambda e, b=b, hh=hh: e.matmul(bank(6), lhsT=sel3[:, b, :], rhs=msb[:, 3, hh * 512:(hh + 1) * 512],
                                                         start=True, stop=True), reads=[], writes=["ps:6"])
                P.op("act", lambda e, b=b, hh=hh: e.copy(out=gt2bc[:, b, hh * 512:(hh + 1) * 512], in_=bank(6)),
                     reads=["ps:6"], writes=["gt2bc"])
        NRING = 8
        uspec = []
        for e2 in range(NE):
            for g in range(4):
                uspec.append(("a", w1[e2][:, g * 512:(g + 1) * 512].rearrange("(k p) f -> p k f", p=128)))
                uspec.append(("a", w3[e2][:, g * 512:(g + 1) * 512].rearrange("(k p) f -> p k f", p=128)))
            for dq in range(4):
                uspec.append(("b", [w2[e2][hf * 1024:(hf + 1) * 1024, dq * 256:(dq + 1) * 256]
                                    .rearrange("(c p) d -> p c d", p=128) for hf in range(2)]))
        issued = [0]

        def uview(u):
            s_ = u % NRING
            if uspec[u][0] == "a":
                return ring[s_], "ring%d" % s_
            return ring[s_].rearrange("p a b -> p (a b)").rearrange("p (c d) -> p c d", c=16), "ring%d" % s_

        def ensure_issued(upto):
            upto = min(upto, len(uspec) - 1)
            while issued[0] <= upto:
                u = issued[0]
                v, key = uview(u)
                if uspec[u][0] == "a":
                    load("pool", v, uspec[u][1], key)
                else:
                    for hf in range(2):
                        load("pool", v[:, hf * 8:(hf + 1) * 8, :], uspec[u][1][hf], key)
                issued[0] += 1

        def prep(e_):
            par = e_ % 2
            for b in range(2):
                col = 32 * b + e_
                for t in range(NT):
                    P.op("dve", lambda e, b=b, t=t, col=col: e.tensor_scalar(out=S[b][:, t, :], in0=iota256[:],
                                                                           scalar1=posm[:, t, col:col + 1], scalar2=None,
                                                                           op0=ALU.is_equal),
                         reads=[], writes=["S%d" % b])
            pg = bank(5)[:, 256:272]
            for b in range(2):
                col = 32 * b + e_
                for half in range(2):
                    gi = b * 2 + half
                    for t in range(NT):
                        P.op("pe", lambda e, b=b, t=t, half=half, gi=gi, col=col: e.matmul(
                            pg[:, gi * 4:gi * 4 + 4], lhsT=S[b][:, t, half * 128:(half + 1) * 128],
                            rhs=ghl[:, t, col, :], start=(t == 0), stop=(t == NT - 1)),
                             reads=["S%d" % b], writes=["ps:5"])
            P.op("dve", lambda e: e.tensor_copy(out=pgs[:, par, :], in_=pg), reads=["ps:5"], writes=["pgs%d" % par])
            pv4 = pgs[:, par, :].rearrange("p (a b) -> p a b", a=4)
            P.op("dve", lambda e: e.tensor_tensor(out=gsb[:, par, :], in0=pv4[:, :, 0], in1=pv4[:, :, 1], op=ALU.add),
                 reads=["pgs%d" % par], writes=["gs%d" % par, "x1gen"])
            P.op("dve", lambda e: e.scalar_tensor_tensor(out=idxf[:, par, :], in0=pv4[:, :, 2], scalar=256.0,
                                                         in1=pv4[:, :, 3], op0=ALU.mult, op1=ALU.add),
                 reads=["pgs%d" % par], writes=["idxf%d" % par])
            P.op("dve", lambda e: e.tensor_copy(out=idxi[:, par * 4:par * 4 + 4], in_=idxf[:, par, :]),
                 reads=["idxf%d" % par], writes=["idxi%d" % par])
            for gi in range(4):
                P.dma("pool", lambda e, gi=gi: e.indirect_dma_start(
                    out=xetok[par][:, gi, :], out_offset=None, in_=h2s[:, :],
                    in_offset=bass.IndirectOffsetOnAxis(ap=idxi[:, par * 4 + gi:par * 4 + gi + 1], axis=0)),
                      d_g[par][gi], reads=["idxi%d" % par], writes=["xetok%d_%d" % (par, gi)])

        import os
        ne_dbg = int(os.environ.get("K_NE", NE))
        prep(0)
        for e_ in range(ne_dbg):
            par = e_ % 2
            gs = gsb[:, par, :]
            for gi in range(4):
                pT = bankb(6 + gi % 2)
                for k in range(8):
                    P.op("pe", lambda e, gi=gi, k=k, pT=pT, par=par: e.transpose(out=pT[:, k * 128:(k + 1) * 128],
                                                                        in_=xetok[par][:, gi, k * 128:(k + 1) * 128],
                                                                        identity=identb[:]),
                         reads=["xetok%d_%d" % (par, gi)], writes=["ps:%d" % (6 + gi % 2)])
                P.op("act", lambda e, gi=gi, pT=pT: e.copy(out=xeT[:, :, gi * 128:(gi + 1) * 128],
                                                           in_=pT.rearrange("p (a b) -> p a b", a=8)),
                     reads=["ps:%d" % (6 + gi % 2)], writes=["xeT"])
            for g in range(4):
                ua = 12 * e_ + 2 * g
                ensure_issued(ua + 1)
                wa, ka = uview(ua)
                wb, kb = uview(ua + 1)
                for mm in range(4):
                    m = g * 4 + mm
                    ph1 = bank(m % 2)
                    ph3 = bank(2 + m % 2)
                    for k in range(8):
                        P.op("pe", lambda e, k=k, mm=mm, wa=wa, ph1=ph1: e.matmul(
                            ph1, lhsT=wa[:, k, mm * 128:(mm + 1) * 128], rhs=xeT[:, k, :], start=(k == 0), stop=(k == 7)),
                             reads=[ka, "xeT"], writes=["ps:%d" % (m % 2)])
                    for k in range(8):
                        P.op("pe", lambda e, k=k, mm=mm, wb=wb, ph3=ph3: e.matmul(
                            ph3, lhsT=wb[:, k, mm * 128:(mm + 1) * 128], rhs=xeT[:, k, :], start=(k == 0), stop=(k == 7)),
                             reads=[kb, "xeT"], writes=["ps:%d" % (2 + m % 2)])
                    P.op("act", lambda e, m=m, ph1=ph1: e.activation(out=sil[m % 2], in_=ph1, func=AF.Silu),
                         reads=["ps:%d" % (m % 2)], writes=["sil%d" % (m % 2)])
                    P.op("dve", lambda e, m=m, ph3=ph3: e.tensor_tensor(out=hidT[:, m, :], in0=sil[m % 2], in1=ph3,
                                                                        op=ALU.mult),
                         reads=["sil%d" % (m % 2), "ps:%d" % (2 + m % 2)], writes=["hidT"])
                ensure_issued(min(ua + 1 + NRING, 12 * e_ + 11))
            ensure_issued(12 * e_ + 11)
            if e_ + 1 < ne_dbg:
                prep(e_ + 1)
            for dq in range(4):
                uw = 12 * e_ + 8 + dq
                wv, kw = uview(uw)
                for sc in range(4):
                    pye = bank(4 + sc % 2)[:, 0:256]
                    for m in range(16):
                        P.op("pe", lambda e, m=m, sc=sc, wv=wv, pye=pye: e.matmul(
                            pye, lhsT=hidT[:, m, sc * 128:(sc + 1) * 128], rhs=wv[:, m, :], start=(m == 0), stop=(m == 15)),
                             reads=[kw, "hidT"], writes=["ps:%d" % (4 + sc % 2)])
                    P.op("dve", lambda e, sc=sc, dq=dq, pye=pye, gs=gs: e.scalar_tensor_tensor(
                        out=ye_sb[:, sc, dq * 256:(dq + 1) * 256], in0=pye, scalar=gs[:, sc:sc + 1],
                        in1=gt2bc[:, sc // 2, dq * 256:(dq + 1) * 256], op0=ALU.mult, op1=ALU.mult),
                         reads=["ps:%d" % (4 + sc % 2), "gs%d" % par, "gt2bc"], writes=["ye_sb"])
                ensure_issued(uw + NRING)
            for gi in range(4):
                P.dma("pool", lambda e, gi=gi, par=par: e.indirect_dma_start(
                    out=x1flat[:, :], out_offset=bass.IndirectOffsetOnAxis(ap=idxi[:, par * 4 + gi:par * 4 + gi + 1], axis=0),
                    in_=ye_sb[:, gi, :], in_offset=None, compute_op=ALU.add),
                      d_sc[gi], reads=["ye_sb", "idxi%d" % par, "x1gen"])

    def epoch_f(b):
        cv = Carver()
        x1t = [cv.get([128, D], F32) for _ in range(3)]
        outt = [cv.get([128, D], F32) for _ in range(2)]
        sqj = cv.get([128, D], BF16)
        for t in range(NT):
            xb = x1t[t % 3]
            xk = "x1t%d" % (t % 3)
            ob = outt[t % 2]
            okk = "outt%d" % (t % 2)
            c0 = 2 * (t % 2)
            load("sync", xb, x1s[b, t * 128:(t + 1) * 128, :], xk)
            P.op("act", lambda e, xb=xb, c0=c0: e.activation(out=sqj, in_=xb, func=AF.Square, accum_out=st[:, c0:c0 + 1]),
                 reads=[xk], writes=["sqj", "st%d" % c0])
            rstd_chain(c0, 1.0 / D)
            P.op("dve", lambda e, xb=xb, ob=ob, c0=c0: e.scalar_tensor_tensor(out=ob, in0=xb, scalar=st[:, c0 + 1:c0 + 2],
                                                                             in1=gfinbc[:], op0=ALU.mult, op1=ALU.mult),
                 reads=[xk, "st%d" % (c0 + 1)], writes=[okk])
            P.dma("sync", lambda e, ob=ob, t=t: e.dma_start(out=y[b, t * 128:(t + 1) * 128, :], in_=ob),
                  d_outs[t % 2], reads=[okk])

    if stage >= 3:
        epoch_d()
        P.barrier_all()
        if stage == 25:
            P.final_wait("sync", d_outs)
            P.emit()
            return nc
        epoch_e()
        P.barrier_all()
        if stage == 26:
            P.final_wait("sync", d_outs)
            P.emit()
            return nc
        for b in range(2):
            epoch_f(b)
            P.barrier_all()

    P.final_wait("sync", d_outs)
    P.emit()
    return nc


def build_moe(nc, P, env):
    raise NotImplementedError


_STAGE = 3


def kernel(x, c, ctx, c_ctx, w_mod, b_mod, g_mix, g_ffn, w_in, q_gain, k_gain, v_gain,
           w_s, b_s, w_out, w_router, w1, w3, w2, g_final):
    f = lambda a: np.ascontiguousarray(np.asarray(a, dtype=np.float32))
    x, c, ctx, c_ctx = f(x), f(c), f(ctx), f(c_ctx)
    consts = _consts()
    rows = np.concatenate([f(g_ffn)[0], f(g_final), np.tile(f(q_gain)[0], 8), np.tile(f(k_gain)[0], 2),
                           f(v_gain)[0]])
    assert rows.shape[0] == 3200
    shared = {
        "w_mod": f(w_mod)[0], "bmod3": np.tile(f(b_mod)[0][None, :], (3, 1)),
        "rows3": np.tile(rows[None, :], (3, 1)),
        "g2": np.concatenate([f(g_mix)[0].reshape(8, 128), f(g_ffn)[0].reshape(8, 128)], axis=0),
        "w_in": f(w_in)[0], "w_s": f(w_s)[0], "b_s": f(b_s)[0], "w_out": f(w_out)[0],
        "w_router": f(w_router)[0], "w1": f(w1)[0], "w3": f(w3)[0], "w2": f(w2)[0],
    }
    shared.update(consts)
    in_maps = []
    for i in range(NCORES):
        m = dict(shared)
        m["x"] = x[2 * i:2 * i + 2]
        m["ctx"] = ctx[2 * i:2 * i + 2]
        m["c3"] = np.concatenate([c[2 * i:2 * i + 2], c_ctx[None, :]], axis=0)
        in_maps.append(m)
    nc = build_nc(_STAGE)
    res = run_bass_kernel_spmd(nc, in_maps, core_ids=list(range(NCORES)))
    return np.concatenate([r["y"] for r in res.results], axis=0)
```

```python
import numpy as np
import concourse.bass as bass
import concourse.mybir as mybir
from concourse.bass_utils import run_bass_kernel_spmd
from contextlib import ExitStack

F32 = mybir.dt.float32
BF16 = mybir.dt.bfloat16
U8 = mybir.dt.uint8
ALU = mybir.AluOpType
AF = mybir.ActivationFunctionType
AX = mybir.AxisListType

COMPUTE = ("pe", "act", "dve", "pool")
POOL_TO_DVE = True
CARVE_LOG = []
EPS = 1e-6
NCORES = 8
T = 2048
NT = 16
D = 1024
NE = 16
CAP = 256


class DSem:
    def __init__(self, name):
        self.name = name
        self.sem = None
        self.count = 0


class Ins:
    __slots__ = ("eng", "fn", "waits", "flag", "idx", "dsem")

    def __init__(self, eng, fn):
        self.eng = eng
        self.fn = fn
        self.waits = []
        self.flag = False
        self.idx = None
        self.dsem = None


class Prog:
    def __init__(self, nc):
        self.nc = nc
        self.q = {e: [] for e in COMPUTE + ("sync",)}
        self.last_w = {}
        self.readers = {}
        self.waited_c = {e: {b: -1 for b in COMPUTE} for e in self.q}
        self.waited_d = {e: {} for e in self.q}
        self.dsems = []

    def dsem(self, name):
        d = DSem(name)
        self.dsems.append(d)
        return d

    def _add_wait(self, ins, dep):
        q = ins.eng
        if dep[0] == "c":
            _, b, idx = dep
            if b == "pe" and q == "pe":
                return
            while idx >= 0 and (self.q[b][idx].fn is None or self.q[b][idx].dsem is not None):
                idx -= 1
            if idx < 0 or self.waited_c[q][b] >= idx:
                return
            self.waited_c[q][b] = idx
            self.q[b][idx].flag = True
            ins.waits.append(("c", b, idx))
        else:
            _, d, cnt = dep
            if self.waited_d[q].get(id(d), 0) >= cnt:
                return
            self.waited_d[q][id(d)] = cnt
            ins.waits.append(dep)

    def _deps(self, ins, reads, writes, token):
        ex = [r for r in reads if r.startswith("ps:")]
        if ex:
            reads = [r for r in reads if not r.startswith("ps:")]
            writes = list(writes) + ex
        for r in reads:
            w = self.last_w.get(r)
            if w is not None:
                self._add_wait(ins, w)
        for w_ in writes:
            w = self.last_w.get(w_)
            if w is not None:
                self._add_wait(ins, w)
            for rd in self.readers.get(w_, {}).values():
                self._add_wait(ins, rd)
        key = token[1] if token[0] == "c" else id(token[1])
        for r in reads:
            self.readers.setdefault(r, {})[key] = token
        for w_ in writes:
            self.last_w[w_] = token
            self.readers[w_] = {}

    def op(self, eng, fn, reads=(), writes=()):
        if eng == "pool!":
            eng = "pool"
        elif eng == "pool" and POOL_TO_DVE:
            eng = "dve"
        ins = Ins(eng, fn)
        ins.idx = len(self.q[eng])
        self._deps(ins, reads, writes, ("c", eng, ins.idx))
        self.q[eng].append(ins)
        return ins

    def dma(self, queue, fn, dsem, reads=(), writes=()):
        ins = Ins(queue, fn)
        ins.idx = len(self.q[queue])
        dsem.count += 16
        ins.dsem = dsem
        self._deps(ins, reads, writes, ("d", dsem, dsem.count))
        self.q[queue].append(ins)
        return ins

    def barrier_all(self):
        for q in self.q:
            ins = Ins(q, None)
            ins.idx = len(self.q[q])
            for b in COMPUTE:
                j = len(self.q[b]) - 1
                if j >= 0 and b != q:
                    self._add_wait(ins, ("c", b, j))
            for d in self.dsems:
                if d.count > 0:
                    self._add_wait(ins, ("d", d, d.count))
            self.q[q].append(ins)
        self.last_w = {}
        self.readers = {}

    def final_wait(self, queue, dsems):
        ins = Ins(queue, None)
        ins.idx = len(self.q[queue])
        for d in dsems:
            if d.count > 0:
                self._add_wait(ins, ("d", d, d.count))
        self.q[queue].append(ins)

    def emit(self):
        nc = self.nc
        with ExitStack() as st:
            csem = {e: st.enter_context(nc.semaphore("cs_" + e)) for e in COMPUTE}
            for d in self.dsems:
                if d.count > 0:
                    d.sem = st.enter_context(nc.semaphore("ds_" + d.name))
            mile = {}
            for e in COMPUTE:
                c = 0
                arr = []
                for ins in self.q[e]:
                    if ins.flag:
                        c += 1
                    arr.append(c)
                mile[e] = arr
            block = st.enter_context(nc.Block())

            def run(qname, eng):
                for ins in self.q[qname]:
                    for dep in ins.waits:
                        if dep[0] == "c":
                            eng.wait_ge(csem[dep[1]], mile[dep[1]][dep[2]])
                        else:
                            eng.wait_ge(dep[1].sem, dep[2])
                    if ins.fn is None:
                        continue
                    r = ins.fn(eng)
                    if ins.dsem is not None:
                        r.then_inc(ins.dsem.sem, 16)
                    elif ins.flag:
                        r.then_inc(csem[qname], 1)

            @block.sync
            def _(sync):
                run("sync", sync)

            @block.scalar
            def _(scalar):
                run("act", scalar)

            @block.vector
            def _(vector):
                run("dve", vector)

            @block.gpsimd
            def _(gpsimd):
                run("pool", gpsimd)

            @block.tensor
            def _(tensor):
                run("pe", tensor)


def _consts():
    c = {}
    c["identf"] = np.eye(128, dtype=np.float32)
    k = np.arange(128)
    c["tri"] = (k[:, None] < k[None, :]).astype(np.float32)
    c["iota256"] = np.tile(np.arange(256, dtype=np.float32)[None, :], (128, 1))
    c["iotap"] = np.stack([k, k + 128], axis=1).astype(np.float32)
    sel48 = np.zeros((48, 32, 128), np.float32)
    for b in range(2):
        for e in range(16):
            sel48[32 * b + e, 16 * b + e, :] = 1.0
    c["sel48"] = sel48.reshape(48, 32 * 128)
    sel3 = np.zeros((3, 3, 128), np.float32)
    for j in range(3):
        sel3[j, j, :] = 1.0
    c["sel3"] = sel3.reshape(3, 384)
    nf = 16
    inv = (10000.0 ** (-np.arange(nf, dtype=np.float32) / nf)).astype(np.float32)
    tok = np.arange(T)
    row = (tok // 64).astype(np.float32)
    col = (tok % 64).astype(np.float32)
    ang = np.concatenate([row[:, None] * inv[None, :], col[:, None] * inv[None, :]], axis=-1).astype(np.float32)
    c["cs"] = np.concatenate([np.cos(ang), np.sin(ang)], axis=-1).astype(np.float32)
    tokc = np.zeros((128, NT, 48, 2), np.float32)
    for t in range(NT):
        for col in range(48):
            bb = col // 32
            tokc[:, t, col, 0] = bb * 8 + t // 2
            tokc[:, t, col, 1] = (t % 2) * 128 + np.arange(128)
    c["tokc"] = tokc.reshape(128, NT * 48 * 2)
    return c


def build_nc(stage=9):
    nc = bass.Bass("TRN2", target_bir_lowering=False)
    P = Prog(nc)

    def din(name, shape):
        return nc.dram_tensor(name, list(shape), F32, kind="ExternalInput").ap()

    x = din("x", [2, T, D])
    ctx = din("ctx", [2, 256, D])
    c3 = din("c3", [3, D])
    w_mod = din("w_mod", [D, 6 * D])
    bmod3 = din("bmod3", [3, 6 * D])
    rows3 = din("rows3", [3, 3200])
    g2 = din("g2", [16, 128])
    w_in = din("w_in", [D, 1792])
    w_s = din("w_s", [8, 128, 128])
    b_s = din("b_s", [8, 128])
    w_out = din("w_out", [D, D])
    w_router = din("w_router", [D, NE])
    w1 = din("w1", [NE, D, 2048])
    w3 = din("w3", [NE, D, 2048])
    w2 = din("w2", [NE, 2048, D])
    k_identf = din("identf", [128, 128])
    k_tri = din("tri", [128, 128])
    k_iota256 = din("iota256", [128, 256])
    k_iotap = din("iotap", [128, 2])
    k_sel48 = din("sel48", [48, 4096])
    k_sel3 = din("sel3", [3, 384])
    k_cs = din("cs", [T, 64])
    y = nc.dram_tensor("y", [2, T, D], F32, kind="ExternalOutput").ap()
    x1s = nc.dram_tensor("x1s", [2, T, D], F32, kind="Internal").ap()
    yes = nc.dram_tensor("yes", [2, NE, 2, 128, D], BF16, kind="Internal").ap()
    h2s = nc.dram_tensor("h2s", [2 * T, D], BF16, kind="Internal").ap()
    k_tokc = din("tokc", [128, NT * 48 * 2])

    def sb(name, shape, dt):
        return nc.alloc_sbuf_tensor(name, list(shape), dt)

    identb = sb("identb", [128, 128], BF16)
    identf = sb("identf_sb", [128, 128], F32)
    trib = sb("trib", [128, 128], BF16)
    onesb = sb("onesb", [128, 128], BF16)
    iota256 = sb("iota256_sb", [128, 256], F32)
    iotap = sb("iotap_sb", [128, 2], F32)
    sel48 = sb("sel48_sb", [48, 32, 128], BF16)
    sel3 = sb("sel3_sb", [3, 3, 128], F32)
    cs = sb("cs_sb", [128, NT, 64], F32)
    gain640 = sb("gain640", [128, 640], F32)
    vgainbc = sb("vgainbc", [128, 512], F32)
    gfinbc = sb("gfinbc", [128, D], F32)
    wsT = sb("wsT", [128, 8, 128], BF16)
    bsT = sb("bsT", [128, 8], F32)
    wr = sb("wr", [128, 8, NE], BF16)
    scT = sb("scT", [128, 8, 3], BF16)
    mT = sb("mT", [128, 48, 3], F32)
    gT = sb("gT", [128, 16], F32)
    s1T = sb("s1T", [128, 8, 3], F32)
    msb = sb("msb", [3, 4, D], F32)
    posm = sb("posm", [128, NT, 48], F32)
    ghl = sb("ghl", [128, NT, 48, 4], BF16)
    idxi = sb("idxi", [128, 8], mybir.dt.int32)
    posmT = sb("posmT", [48, T], BF16)
    st = sb("st", [128, 64], F32)
    epsb = sb("epsb", [128, 1], F32)

    RBYTES = 150 * 1024
    R = sb("R", [128, RBYTES], U8)

    class Carver:
        def __init__(self):
            self.off = 0
            CARVE_LOG.append([])

        def get(self, shape, dt, parts=128):
            es = 2 if dt == BF16 else 4
            n = int(np.prod(shape[1:]))
            nb = n * es
            off = (self.off + 63) // 64 * 64
            assert off + nb <= RBYTES, (off, nb)
            v = R[0:parts, off:off + nb].bitcast(dt)
            CARVE_LOG[-1].append((off, tuple(shape), "bf16" if dt == BF16 else "f32"))
            self.off = off + nb
            if len(shape) == 3:
                v = v.rearrange("p (a b) -> p a b", a=shape[1])
            elif len(shape) == 4:
                v = v.rearrange("p (a b c) -> p a b c", a=shape[1], b=shape[2])
            return v

    pp = [nc.alloc_psum_tensor("pp%d" % i, [128, 1024], F32) for i in range(4)]

    def bank(i):
        return pp[i // 2][:, (i % 2) * 512:(i % 2) * 512 + 512]

    def BP(j):
        return ["ps:%d" % (2 * j), "ps:%d" % (2 * j + 1)]

    def bankb(i):
        return bank(i).bitcast(BF16)

    dcache = {}

    def dget(key):
        if key not in dcache:
            dcache[key] = P.dsem("d%d" % len(dcache))
        return dcache[key]

    def load(queue, dst, src, key, reads=()):
        P.dma(queue, lambda e: e.dma_start(out=dst, in_=src), dget(key), reads=reads, writes=[key])

    d_outs = [P.dsem("out0"), P.dsem("out1")]

    cv = Carver()
    c3_sb = cv.get([3, D], F32, parts=3)
    sc3 = cv.get([3, D], F32, parts=3)
    rows3_sb = cv.get([3, 3200], F32, parts=3)
    bmod_sb = cv.get([3, 6 * D], F32, parts=3)
    wm = [cv.get([128, 8, 512], BF16) for _ in range(3)]
    wstmp = cv.get([128, 8, 128], F32)
    g2_sb = cv.get([16, 128], F32, parts=16)
    bs_sb = cv.get([8, 128], F32, parts=8)
    mtmp = [cv.get([3, 512], F32, parts=3) for _ in range(2)]
    trif = cv.get([128, 128], F32)
    sel48f = cv.get([48, 4096], F32, parts=48)

    load("sync", identf[:], k_identf, "identf")
    load("pool", identb[:], k_identf, "identb")
    load("pool", trib[:], k_tri, "trib")
    load("sync", iota256[:], k_iota256, "iota256")
    load("sync", iotap[:], k_iotap, "iotap")
    load("pool", sel48[:].rearrange("p a b -> p (a b)"), k_sel48, "sel48")
    load("sync", sel3[:].rearrange("p a b -> p (a b)"), k_sel3, "sel3")
    load("sync", cs[:], k_cs.rearrange("(i p) c -> p i c", p=128), "cs")
    load("sync", c3_sb, c3, "c3")
    load("sync", rows3_sb, rows3, "rows3")
    load("sync", bmod_sb, bmod3, "bmod")
    load("sync", g2_sb, g2, "g2")
    load("sync", bs_sb, b_s, "bs")
    load("sync", wstmp, w_s.rearrange("g i j -> i g j"), "wstmp")
    load("pool", wr[:], w_router.rearrange("(k p) e -> p k e", p=128), "wr")
    P.op("pool!", lambda e: e.memset(onesb[:], 1.0), writes=["onesb"])
    P.op("pool!", lambda e: e.memset(epsb[:], EPS), writes=["epsb"])

    P.op("act", lambda e: e.activation(out=sc3, in_=c3_sb, func=AF.Silu), reads=["c3"], writes=["sc3"])
    for k in range(8):
        P.op("pe", lambda e, k=k: e.transpose(out=bank(0)[:, k * 3:k * 3 + 3], in_=sc3[:, k * 128:(k + 1) * 128],
                                              identity=identf[0:3, 0:3]), reads=["sc3", "identf"], writes=["ps:0"])
    P.op("dve", lambda e: e.tensor_copy(out=scT[:].rearrange("p a b -> p (a b)"), in_=bank(0)[:, 0:24]),
         reads=["ps:0"], writes=["scT"])

    psT = bank(3)
    for n in range(12):
        seg, hh = n // 2, n % 2
        wmb = wm[n % 3]
        load("pool", wmb, w_mod[:, n * 512:(n + 1) * 512].rearrange("(k p) n -> p k n", p=128), "wm%d" % (n % 3))
        pm = bank(1 + n % 2)
        pk = "ps:%d" % (1 + n % 2)
        for k in range(8):
            P.op("pe", lambda e, k=k, wmb=wmb, pm=pm: e.matmul(pm[0:3, :], lhsT=scT[:, k, :], rhs=wmb[:, k, :],
                                                               start=(k == 0), stop=(k == 7)),
                 reads=["scT", "wm%d" % (n % 3)], writes=[pk])
        mt = mtmp[n % 2]
        mk = "mtmp%d" % (n % 2)
        P.op("dve", lambda e, pm=pm, mt=mt, n=n: e.tensor_tensor(out=mt, in0=pm[0:3, :],
                                                                in1=bmod_sb[:, n * 512:(n + 1) * 512], op=ALU.add),
             reads=[pk, "bmod"], writes=[mk])
        for j in range(4):
            cidx = (n * 4 + j) * 3
            P.op("pe", lambda e, mt=mt, j=j, cidx=cidx: e.transpose(out=psT[:, cidx:cidx + 3],
                                                                   in_=mt[:, j * 128:(j + 1) * 128],
                                                                   identity=identf[0:3, 0:3]),
                 reads=[mk, "identf"], writes=["ps:3"])
        if seg == 2:
            P.op("act", lambda e, mt=mt, hh=hh: e.copy(out=msb[:, 0, hh * 512:(hh + 1) * 512], in_=mt),
                 reads=[mk], writes=["msb"])
        elif seg == 3:
            P.op("act", lambda e, mt=mt, hh=hh: e.copy(out=msb[:, 2, hh * 512:(hh + 1) * 512], in_=mt),
                 reads=[mk], writes=["msb"])
        elif seg == 5:
            P.op("act", lambda e, mt=mt, hh=hh: e.copy(out=msb[:, 3, hh * 512:(hh + 1) * 512], in_=mt),
                 reads=[mk], writes=["msb"])
        elif seg == 4:
            P.op("dve", lambda e, mt=mt, hh=hh: e.scalar_tensor_tensor(
                out=msb[:, 1, hh * 512:(hh + 1) * 512], in0=mt, scalar=1.0,
                in1=rows3_sb[:, hh * 512:(hh + 1) * 512], op0=ALU.add, op1=ALU.mult),
                 reads=[mk, "rows3"], writes=["msb"])
    P.op("dve", lambda e: e.tensor_copy(out=mT[:].rearrange("p a b -> p (a b)"), in_=psT[:, 0:144]),
         reads=["ps:3"], writes=["mT"])
    P.op("pe", lambda e: e.transpose(out=bank(0)[:, 32:48], in_=g2_sb, identity=identf[0:16, 0:16]),
         reads=["g2", "identf"], writes=["ps:0"])
    P.op("dve", lambda e: e.tensor_copy(out=gT[:], in_=bank(0)[:, 32:48]), reads=["ps:0"], writes=["gT"])
    P.op("dve", lambda e: e.scalar_tensor_tensor(out=s1T[:], in0=mT[:, 8:16, :], scalar=1.0,
                                                 in1=gT[:, 0:8].unsqueeze(2).to_broadcast([128, 8, 3]),
                                                 op0=ALU.add, op1=ALU.mult),
         reads=["mT", "gT"], writes=["s1T"])
    for (dst, c0, n_) in ((gfinbc[:, 0:512], 1024, 512), (gfinbc[:, 512:1024], 1536, 512),
                          (gain640[:, 0:512], 2048, 512), (gain640[:, 512:640], 2560, 128),
                          (vgainbc[:, 0:512], 2688, 512)):
        P.op("pe", lambda e, c0=c0, n_=n_: e.matmul(bank(4)[:, 0:n_], lhsT=sel3[:, 0, :], rhs=rows3_sb[:, c0:c0 + n_],
                                                    start=True, stop=True), reads=["sel3", "rows3"], writes=["ps:4"])
        P.op("act", lambda e, dst=dst, n_=n_: e.copy(out=dst, in_=bank(4)[:, 0:n_]), reads=["ps:4"], writes=["bcst"])
    for g in range(8):
        P.op("pe", lambda e, g=g: e.transpose(out=bank(5 + g // 4)[:, (g % 4) * 128:(g % 4) * 128 + 128],
                                              in_=wstmp[:, g, :], identity=identf[:]),
             reads=["wstmp", "identf"], writes=["ps:%d" % (5 + g // 4)])
    for h_ in range(2):
        P.op("act", lambda e, h_=h_: e.copy(out=wsT[:, h_ * 4:(h_ + 1) * 4, :].rearrange("p a b -> p (a b)"),
                                            in_=bank(5 + h_)), reads=["ps:%d" % (5 + h_)], writes=["wsT"])
    P.op("pe", lambda e: e.transpose(out=bank(0)[:, 64:72], in_=bs_sb, identity=identf[0:8, 0:8]),
         reads=["bs", "identf", "gT"], writes=["ps:0"])
    P.op("dve", lambda e: e.tensor_copy(out=bsT[:], in_=bank(0)[:, 64:72]), reads=["ps:0"], writes=["bsT"])
    P.barrier_all()
    if stage == 0:
        P.dma("sync", lambda e: e.dma_start(out=y[0, 0:128, :], in_=gfinbc[:]), d_outs[0], reads=["bcst"])
        P.dma("sync", lambda e: e.dma_start(out=y[0, 128:256, 0:640], in_=gain640[:]), d_outs[0], reads=["bcst"])
        P.dma("sync", lambda e: e.dma_start(out=y[0, 256:384, 0:144], in_=mT[:].rearrange("p a b -> p (a b)")), d_outs[0])
        P.dma("sync", lambda e: e.dma_start(out=y[0, 384:387, :], in_=msb[:, 1, :]), d_outs[0])
        P.dma("sync", lambda e: e.dma_start(out=y[0, 512:640, 0:24], in_=s1T[:].rearrange("p a b -> p (a b)")), d_outs[0])
        P.final_wait("sync", d_outs)
        P.emit()
        return nc

    def rstd_chain(col, scale):
        P.op("act", lambda e: e.activation(out=st[:, col + 1:col + 2], in_=st[:, col:col + 1], func=AF.Sqrt,
                                           scale=scale, bias=epsb[:, 0:1]),
             reads=["st%d" % col, "epsb"], writes=["st%d" % (col + 1)])
        P.op("dve", lambda e: e.reciprocal(out=st[:, col + 1:col + 2], in_=st[:, col + 1:col + 2]),
             reads=["st%d" % (col + 1)], writes=["st%d" % (col + 1)])

    def epoch_ac(b):
        cv = Carver()
        wio = cv.get([128, 8, 1792], BF16)
        qa = cv.get([128, 4, T], BF16)
        gmT = cv.get([128, 4, T], BF16)
        kT = cv.get([128, 2, 2304], BF16)
        vaug = cv.get([128, 18, 2, 192], BF16)
        xt = [cv.get([128, D], F32) for _ in range(2)]
        sqj = cv.get([128, D], BF16)
        xn = cv.get([128, D], BF16)
        hTt = cv.get([128, 8, 128], BF16)
        qk = cv.get([128, 10, 64], F32)
        qk2 = cv.get([128, 10, 64], F32)
        qkr = cv.get([128, 10, 64], BF16)
        kd = cv.get([128, 4, 64], BF16)
        rp = cv.get([128, 4, 320], F32)
        u_sb = cv.get([128, 512], F32)
        vv_sb = cv.get([128, 512], F32)
        vvn = cv.get([128, 512], BF16)
        gtmp = cv.get([128, 512], F32)
        gmb = cv.get([128, 512], BF16)
        PT = [cv.get([128, 1024], BF16) for _ in range(4)]
        rd = cv.get([128, 512], F32)
        gt1bc = cv.get([128, D], F32)
        tmpo = cv.get([128, D], F32)
        s10 = st[:, 16:26]
        r10 = st[:, 32:42]

        for kk in range(4):
            load("pool", wio[:, 2 * kk:2 * kk + 2, :],
                 w_in[kk * 256:(kk + 1) * 256, :].rearrange("(k p) n -> p k n", p=128), "wio")
        for hh in range(2):
            P.op("pe", lambda e, hh=hh: e.matmul(bank(0), lhsT=sel3[:, b, :], rhs=msb[:, 0, hh * 512:(hh + 1) * 512],
                                                 start=True, stop=True), reads=["sel3", "msb"], writes=["ps:0"])
            P.op("act", lambda e, hh=hh: e.copy(out=gt1bc[:, hh * 512:(hh + 1) * 512], in_=bank(0)),
                 reads=["ps:0"], writes=["gt1bc"])
        P.op("pool!", lambda e: e.memset(vaug.rearrange("p a b c -> p (a b c)"), 1.0), writes=["vaug"])

        g3 = gain640[:].rearrange("p (a b) -> p a b", a=10)
        qkf = qk.rearrange("p a b -> p (a b)")
        qkrf = qkr.rearrange("p a b -> p (a b)")
        kdf = kd.rearrange("p a b -> p (a b)")
        kdv = kd.rearrange("p (a b) c -> p a b c", a=2)
        rpv = [rp[:, i, :].rearrange("p (a b) -> p a b", a=10) for i in range(4)]
        pA = bankb(0)
        pB = bankb(5)
        pC = bankb(7)

        def X1(t):
            lat = t < 16
            xb = xt[t % 2]
            xk = "xt%d" % (t % 2)
            src = x[b, t * 128:(t + 1) * 128, :] if lat else ctx[b, (t - 16) * 128:(t - 15) * 128, :]
            load("sync", xb, src, xk)
            P.op("act", lambda e: e.activation(out=sqj, in_=xb, func=AF.Square, accum_out=st[:, 0:1]),
                 reads=[xk], writes=["sqj", "st0"])
            rstd_chain(0, 1.0 / D)
            P.op("act", lambda e: e.activation(out=xn, in_=xb, func=AF.Copy, scale=st[:, 1:2]),
                 reads=[xk, "st1"], writes=["xn"])
            for k in range(8):
                P.op("pe", lambda e, k=k: e.transpose(out=pA[:, k * 128:(k + 1) * 128], in_=xn[:, k * 128:(k + 1) * 128],
                                                      identity=identb[:]), reads=["xn", "identb"], writes=["ps:0"])

        def X2(t):
            lat = t < 16
            j = b if lat else 2
            for k in range(8):
                eng_ = "dve"
                if eng_ == "act":
                    P.op("act", lambda e, k=k: e.activation(out=hTt[:, k, :], in_=pA[:, k * 128:(k + 1) * 128],
                                                            func=AF.Identity, scale=s1T[:, k, j:j + 1],
                                                            bias=mT[:, k, j:j + 1]),
                         reads=["ps:0", "s1T", "mT"], writes=["hTt%d" % k])
                else:
                    P.op("dve", lambda e, k=k: e.tensor_scalar(out=hTt[:, k, :], in0=pA[:, k * 128:(k + 1) * 128],
                                                              scalar1=s1T[:, k, j:j + 1], scalar2=mT[:, k, j:j + 1],
                                                              op0=ALU.mult, op1=ALU.add),
                         reads=["ps:0", "s1T", "mT"], writes=["hTt%d" % k])
            slices = [(1, 0, 512), (2, 512, 256), (3, 768, 512), (4, 1280, 512)] if lat else [(2, 512, 256)]
            for (bi, c0, n_) in slices:
                for k in range(8):
                    P.op("pe", lambda e, bi=bi, c0=c0, n_=n_, k=k: e.matmul(bank(bi)[:, 0:n_], lhsT=hTt[:, k, :],
                                                                           rhs=wio[:, k, c0:c0 + n_],
                                                                           start=(k == 0), stop=(k == 7)),
                         reads=["hTt%d" % k, "wio"], writes=["ps:%d" % bi])

        def Ya(t):
            lat = t < 16
            if lat:
                P.op("dve", lambda e: e.tensor_copy(out=qkf[:, 0:512], in_=bank(1)), reads=["ps:1"], writes=["qk"])
            P.op("dve", lambda e: e.tensor_copy(out=qkf[:, 512:640], in_=bank(2)[:, 0:128]), reads=["ps:2"], writes=["qk"])
            P.op("act", lambda e: e.copy(out=vaug[:, t, :, 64:128],
                                         in_=bank(2)[:, 128:256].rearrange("p (a b) -> p a b", a=2)),
                 reads=["ps:2"], writes=["vaug"])
            if lat:
                P.op("act", lambda e: e.activation(out=u_sb, in_=bank(3), func=AF.Gelu), reads=["ps:3"], writes=["u_sb"])
                P.op("act", lambda e: e.activation(out=vv_sb, in_=bank(4), func=AF.Gelu), reads=["ps:4"], writes=["vv_sb"])

        def Yb(t):
            lat = t < 16
            h0 = 0 if lat else 8
            nh = 10 - h0
            if lat:
                P.op("act", lambda e: e.activation(out=gtmp, in_=vv_sb, func=AF.Square, accum_out=st[:, 4:5]),
                     reads=["vv_sb"], writes=["gtmp", "st4"])
                P.op("act", lambda e: e.activation(out=st[:, 5:6], in_=st[:, 4:5], func=AF.Sqrt, scale=1.0 / 512,
                                                   bias=epsb[:, 0:1]), reads=["st4", "epsb"], writes=["st5"])
            P.op("dve", lambda e: e.tensor_tensor(out=qk2[:, h0:10, :], in0=qk[:, h0:10, :], in1=qk[:, h0:10, :],
                                                  op=ALU.mult), reads=["qk"], writes=["qk2"])
            P.op("dve", lambda e: e.tensor_reduce(out=s10[:, h0:10], in_=qk2[:, h0:10, :], axis=AX.X, op=ALU.add),
                 reads=["qk2"], writes=["s10"])
            P.op("act", lambda e: e.activation(out=r10[:, h0:10], in_=s10[:, h0:10], func=AF.Sqrt,
                                               scale=1.0 / 64, bias=epsb[:, 0:1]),
                 reads=["s10", "epsb"], writes=["r10"])
            if lat:
                P.op("dve", lambda e: e.reciprocal(out=st[:, 5:6], in_=st[:, 5:6]), reads=["st5"], writes=["st5"])
                P.op("dve", lambda e: e.scalar_tensor_tensor(out=vvn, in0=vv_sb, scalar=st[:, 5:6], in1=vgainbc[:],
                                                             op0=ALU.mult, op1=ALU.mult),
                     reads=["vv_sb", "st5", "bcst"], writes=["vvn"])
                for g in range(8):
                    P.op("pe", lambda e, g=g: e.matmul(bank(6)[:, g * 64:(g + 1) * 64], lhsT=wsT[:, g, :],
                                                       rhs=vvn[:, g * 64:(g + 1) * 64], start=True, stop=True),
                         reads=["wsT", "vvn"], writes=["ps:6"])
            P.op("dve", lambda e: e.reciprocal(out=r10[:, h0:10], in_=r10[:, h0:10]), reads=["r10"], writes=["r10"])
            P.op("dve", lambda e: e.tensor_tensor(out=qk2[:, h0:10, :], in0=qk[:, h0:10, :],
                                                  in1=r10[:, h0:10].unsqueeze(2).to_broadcast([128, nh, 64]),
                                                  op=ALU.mult), reads=["qk", "r10"], writes=["qk2"])
            P.op("dve", lambda e: e.tensor_tensor(out=qk2[:, h0:10, :], in0=qk2[:, h0:10, :], in1=g3[:, h0:10, :],
                                                  op=ALU.mult), reads=["qk2", "bcst"], writes=["qk2"])
            if lat:
                cosb = cs[:, t, 0:32].unsqueeze(1).to_broadcast([128, 10, 32])
                sinb = cs[:, t, 32:64].unsqueeze(1).to_broadcast([128, 10, 32])
                x1v = qk2[:, :, 0:32]
                x2v = qk2[:, :, 32:64]
                P.op("dve", lambda e: e.tensor_tensor(out=rpv[0], in0=x1v, in1=cosb, op=ALU.mult),
                     reads=["qk2", "cs"], writes=["rp0"])
                P.op("dve", lambda e: e.tensor_tensor(out=rpv[1], in0=x2v, in1=sinb, op=ALU.mult),
                     reads=["qk2", "cs"], writes=["rp1"])
                P.op("pool", lambda e: e.tensor_tensor(out=rpv[2], in0=x1v, in1=sinb, op=ALU.mult),
                     reads=["qk2", "cs"], writes=["rp2"])
                P.op("pool", lambda e: e.tensor_tensor(out=rpv[3], in0=x2v, in1=cosb, op=ALU.mult),
                     reads=["qk2", "cs"], writes=["rp3"])
                P.op("dve", lambda e: e.tensor_tensor(out=qkr[:, :, 0:32], in0=rpv[0], in1=rpv[1], op=ALU.subtract),
                     reads=["rp0", "rp1"], writes=["qkr"])
                P.op("pool", lambda e: e.tensor_tensor(out=qkr[:, :, 32:64], in0=rpv[2], in1=rpv[3], op=ALU.add),
                     reads=["rp2", "rp3"], writes=["qkr"])
            else:
                P.op("dve", lambda e: e.tensor_copy(out=qkr[:, 8:10, :], in_=qk2[:, 8:10, :]), reads=["qk2"], writes=["qkr"])
            P.op("pool", lambda e: e.tensor_copy(out=kdv, in_=qkr[:, 8:10, :].unsqueeze(2).to_broadcast([128, 2, 2, 64])),
                 reads=["qkr"], writes=["kd"])
            if lat:
                for c in range(4):
                    P.op("pe", lambda e, c=c: e.transpose(out=pB[:, c * 128:(c + 1) * 128], in_=qkrf[:, c * 128:(c + 1) * 128],
                                                          identity=identb[:]), reads=["qkr", "identb"], writes=["ps:5"])
            for kv in range(2):
                P.op("pe", lambda e, kv=kv: e.transpose(out=pB[:, 512 + kv * 128:512 + (kv + 1) * 128],
                                                        in_=kdf[:, kv * 128:(kv + 1) * 128], identity=identb[:]),
                     reads=["kd", "identb"], writes=["ps:5"])
            if lat:
                P.op("act", lambda e: e.copy(out=qa[:, :, t * 128:(t + 1) * 128],
                                             in_=pB[:, 0:512].rearrange("p (a b) -> p a b", a=4)),
                     reads=["ps:5"], writes=["qa"])
            P.op("act", lambda e: e.copy(out=kT[:, :, t * 128:(t + 1) * 128],
                                         in_=pB[:, 512:768].rearrange("p (a b) -> p a b", a=2)),
                 reads=["ps:5"], writes=["kT"])
            if not lat:
                return
            P.op("dve", lambda e: e.tensor_tensor(out=gtmp.rearrange("p (a b) -> p a b", a=8),
                                                  in0=bank(6).rearrange("p (a b) -> p a b", a=8),
                                                  in1=bsT[:].unsqueeze(2).to_broadcast([128, 8, 64]), op=ALU.add),
                 reads=["ps:6", "bsT"], writes=["gtmp"])
            P.op("pool", lambda e: e.tensor_tensor(out=gmb, in0=gtmp, in1=u_sb, op=ALU.mult),
                 reads=["gtmp", "u_sb"], writes=["gmb"])
            for c in range(4):
                P.op("pe", lambda e, c=c: e.transpose(out=pC[:, c * 128:(c + 1) * 128], in_=gmb[:, c * 128:(c + 1) * 128],
                                                      identity=identb[:]), reads=["gmb", "identb"], writes=["ps:7"])
            P.op("act", lambda e: e.copy(out=gmT[:, :, t * 128:(t + 1) * 128],
                                         in_=pC[:, 0:512].rearrange("p (a b) -> p a b", a=4)),
                 reads=["ps:7"], writes=["gmT"])

        NTA = 18
        X1(0)
        X2(0)
        X1(1)
        for t in range(NTA):
            Ya(t)
            if t + 1 < NTA:
                X2(t + 1)
            if t + 2 < NTA:
                X1(t + 2)
            Yb(t)

        P.barrier_all()
        steps = [(c, qg, sc_) for c in range(4) for qg in range(4) for sc_ in range(18)]
        Osb = cv.get([128, 1024], F32)

        def qk_step(i):
            c, qg, sc_ = steps[i]
            kv = c // 2
            sj = (0, 1, 3)[i % 3]
            S = pp[sj]
            for half in range(2):
                r0 = half * 64
                P.op("pe", lambda e, half=half, r0=r0: e.matmul(
                    S[:, half * 512:(half + 1) * 512], lhsT=kT[r0:r0 + 64, kv, sc_ * 128:(sc_ + 1) * 128],
                    rhs=qa[r0:r0 + 64, c, qg * 512:(qg + 1) * 512], start=True, stop=True),
                     reads=["kT", "qa%d_%d_%d" % (c, half, qg), "qa"], writes=BP(sj))
            P.op("act", lambda e: e.activation(out=PT[i % 4], in_=S[:], func=AF.Exp, scale=0.125),
                 reads=BP(sj), writes=["PT%d" % (i % 4)])

        def pv_step(i):
            c, qg, sc_ = steps[i]
            kv = c // 2
            for half in range(2):
                off = 64 if half == 0 else 0
                P.op("pe", lambda e, half=half, off=off: e.matmul(
                    bank(4 + half), lhsT=vaug[:, sc_, kv, off:off + 128],
                    rhs=PT[i % 4][:, half * 512:(half + 1) * 512], start=(sc_ == 0), stop=(sc_ == 17)),
                     reads=["vaug", "PT%d" % (i % 4)], writes=["ps:%d" % (4 + half)])
            if sc_ == 17:
                P.op("dve", lambda e: e.tensor_copy(out=Osb, in_=pp[2][:]), reads=BP(2), writes=["Osb"])
                for half in range(2):
                    nr = half * 64
                    dr = 64 - nr
                    P.op("dve", lambda e, half=half, nr=nr, dr=dr: e.reciprocal(
                        out=rd[nr:nr + 64, :], in_=Osb[dr:dr + 64, half * 512:(half + 1) * 512]),
                         reads=["Osb"], writes=["rd%d" % half])
                    P.op("dve", lambda e, half=half, nr=nr: e.tensor_tensor(
                        out=qa[nr:nr + 64, c, qg * 512:(qg + 1) * 512], in0=Osb[nr:nr + 64, half * 512:(half + 1) * 512],
                        in1=rd[nr:nr + 64, :], op=ALU.mult),
                         reads=["Osb", "rd%d" % half], writes=["qa%d_%d_%d" % (c, half, qg)])

        if stage >= 2:
            n = len(steps)
            qk_step(0)
            qk_step(1)
            for i in range(n):
                if i + 2 < n:
                    qk_step(i + 2)
                pv_step(i)

        for kk in range(4):
            load("pool", wio[:, 2 * kk:2 * kk + 2, 0:1024],
                 w_out[kk * 256:(kk + 1) * 256, :].rearrange("(k p) n -> p k n", p=128), "wio")
        tmpo2 = [tmpo, Osb]
        for t in range(16):
            xb = xt[t % 2]
            xk = "xt%d" % (t % 2)
            load("sync", xb, x[b, t * 128:(t + 1) * 128, :], xk)
            pj = 2 + t % 2
            pO = pp[pj]
            tb = tmpo2[t % 2]
            tk = ("tmpo", "Osb")[t % 2]
            for hh in range(2):
                for k in range(8):
                    src = qa[:, k, t * 128:(t + 1) * 128] if k < 4 else gmT[:, k - 4, t * 128:(t + 1) * 128]
                    rk = ["qa%d_%d_%d" % (k, hf, t // 4) for hf in range(2)] + ["qa"] if k < 4 else ["gmT"]
                    P.op("pe", lambda e, hh=hh, k=k, src=src, pO=pO: e.matmul(pO[:, hh * 512:(hh + 1) * 512], lhsT=src,
                                                                             rhs=wio[:, k, hh * 512:(hh + 1) * 512],
                                                                             start=(k == 0), stop=(k == 7)),
                         reads=rk + ["wio"], writes=BP(pj))
            P.op("dve", lambda e, pO=pO, tb=tb: e.tensor_tensor(out=tb, in0=pO[:], in1=gt1bc, op=ALU.mult),
                 reads=BP(pj) + ["gt1bc"], writes=[tk])
            P.op("pool", lambda e, xb=xb, tb=tb: e.tensor_tensor(out=tb, in0=xb, in1=tb, op=ALU.add),
                 reads=[tk, xk], writes=[tk])
            dst = x1s[b, t * 128:(t + 1) * 128, :] if stage >= 3 else y[b, t * 128:(t + 1) * 128, :]
            P.dma("sync", lambda e, tb=tb, dst=dst: e.dma_start(out=dst, in_=tb), d_outs[t % 2],
                  reads=[tk], writes=["x1s%d_%d" % (b, t)])

    for b in range(2):
        epoch_ac(b)
        P.barrier_all()


    def epoch_d():
        cv = Carver()
        h2 = cv.get([128, 2 * NT, D], BF16)
        x1t = [cv.get([128, D], F32) for _ in range(2)]
        sqj = cv.get([128, D], BF16)
        tmp = cv.get([128, D], F32)
        s2bc = cv.get([128, D], F32)
        sh2bc = cv.get([128, D], F32)
        h2Tt = cv.get([128, D], BF16)
        ex = cv.get([128, 16], F32)
        aff2 = cv.get([128, NT, 48], F32)
        affT = cv.get([48, T], F32, parts=48)
        work = cv.get([48, T], F32, parts=48)
        mx8 = cv.get([48, 8], F32, parts=48)
        gate2 = cv.get([128, NT, 48], F32)
        mask2 = cv.get([128, NT, 48], BF16)
        mask2f = cv.get([128, NT, 48], F32)
        totsb = cv.get([128, NT, 48], F32)
        base = cv.get([128, NT, 48], F32)
        pos = cv.get([128, NT, 48], F32)
        ghf = cv.get([128, NT, 48], F32)
        gh = cv.get([128, NT, 48], BF16)
        fl = lambda v: v.rearrange("p a b -> p (a b)")

        P.op("pool!", lambda e: e.memset(fl(aff2), 0.0), writes=["aff2"])
        d_h2s = [P.dsem("h2s0"), P.dsem("h2s1")]
        tokf = cv.get([128, NT * 48, 2], F32)
        load("sync", tokf.rearrange("p a b -> p (a b)"), k_tokc, "tokf")
        P.op("dve", lambda e: e.tensor_copy(out=ghl[:].rearrange("p a b c -> p (a b) c")[:, :, 2:4], in_=tokf),
             reads=["tokf"], writes=["ghl23"])
        for b in range(2):
            for (dst, ri, key) in ((s2bc, 1, "s2bc"), (sh2bc, 2, "sh2bc")):
                for hh in range(2):
                    P.op("pe", lambda e, ri=ri, hh=hh, b=b: e.matmul(bank(0), lhsT=sel3[:, b, :],
                                                               rhs=msb[:, ri, hh * 512:(hh + 1) * 512],
                                                               start=True, stop=True), reads=[], writes=["ps:0"])
                    P.op("act", lambda e, dst=dst, hh=hh: e.copy(out=dst[:, hh * 512:(hh + 1) * 512], in_=bank(0)),
                         reads=["ps:0"], writes=[key])
            def DX(t, b=b):
                xb = x1t[t % 2]
                xk = "x1t%d" % (t % 2)
                load("sync", xb, x1s[b, t * 128:(t + 1) * 128, :], xk)
                P.op("act", lambda e: e.activation(out=sqj, in_=xb, func=AF.Square, accum_out=st[:, 0:1]),
                     reads=[xk], writes=["sqj", "st0"])
                rstd_chain(0, 1.0 / D)
                P.op("dve", lambda e: e.scalar_tensor_tensor(out=tmp, in0=xb, scalar=st[:, 1:2], in1=s2bc,
                                                             op0=ALU.mult, op1=ALU.mult),
                     reads=[xk, "st1", "s2bc"], writes=["tmp"])
                h2t = h2[:, b * NT + t, :]
                P.op("pool", lambda e: e.tensor_tensor(out=h2t, in0=tmp, in1=sh2bc, op=ALU.add),
                     reads=["tmp", "sh2bc"], writes=["h2t"])
                P.dma("sync", lambda e: e.dma_start(out=h2s[b * T + t * 128:b * T + (t + 1) * 128, :], in_=h2t),
                      d_h2s[t % 2], reads=["h2t"])
                pA = bankb(1)
                for k in range(8):
                    P.op("pe", lambda e, k=k: e.transpose(out=pA[:, k * 128:(k + 1) * 128],
                                                          in_=h2t[:, k * 128:(k + 1) * 128], identity=identb[:]),
                         reads=["h2t"], writes=["ps:1"])
                P.op("act", lambda e: e.copy(out=h2Tt, in_=pA), reads=["ps:1"], writes=["h2Tt"])
                lg = bank(2)[:, t * 16:(t + 1) * 16]
                for k in range(8):
                    P.op("pe", lambda e, k=k: e.matmul(lg, lhsT=h2Tt[:, k * 128:(k + 1) * 128], rhs=wr[:, k, :],
                                                       start=(k == 0), stop=(k == 7)),
                         reads=["h2Tt"], writes=["ps:2"])

            def DY(t, b=b):
                lg = bank(2)[:, t * 16:(t + 1) * 16]
                P.op("dve", lambda e: e.tensor_reduce(out=st[:, 8:9], in_=lg, axis=AX.X, op=ALU.max),
                     reads=["ps:2"], writes=["st8"])
                P.op("dve", lambda e: e.tensor_scalar(out=st[:, 9:10], in0=st[:, 8:9], scalar1=-1.0, scalar2=None,
                                                      op0=ALU.mult), reads=["st8"], writes=["st9"])
                P.op("act", lambda e: e.activation(out=ex, in_=lg, func=AF.Exp, bias=st[:, 9:10],
                                                   accum_out=st[:, 10:11]),
                     reads=["ps:2", "st9"], writes=["ex", "st10"])
                P.op("dve", lambda e: e.reciprocal(out=st[:, 11:12], in_=st[:, 10:11]), reads=["st10"], writes=["st11"])
                P.op("dve", lambda e: e.tensor_scalar(out=aff2[:, t, 32 * b:32 * b + 16], in0=ex,
                                                      scalar1=st[:, 11:12], scalar2=None, op0=ALU.mult),
                     reads=["ex", "st11"], writes=["aff2"])

            DX(0)
            for t in range(NT):
                if t + 1 < NT:
                    DX(t + 1)
                DY(t)
        for t in range(NT):
            P.op("pe", lambda e, t=t: e.transpose(out=pp[2 + t // 8][0:48, (t % 8) * 128:(t % 8) * 128 + 128],
                                                  in_=aff2[:, t, :], identity=identf[:]),
                 reads=["aff2"], writes=BP(2 + t // 8))
        for hh in range(2):
            P.op("act", lambda e, hh=hh: e.copy(out=affT[:, hh * 1024:(hh + 1) * 1024], in_=pp[2 + hh][0:48, :]),
                 reads=BP(2 + hh), writes=["affT"])
            P.op("dve", lambda e, hh=hh: e.tensor_copy(out=work[:, hh * 1024:(hh + 1) * 1024], in_=pp[2 + hh][0:48, :]),
                 reads=BP(2 + hh), writes=["work"])
        for r in range(CAP // 8):
            P.op("dve", lambda e: e.max(out=mx8, in_=work), reads=["work"], writes=["mx8"])
            P.op("dve", lambda e: e.match_replace(out=work, in_to_replace=mx8, in_values=work, imm_value=0.0),
                 reads=["work", "mx8"], writes=["work"])
        P.op("dve", lambda e: e.tensor_tensor(out=work, in0=affT, in1=work, op=ALU.subtract),
             reads=["affT", "work"], writes=["work"])
        for t in range(NT):
            P.op("pe", lambda e, t=t: e.transpose(out=pp[t // 8][:, (t % 8) * 64:(t % 8) * 64 + 48],
                                                  in_=work[:, t * 128:(t + 1) * 128], identity=identf[0:48, 0:48]),
                 reads=["work"], writes=BP(t // 8))
        for hh in range(2):
            P.op("act", lambda e, hh=hh: e.copy(out=gate2[:, hh * 8:(hh + 1) * 8, :],
                                                in_=pp[hh][:, 0:512].rearrange("p (a b) -> p a b", a=8)[:, :, 0:48]),
                 reads=BP(hh), writes=["gate2"])
        P.op("dve", lambda e: e.tensor_scalar(out=fl(mask2), in0=fl(gate2), scalar1=0.0, scalar2=None, op0=ALU.is_gt),
             reads=["gate2"], writes=["mask2"])
        P.op("dve", lambda e: e.tensor_scalar(out=fl(mask2f), in0=fl(gate2), scalar1=0.0, scalar2=None, op0=ALU.is_gt),
             reads=["gate2"], writes=["mask2f"])
        for hh in range(2):
            P.op("pe", lambda e, hh=hh: e.matmul(bank(4 + hh)[:, 0:384], lhsT=trib[:], rhs=fl(mask2)[:, hh * 384:(hh + 1) * 384],
                                                 start=True, stop=True), reads=["mask2"], writes=["ps:%d" % (4 + hh)])
            P.op("pe", lambda e, hh=hh: e.matmul(bank(6 + hh)[:, 0:384], lhsT=onesb[:], rhs=fl(mask2)[:, hh * 384:(hh + 1) * 384],
                                                 start=True, stop=True), reads=["mask2"], writes=["ps:%d" % (6 + hh)])
            P.op("act", lambda e, hh=hh: e.copy(out=fl(totsb)[:, hh * 384:(hh + 1) * 384], in_=bank(6 + hh)[:, 0:384]),
                 reads=["ps:%d" % (6 + hh)], writes=["totsb"])
        P.op("dve", lambda e: e.memset(base[:, 0, :], 0.0), writes=["base"])
        for t in range(1, NT):
            P.op("dve", lambda e, t=t: e.tensor_tensor(out=base[:, t, :], in0=base[:, t - 1, :], in1=totsb[:, t - 1, :],
                                                      op=ALU.add), reads=["base", "totsb"], writes=["base"])
        for hh in range(2):
            P.op("dve", lambda e, hh=hh: e.tensor_tensor(out=fl(pos)[:, hh * 384:(hh + 1) * 384], in0=bank(4 + hh)[:, 0:384],
                                                        in1=fl(base)[:, hh * 384:(hh + 1) * 384], op=ALU.add),
                 reads=["ps:%d" % (4 + hh), "base"], writes=["pos"])
        P.op("dve", lambda e: e.scalar_tensor_tensor(out=fl(pos), in0=fl(pos), scalar=1.0, in1=fl(mask2f),
                                                     op0=ALU.add, op1=ALU.mult), reads=["pos", "mask2f"], writes=["pos"])
        P.op("dve", lambda e: e.tensor_scalar(out=fl(posm[:]), in0=fl(pos), scalar1=-1.0, scalar2=None, op0=ALU.add),
             reads=["pos"], writes=["posm"])
        for t in range(NT):
            P.op("pe", lambda e, t=t: e.transpose(out=pp[2 + t // 8][0:48, (t % 8) * 128:(t % 8) * 128 + 128],
                                                  in_=posm[:, t, :], identity=identf[:]),
                 reads=["posm"], writes=BP(2 + t // 8))
        for hh in range(2):
            P.op("act", lambda e, hh=hh: e.copy(out=posmT[:, hh * 1024:(hh + 1) * 1024], in_=pp[2 + hh][0:48, :]),
                 reads=BP(2 + hh), writes=["posmT"])
        P.op("dve", lambda e: e.tensor_copy(out=fl(gh), in_=fl(gate2)), reads=["gate2"], writes=["gh"])
        P.op("dve", lambda e: e.tensor_copy(out=fl(ghf), in_=fl(gh)), reads=["gh"], writes=["ghf"])
        P.op("dve", lambda e: e.tensor_tensor(out=ghl[:, :, :, 1], in0=gate2, in1=ghf, op=ALU.subtract),
             reads=["gate2", "ghf"], writes=["ghl1"])
        P.op("dve", lambda e: e.tensor_copy(out=ghl[:, :, :, 0], in_=gh), reads=["gh"], writes=["ghl0"])

    def epoch_e():
        cv = Carver()
        ring = [cv.get([128, 8, 512], BF16) for _ in range(8)]
        S = [cv.get([128, NT, 256], BF16) for _ in range(2)]
        xeT = cv.get([128, 8, 512], BF16)
        hidT = cv.get([128, 16, 512], BF16)
        sil = [cv.get([128, 512], F32) for _ in range(2)]
        ye_sb = cv.get([128, 4, D], F32)
        gt2bc = cv.get([128, 2, D], F32)
        pgs = cv.get([128, 2, 16], F32)
        idxf = cv.get([128, 2, 4], F32)
        gsb = cv.get([128, 2, 4], F32)
        xetok = [cv.get([128, 4, D], BF16) for _ in range(2)]
        d_sc = [P.dsem("scat%d" % gi) for gi in range(4)]
        d_g = [[P.dsem("gath%d_%d" % (pp_, gi)) for gi in range(4)] for pp_ in range(2)]
        x1flat = x1s.rearrange("b t d -> (b t) d")
        for b in range(2):
            for hh in range(2):
                P.op("pe", lambda e, b=b, hh=hh: e.matmul(bank(6), lhsT=sel3[:, b, :], rhs=msb[:, 3, hh * 512:(hh + 1) * 512],
                                                         start=True, stop=True), reads=[], writes=["ps:6"])
                P.op("act", lambda e, b=b, hh=hh: e.copy(out=gt2bc[:, b, hh * 512:(hh + 1) * 512], in_=bank(6)),
                     reads=["ps:6"], writes=["gt2bc"])
        NRING = 8
        uspec = []
        for e2 in range(NE):
            for g in range(4):
                uspec.append(("a", w1[e2][:, g * 512:(g + 1) * 512].rearrange("(k p) f -> p k f", p=128)))
                uspec.append(("a", w3[e2][:, g * 512:(g + 1) * 512].rearrange("(k p) f -> p k f", p=128)))
            for dq in range(4):
                uspec.append(("b", [w2[e2][hf * 1024:(hf + 1) * 1024, dq * 256:(dq + 1) * 256]
                                    .rearrange("(c p) d -> p c d", p=128) for hf in range(2)]))
        issued = [0]

        def uview(u):
            s_ = u % NRING
            if uspec[u][0] == "a":
                return ring[s_], "ring%d" % s_
            return ring[s_].rearrange("p a b -> p (a b)").rearrange("p (c d) -> p c d", c=16), "ring%d" % s_

        def ensure_issued(upto):
            upto = min(upto, len(uspec) - 1)
            while issued[0] <= upto:
                u = issued[0]
                v, key = uview(u)
                if uspec[u][0] == "a":
                    load("pool", v, uspec[u][1], key)
                else:
                    for hf in range(2):
                        load("pool", v[:, hf * 8:(hf + 1) * 8, :], uspec[u][1][hf], key)
                issued[0] += 1

        def prep(e_):
            par = e_ % 2
            for b in range(2):
                col = 32 * b + e_
                for t in range(NT):
                    P.op("dve", lambda e, b=b, t=t, col=col: e.tensor_scalar(out=S[b][:, t, :], in0=iota256[:],
                                                                           scalar1=posm[:, t, col:col + 1], scalar2=None,
                                                                           op0=ALU.is_equal),
                         reads=[], writes=["S%d" % b])
            pg = bank(5)[:, 256:272]
            for b in range(2):
                col = 32 * b + e_
                for half in range(2):
                    gi = b * 2 + half
                    for t in range(NT):
                        P.op("pe", lambda e, b=b, t=t, half=half, gi=gi, col=col: e.matmul(
                            pg[:, gi * 4:gi * 4 + 4], lhsT=S[b][:, t, half * 128:(half + 1) * 128],
                            rhs=ghl[:, t, col, :], start=(t == 0), stop=(t == NT - 1)),
                             reads=["S%d" % b], writes=["ps:5"])
            P.op("dve", lambda e: e.tensor_copy(out=pgs[:, par, :], in_=pg), reads=["ps:5"], writes=["pgs%d" % par])
            pv4 = pgs[:, par, :].rearrange("p (a b) -> p a b", a=4)
            P.op("dve", lambda e: e.tensor_tensor(out=gsb[:, par, :], in0=pv4[:, :, 0], in1=pv4[:, :, 1], op=ALU.add),
                 reads=["pgs%d" % par], writes=["gs%d" % par, "x1gen"])
            P.op("dve", lambda e: e.scalar_tensor_tensor(out=idxf[:, par, :], in0=pv4[:, :, 2], scalar=256.0,
                                                         in1=pv4[:, :, 3], op0=ALU.mult, op1=ALU.add),
                 reads=["pgs%d" % par], writes=["idxf%d" % par])
            P.op("dve", lambda e: e.tensor_copy(out=idxi[:, par * 4:par * 4 + 4], in_=idxf[:, par, :]),
                 reads=["idxf%d" % par], writes=["idxi%d" % par])
            for gi in range(4):
                P.dma("pool", lambda e, gi=gi: e.indirect_dma_start(
                    out=xetok[par][:, gi, :], out_offset=None, in_=h2s[:, :],
                    in_offset=bass.IndirectOffsetOnAxis(ap=idxi[:, par * 4 + gi:par * 4 + gi + 1], axis=0)),
                      d_g[par][gi], reads=["idxi%d" % par], writes=["xetok%d_%d" % (par, gi)])

        import os
        ne_dbg = int(os.environ.get("K_NE", NE))
        prep(0)
        for e_ in range(ne_dbg):
            par = e_ % 2
            gs = gsb[:, par, :]
            for gi in range(4):
                pT = bankb(6 + gi % 2)
                for k in range(8):
                    P.op("pe", lambda e, gi=gi, k=k, pT=pT, par=par: e.transpose(out=pT[:, k * 128:(k + 1) * 128],
                                                                        in_=xetok[par][:, gi, k * 128:(k + 1) * 128],
                                                                        identity=identb[:]),
                         reads=["xetok%d_%d" % (par, gi)], writes=["ps:%d" % (6 + gi % 2)])
                P.op("act", lambda e, gi=gi, pT=pT: e.copy(out=xeT[:, :, gi * 128:(gi + 1) * 128],
                                                           in_=pT.rearrange("p (a b) -> p a b", a=8)),
                     reads=["ps:%d" % (6 + gi % 2)], writes=["xeT"])
            for g in range(4):
                ua = 12 * e_ + 2 * g
                ensure_issued(ua + 1)
                wa, ka = uview(ua)
                wb, kb = uview(ua + 1)
                for mm in range(4):
                    m = g * 4 + mm
                    ph1 = bank(m % 2)
                    ph3 = bank(2 + m % 2)
                    for k in range(8):
                        P.op("pe", lambda e, k=k, mm=mm, wa=wa, ph1=ph1: e.matmul(
                            ph1, lhsT=wa[:, k, mm * 128:(mm + 1) * 128], rhs=xeT[:, k, :], start=(k == 0), stop=(k == 7)),
                             reads=[ka, "xeT"], writes=["ps:%d" % (m % 2)])
                    for k in range(8):
                        P.op("pe", lambda e, k=k, mm=mm, wb=wb, ph3=ph3: e.matmul(
                            ph3, lhsT=wb[:, k, mm * 128:(mm + 1) * 128], rhs=xeT[:, k, :], start=(k == 0), stop=(k == 7)),
                             reads=[kb, "xeT"], writes=["ps:%d" % (2 + m % 2)])
                    P.op("act", lambda e, m=m, ph1=ph1: e.activation(out=sil[m % 2], in_=ph1, func=AF.Silu),
                         reads=["ps:%d" % (m % 2)], writes=["sil%d" % (m % 2)])
                    P.op("dve", lambda e, m=m, ph3=ph3: e.tensor_tensor(out=hidT[:, m, :], in0=sil[m % 2], in1=ph3,
                                                                        op=ALU.mult),
                         reads=["sil%d" % (m % 2), "ps:%d" % (2 + m % 2)], writes=["hidT"])
                ensure_issued(min(ua + 1 + NRING, 12 * e_ + 11))
            ensure_issued(12 * e_ + 11)
            if e_ + 1 < ne_dbg:
                prep(e_ + 1)
            for dq in range(4):
                uw = 12 * e_ + 8 + dq
                wv, kw = uview(uw)
                for sc in range(4):
                    pye = bank(4 + sc % 2)[:, 0:256]
                    for m in range(16):
                        P.op("pe", lambda e, m=m, sc=sc, wv=wv, pye=pye: e.matmul(
                            pye, lhsT=hidT[:, m, sc * 128:(sc + 1) * 128], rhs=wv[:, m, :], start=(m == 0), stop=(m == 15)),
                             reads=[kw, "hidT"], writes=["ps:%d" % (4 + sc % 2)])
                    P.op("dve", lambda e, sc=sc, dq=dq, pye=pye, gs=gs: e.scalar_tensor_tensor(
                        out=ye_sb[:, sc, dq * 256:(dq + 1) * 256], in0=pye, scalar=gs[:, sc:sc + 1],
                        in1=gt2bc[:, sc // 2, dq * 256:(dq + 1) * 256], op0=ALU.mult, op1=ALU.mult),
                         reads=["ps:%d" % (4 + sc % 2), "gs%d" % par, "gt2bc"], writes=["ye_sb"])
                ensure_issued(uw + NRING)
            for gi in range(4):
                P.dma("pool", lambda e, gi=gi, par=par: e.indirect_dma_start(
                    out=x1flat[:, :], out_offset=bass.IndirectOffsetOnAxis(ap=idxi[:, par * 4 + gi:par * 4 + gi + 1], axis=0),
                    in_=ye_sb[:, gi, :], in_offset=None, compute_op=ALU.add),
                      d_sc[gi], reads=["ye_sb", "idxi%d" % par, "x1gen"])

    def epoch_f(b):
        cv = Carver()
        x1t = [cv.get([128, D], F32) for _ in range(3)]
        outt = [cv.get([128, D], F32) for _ in range(2)]
        sqj = cv.get([128, D], BF16)
        for t in range(NT):
            xb = x1t[t % 3]
            xk = "x1t%d" % (t % 3)
            ob = outt[t % 2]
            okk = "outt%d" % (t % 2)
            c0 = 2 * (t % 2)
            load("sync", xb, x1s[b, t * 128:(t + 1) * 128, :], xk)
            P.op("act", lambda e, xb=xb, c0=c0: e.activation(out=sqj, in_=xb, func=AF.Square, accum_out=st[:, c0:c0 + 1]),
                 reads=[xk], writes=["sqj", "st%d" % c0])
            rstd_chain(c0, 1.0 / D)
            P.op("dve", lambda e, xb=xb, ob=ob, c0=c0: e.scalar_tensor_tensor(out=ob, in0=xb, scalar=st[:, c0 + 1:c0 + 2],
                                                                             in1=gfinbc[:], op0=ALU.mult, op1=ALU.mult),
                 reads=[xk, "st%d" % (c0 + 1)], writes=[okk])
            P.dma("sync", lambda e, ob=ob, t=t: e.dma_start(out=y[b, t * 128:(t + 1) * 128, :], in_=ob),
                  d_outs[t % 2], reads=[okk])

    if stage >= 3:
        epoch_d()
        P.barrier_all()
        if stage == 25:
            P.final_wait("sync", d_outs)
            P.emit()
            return nc
        epoch_e()
        P.barrier_all()
        if stage == 26:
            P.final_wait("sync", d_outs)
            P.emit()
            return nc
        for b in range(2):
            epoch_f(b)
            P.barrier_all()

    P.final_wait("sync", d_outs)
    P.emit()
    return nc


def build_moe(nc, P, env):
    raise NotImplementedError


_STAGE = 3


def kernel(x, c, ctx, c_ctx, w_mod, b_mod, g_mix, g_ffn, w_in, q_gain, k_gain, v_gain,
           w_s, b_s, w_out, w_router, w1, w3, w2, g_final):
    f = lambda a: np.ascontiguousarray(np.asarray(a, dtype=np.float32))
    x, c, ctx, c_ctx = f(x), f(c), f(ctx), f(c_ctx)
    consts = _consts()
    rows = np.concatenate([f(g_ffn)[0], f(g_final), np.tile(f(q_gain)[0], 8), np.tile(f(k_gain)[0], 2),
                           f(v_gain)[0]])
    assert rows.shape[0] == 3200
    shared = {
        "w_mod": f(w_mod)[0], "bmod3": np.tile(f(b_mod)[0][None, :], (3, 1)),
        "rows3": np.tile(rows[None, :], (3, 1)),
        "g2": np.concatenate([f(g_mix)[0].reshape(8, 128), f(g_ffn)[0].reshape(8, 128)], axis=0),
        "w_in": f(w_in)[0], "w_s": f(w_s)[0], "b_s": f(b_s)[0], "w_out": f(w_out)[0],
        "w_router": f(w_router)[0], "w1": f(w1)[0], "w3": f(w3)[0], "w2": f(w2)[0],
    }
    shared.update(consts)
    in_maps = []
    for i in range(NCORES):
        m = dict(shared)
        m["x"] = x[2 * i:2 * i + 2]
        m["ctx"] = ctx[2 * i:2 * i + 2]
        m["c3"] = np.concatenate([c[2 * i:2 * i + 2], c_ctx[None, :]], axis=0)
        in_maps.append(m)
    nc = build_nc(_STAGE)
    res = run_bass_kernel_spmd(nc, in_maps, core_ids=list(range(NCORES)))
    return np.concatenate([r["y"] for r in res.results], axis=0)
```

```python
import numpy as np
import concourse.bass as bass
import concourse.mybir as mybir
from concourse.bass_utils import run_bass_kernel_spmd
from contextlib import ExitStack

F32 = mybir.dt.float32
BF16 = mybir.dt.bfloat16
U8 = mybir.dt.uint8
ALU = mybir.AluOpType
AF = mybir.ActivationFunctionType
AX = mybir.AxisListType

COMPUTE = ("pe", "act", "dve", "pool")
POOL_TO_DVE = True
CARVE_LOG = []
EPS = 1e-6
NCORES = 8
T = 2048
NT = 16
D = 1024
NE = 16
CAP = 256


class DSem:
    def __init__(self, name):
        self.name = name
        self.sem = None
        self.count = 0


class Ins:
    __slots__ = ("eng", "fn", "waits", "flag", "idx", "dsem")

    def __init__(self, eng, fn):
        self.eng = eng
        self.fn = fn
        self.waits = []
        self.flag = False
        self.idx = None
        self.dsem = None


class Prog:
    def __init__(self, nc):
        self.nc = nc
        self.q = {e: [] for e in COMPUTE + ("sync",)}
        self.last_w = {}
        self.readers = {}
        self.waited_c = {e: {b: -1 for b in COMPUTE} for e in self.q}
        self.waited_d = {e: {} for e in self.q}
        self.dsems = []

    def dsem(self, name):
        d = DSem(name)
        self.dsems.append(d)
        return d

    def _add_wait(self, ins, dep):
        q = ins.eng
        if dep[0] == "c":
            _, b, idx = dep
            if b == "pe" and q == "pe":
                return
            while idx >= 0 and (self.q[b][idx].fn is None or self.q[b][idx].dsem is not None):
                idx -= 1
            if idx < 0 or self.waited_c[q][b] >= idx:
                return
            self.waited_c[q][b] = idx
            self.q[b][idx].flag = True
            ins.waits.append(("c", b, idx))
        else:
            _, d, cnt = dep
            if self.waited_d[q].get(id(d), 0) >= cnt:
                return
            self.waited_d[q][id(d)] = cnt
            ins.waits.append(dep)

    def _deps(self, ins, reads, writes, token):
        ex = [r for r in reads if r.startswith("ps:")]
        if ex:
            reads = [r for r in reads if not r.startswith("ps:")]
            writes = list(writes) + ex
        for r in reads:
            w = self.last_w.get(r)
            if w is not None:
                self._add_wait(ins, w)
        for w_ in writes:
            w = self.last_w.get(w_)
            if w is not None:
                self._add_wait(ins, w)
            for rd in self.readers.get(w_, {}).values():
                self._add_wait(ins, rd)
        key = token[1] if token[0] == "c" else id(token[1])
        for r in reads:
            self.readers.setdefault(r, {})[key] = token
        for w_ in writes:
            self.last_w[w_] = token
            self.readers[w_] = {}

    def op(self, eng, fn, reads=(), writes=()):
        if eng == "pool!":
            eng = "pool"
        elif eng == "pool" and POOL_TO_DVE:
            eng = "dve"
        ins = Ins(eng, fn)
        ins.idx = len(self.q[eng])
        self._deps(ins, reads, writes, ("c", eng, ins.idx))
        self.q[eng].append(ins)
        return ins

    def dma(self, queue, fn, dsem, reads=(), writes=()):
        ins = Ins(queue, fn)
        ins.idx = len(self.q[queue])
        dsem.count += 16
        ins.dsem = dsem
        self._deps(ins, reads, writes, ("d", dsem, dsem.count))
        self.q[queue].append(ins)
        return ins

    def barrier_all(self):
        for q in self.q:
            ins = Ins(q, None)
            ins.idx = len(self.q[q])
            for b in COMPUTE:
                j = len(self.q[b]) - 1
                if j >= 0 and b != q:
                    self._add_wait(ins, ("c", b, j))
            for d in self.dsems:
                if d.count > 0:
                    self._add_wait(ins, ("d", d, d.count))
            self.q[q].append(ins)
        self.last_w = {}
        self.readers = {}

    def final_wait(self, queue, dsems):
        ins = Ins(queue, None)
        ins.idx = len(self.q[queue])
        for d in dsems:
            if d.count > 0:
                self._add_wait(ins, ("d", d, d.count))
        self.q[queue].append(ins)

    def emit(self):
        nc = self.nc
        with ExitStack() as st:
            csem = {e: st.enter_context(nc.semaphore("cs_" + e)) for e in COMPUTE}
            for d in self.dsems:
                if d.count > 0:
                    d.sem = st.enter_context(nc.semaphore("ds_" + d.name))
            mile = {}
            for e in COMPUTE:
                c = 0
                arr = []
                for ins in self.q[e]:
                    if ins.flag:
                        c += 1
                    arr.append(c)
                mile[e] = arr
            block = st.enter_context(nc.Block())

            def run(qname, eng):
                for ins in self.q[qname]:
                    for dep in ins.waits:
                        if dep[0] == "c":
                            eng.wait_ge(csem[dep[1]], mile[dep[1]][dep[2]])
                        else:
                            eng.wait_ge(dep[1].sem, dep[2])
                    if ins.fn is None:
                        continue
                    r = ins.fn(eng)
                    if ins.dsem is not None:
                        r.then_inc(ins.dsem.sem, 16)
                    elif ins.flag:
                        r.then_inc(csem[qname], 1)

            @block.sync
            def _(sync):
                run("sync", sync)

            @block.scalar
            def _(scalar):
                run("act", scalar)

            @block.vector
            def _(vector):
                run("dve", vector)

            @block.gpsimd
            def _(gpsimd):
                run("pool", gpsimd)

            @block.tensor
            def _(tensor):
                run("pe", tensor)


def _consts():
    c = {}
    c["identf"] = np.eye(128, dtype=np.float32)
    k = np.arange(128)
    c["tri"] = (k[:, None] < k[None, :]).astype(np.float32)
    c["iota256"] = np.tile(np.arange(256, dtype=np.float32)[None, :], (128, 1))
    c["iotap"] = np.stack([k, k + 128], axis=1).astype(np.float32)
    sel48 = np.zeros((48, 32, 128), np.float32)
    for b in range(2):
        for e in range(16):
            sel48[32 * b + e, 16 * b + e, :] = 1.0
    c["sel48"] = sel48.reshape(48, 32 * 128)
    sel3 = np.zeros((3, 3, 128), np.float32)
    for j in range(3):
        sel3[j, j, :] = 1.0
    c["sel3"] = sel3.reshape(3, 384)
    nf = 16
    inv = (10000.0 ** (-np.arange(nf, dtype=np.float32) / nf)).astype(np.float32)
    tok = np.arange(T)
    row = (tok // 64).astype(np.float32)
    col = (tok % 64).astype(np.float32)
    ang = np.concatenate([row[:, None] * inv[None, :], col[:, None] * inv[None, :]], axis=-1).astype(np.float32)
    c["cs"] = np.concatenate([np.cos(ang), np.sin(ang)], axis=-1).astype(np.float32)
    tokc = np.zeros((128, NT, 48, 2), np.float32)
    for t in range(NT):
        for col in range(48):
            bb = col // 32
            tokc[:, t, col, 0] = bb * 8 + t // 2
            tokc[:, t, col, 1] = (t % 2) * 128 + np.arange(128)
    c["tokc"] = tokc.reshape(128, NT * 48 * 2)
    return c


def build_nc(stage=9):
    nc = bass.Bass("TRN2", target_bir_lowering=False)
    P = Prog(nc)

    def din(name, shape):
        return nc.dram_tensor(name, list(shape), F32, kind="ExternalInput").ap()

    x = din("x", [2, T, D])
    ctx = din("ctx", [2, 256, D])
    c3 = din("c3", [3, D])
    w_mod = din("w_mod", [D, 6 * D])
    bmod3 = din("bmod3", [3, 6 * D])
    rows3 = din("rows3", [3, 3200])
    g2 = din("g2", [16, 128])
    w_in = din("w_in", [D, 1792])
    w_s = din("w_s", [8, 128, 128])
    b_s = din("b_s", [8, 128])
    w_out = din("w_out", [D, D])
    w_router = din("w_router", [D, NE])
    w1 = din("w1", [NE, D, 2048])
    w3 = din("w3", [NE, D, 2048])
    w2 = din("w2", [NE, 2048, D])
    k_identf = din("identf", [128, 128])
    k_tri = din("tri", [128, 128])
    k_iota256 = din("iota256", [128, 256])
    k_iotap = din("iotap", [128, 2])
    k_sel48 = din("sel48", [48, 4096])
    k_sel3 = din("sel3", [3, 384])
    k_cs = din("cs", [T, 64])
    y = nc.dram_tensor("y", [2, T, D], F32, kind="ExternalOutput").ap()
    x1s = nc.dram_tensor("x1s", [2, T, D], F32, kind="Internal").ap()
    yes = nc.dram_tensor("yes", [2, NE, 2, 128, D], BF16, kind="Internal").ap()
    h2s = nc.dram_tensor("h2s", [2 * T, D], BF16, kind="Internal").ap()
    k_tokc = din("tokc", [128, NT * 48 * 2])

    def sb(name, shape, dt):
        return nc.alloc_sbuf_tensor(name, list(shape), dt)

    identb = sb("identb", [128, 128], BF16)
    identf = sb("identf_sb", [128, 128], F32)
    trib = sb("trib", [128, 128], BF16)
    onesb = sb("onesb", [128, 128], BF16)
    iota256 = sb("iota256_sb", [128, 256], F32)
    iotap = sb("iotap_sb", [128, 2], F32)
    sel48 = sb("sel48_sb", [48, 32, 128], BF16)
    sel3 = sb("sel3_sb", [3, 3, 128], F32)
    cs = sb("cs_sb", [128, NT, 64], F32)
    gain640 = sb("gain640", [128, 640], F32)
    vgainbc = sb("vgainbc", [128, 512], F32)
    gfinbc = sb("gfinbc", [128, D], F32)
    wsT = sb("wsT", [128, 8, 128], BF16)
    bsT = sb("bsT", [128, 8], F32)
    wr = sb("wr", [128, 8, NE], BF16)
    scT = sb("scT", [128, 8, 3], BF16)
    mT = sb("mT", [128, 48, 3], F32)
    gT = sb("gT", [128, 16], F32)
    s1T = sb("s1T", [128, 8, 3], F32)
    msb = sb("msb", [3, 4, D], F32)
    posm = sb("posm", [128, NT, 48], F32)
    ghl = sb("ghl", [128, NT, 48, 4], BF16)
    idxi = sb("idxi", [128, 8], mybir.dt.int32)
    posmT = sb("posmT", [48, T], BF16)
    st = sb("st", [128, 64], F32)
    epsb = sb("epsb", [128, 1], F32)

    RBYTES = 150 * 1024
    R = sb("R", [128, RBYTES], U8)

    class Carver:
        def __init__(self):
            self.off = 0
            CARVE_LOG.append([])

        def get(self, shape, dt, parts=128):
            es = 2 if dt == BF16 else 4
            n = int(np.prod(shape[1:]))
            nb = n * es
            off = (self.off + 63) // 64 * 64
            assert off + nb <= RBYTES, (off, nb)
            v = R[0:parts, off:off + nb].bitcast(dt)
            CARVE_LOG[-1].append((off, tuple(shape), "bf16" if dt == BF16 else "f32"))
            self.off = off + nb
            if len(shape) == 3:
                v = v.rearrange("p (a b) -> p a b", a=shape[1])
            elif len(shape) == 4:
                v = v.rearrange("p (a b c) -> p a b c", a=shape[1], b=shape[2])
            return v

    pp = [nc.alloc_psum_tensor("pp%d" % i, [128, 1024], F32) for i in range(4)]

    def bank(i):
        return pp[i // 2][:, (i % 2) * 512:(i % 2) * 512 + 512]

    def BP(j):
        return ["ps:%d" % (2 * j), "ps:%d" % (2 * j + 1)]

    def bankb(i):
        return bank(i).bitcast(BF16)

    dcache = {}

    def dget(key):
        if key not in dcache:
            dcache[key] = P.dsem("d%d" % len(dcache))
        return dcache[key]

    def load(queue, dst, src, key, reads=()):
        P.dma(queue, lambda e: e.dma_start(out=dst, in_=src), dget(key), reads=reads, writes=[key])

    d_outs = [P.dsem("out0"), P.dsem("out1")]

    cv = Carver()
    c3_sb = cv.get([3, D], F32, parts=3)
    sc3 = cv.get([3, D], F32, parts=3)
    rows3_sb = cv.get([3, 3200], F32, parts=3)
    bmod_sb = cv.get([3, 6 * D], F32, parts=3)
    wm = [cv.get([128, 8, 512], BF16) for _ in range(3)]
    wstmp = cv.get([128, 8, 128], F32)
    g2_sb = cv.get([16, 128], F32, parts=16)
    bs_sb = cv.get([8, 128], F32, parts=8)
    mtmp = [cv.get([3, 512], F32, parts=3) for _ in range(2)]
    trif = cv.get([128, 128], F32)
    sel48f = cv.get([48, 4096], F32, parts=48)

    load("sync", identf[:], k_identf, "identf")
    load("pool", identb[:], k_identf, "identb")
    load("pool", trib[:], k_tri, "trib")
    load("sync", iota256[:], k_iota256, "iota256")
    load("sync", iotap[:], k_iotap, "iotap")
    load("pool", sel48[:].rearrange("p a b -> p (a b)"), k_sel48, "sel48")
    load("sync", sel3[:].rearrange("p a b -> p (a b)"), k_sel3, "sel3")
    load("sync", cs[:], k_cs.rearrange("(i p) c -> p i c", p=128), "cs")
    load("sync", c3_sb, c3, "c3")
    load("sync", rows3_sb, rows3, "rows3")
    load("sync", bmod_sb, bmod3, "bmod")
    load("sync", g2_sb, g2, "g2")
    load("sync", bs_sb, b_s, "bs")
    load("sync", wstmp, w_s.rearrange("g i j -> i g j"), "wstmp")
    load("pool", wr[:], w_router.rearrange("(k p) e -> p k e", p=128), "wr")
    P.op("pool!", lambda e: e.memset(onesb[:], 1.0), writes=["onesb"])
    P.op("pool!", lambda e: e.memset(epsb[:], EPS), writes=["epsb"])

    P.op("act", lambda e: e.activation(out=sc3, in_=c3_sb, func=AF.Silu), reads=["c3"], writes=["sc3"])
    for k in range(8):
        P.op("pe", lambda e, k=k: e.transpose(out=bank(0)[:, k * 3:k * 3 + 3], in_=sc3[:, k * 128:(k + 1) * 128],
                                              identity=identf[0:3, 0:3]), reads=["sc3", "identf"], writes=["ps:0"])
    P.op("dve", lambda e: e.tensor_copy(out=scT[:].rearrange("p a b -> p (a b)"), in_=bank(0)[:, 0:24]),
         reads=["ps:0"], writes=["scT"])

    psT = bank(3)
    for n in range(12):
        seg, hh = n // 2, n % 2
        wmb = wm[n % 3]
        load("pool", wmb, w_mod[:, n * 512:(n + 1) * 512].rearrange("(k p) n -> p k n", p=128), "wm%d" % (n % 3))
        pm = bank(1 + n % 2)
        pk = "ps:%d" % (1 + n % 2)
        for k in range(8):
            P.op("pe", lambda e, k=k, wmb=wmb, pm=pm: e.matmul(pm[0:3, :], lhsT=scT[:, k, :], rhs=wmb[:, k, :],
                                                               start=(k == 0), stop=(k == 7)),
                 reads=["scT", "wm%d" % (n % 3)], writes=[pk])
        mt = mtmp[n % 2]
        mk = "mtmp%d" % (n % 2)
        P.op("dve", lambda e, pm=pm, mt=mt, n=n: e.tensor_tensor(out=mt, in0=pm[0:3, :],
                                                                in1=bmod_sb[:, n * 512:(n + 1) * 512], op=ALU.add),
             reads=[pk, "bmod"], writes=[mk])
        for j in range(4):
            cidx = (n * 4 + j) * 3
            P.op("pe", lambda e, mt=mt, j=j, cidx=cidx: e.transpose(out=psT[:, cidx:cidx + 3],
                                                                   in_=mt[:, j * 128:(j + 1) * 128],
                                                                   identity=identf[0:3, 0:3]),
                 reads=[mk, "identf"], writes=["ps:3"])
        if seg == 2:
            P.op("act", lambda e, mt=mt, hh=hh: e.copy(out=msb[:, 0, hh * 512:(hh + 1) * 512], in_=mt),
                 reads=[mk], writes=["msb"])
        elif seg == 3:
            P.op("act", lambda e, mt=mt, hh=hh: e.copy(out=msb[:, 2, hh * 512:(hh + 1) * 512], in_=mt),
                 reads=[mk], writes=["msb"])
        elif seg == 5:
            P.op("act", lambda e, mt=mt, hh=hh: e.copy(out=msb[:, 3, hh * 512:(hh + 1) * 512], in_=mt),
                 reads=[mk], writes=["msb"])
        elif seg == 4:
            P.op("dve", lambda e, mt=mt, hh=hh: e.scalar_tensor_tensor(
                out=msb[:, 1, hh * 512:(hh + 1) * 512], in0=mt, scalar=1.0,
                in1=rows3_sb[:, hh * 512:(hh + 1) * 512], op0=ALU.add, op1=ALU.mult),
                 reads=[mk, "rows3"], writes=["msb"])
    P.op("dve", lambda e: e.tensor_copy(out=mT[:].rearrange("p a b -> p (a b)"), in_=psT[:, 0:144]),
         reads=["ps:3"], writes=["mT"])
    P.op("pe", lambda e: e.transpose(out=bank(0)[:, 32:48], in_=g2_sb, identity=identf[0:16, 0:16]),
         reads=["g2", "identf"], writes=["ps:0"])
    P.op("dve", lambda e: e.tensor_copy(out=gT[:], in_=bank(0)[:, 32:48]), reads=["ps:0"], writes=["gT"])
    P.op("dve", lambda e: e.scalar_tensor_tensor(out=s1T[:], in0=mT[:, 8:16, :], scalar=1.0,
                                                 in1=gT[:, 0:8].unsqueeze(2).to_broadcast([128, 8, 3]),
                                                 op0=ALU.add, op1=ALU.mult),
         reads=["mT", "gT"], writes=["s1T"])
    for (dst, c0, n_) in ((gfinbc[:, 0:512], 1024, 512), (gfinbc[:, 512:1024], 1536, 512),
                          (gain640[:, 0:512], 2048, 512), (gain640[:, 512:640], 2560, 128),
                          (vgainbc[:, 0:512], 2688, 512)):
        P.op("pe", lambda e, c0=c0, n_=n_: e.matmul(bank(4)[:, 0:n_], lhsT=sel3[:, 0, :], rhs=rows3_sb[:, c0:c0 + n_],
                                                    start=True, stop=True), reads=["sel3", "rows3"], writes=["ps:4"])
        P.op("act", lambda e, dst=dst, n_=n_: e.copy(out=dst, in_=bank(4)[:, 0:n_]), reads=["ps:4"], writes=["bcst"])
    for g in range(8):
        P.op("pe", lambda e, g=g: e.transpose(out=bank(5 + g // 4)[:, (g % 4) * 128:(g % 4) * 128 + 128],
                                              in_=wstmp[:, g, :], identity=identf[:]),
             reads=["wstmp", "identf"], writes=["ps:%d" % (5 + g // 4)])
    for h_ in range(2):
        P.op("act", lambda e, h_=h_: e.copy(out=wsT[:, h_ * 4:(h_ + 1) * 4, :].rearrange("p a b -> p (a b)"),
                                            in_=bank(5 + h_)), reads=["ps:%d" % (5 + h_)], writes=["wsT"])
    P.op("pe", lambda e: e.transpose(out=bank(0)[:, 64:72], in_=bs_sb, identity=identf[0:8, 0:8]),
         reads=["bs", "identf", "gT"], writes=["ps:0"])
    P.op("dve", lambda e: e.tensor_copy(out=bsT[:], in_=bank(0)[:, 64:72]), reads=["ps:0"], writes=["bsT"])
    P.barrier_all()
    if stage == 0:
        P.dma("sync", lambda e: e.dma_start(out=y[0, 0:128, :], in_=gfinbc[:]), d_outs[0], reads=["bcst"])
        P.dma("sync", lambda e: e.dma_start(out=y[0, 128:256, 0:640], in_=gain640[:]), d_outs[0], reads=["bcst"])
        P.dma("sync", lambda e: e.dma_start(out=y[0, 256:384, 0:144], in_=mT[:].rearrange("p a b -> p (a b)")), d_outs[0])
        P.dma("sync", lambda e: e.dma_start(out=y[0, 384:387, :], in_=msb[:, 1, :]), d_outs[0])
        P.dma("sync", lambda e: e.dma_start(out=y[0, 512:640, 0:24], in_=s1T[:].rearrange("p a b -> p (a b)")), d_outs[0])
        P.final_wait("sync", d_outs)
        P.emit()
        return nc

    def rstd_chain(col, scale):
        P.op("act", lambda e: e.activation(out=st[:, col + 1:col + 2], in_=st[:, col:col + 1], func=AF.Sqrt,
                                           scale=scale, bias=epsb[:, 0:1]),
             reads=["st%d" % col, "epsb"], writes=["st%d" % (col + 1)])
        P.op("dve", lambda e: e.reciprocal(out=st[:, col + 1:col + 2], in_=st[:, col + 1:col + 2]),
             reads=["st%d" % (col + 1)], writes=["st%d" % (col + 1)])

    def epoch_ac(b):
        cv = Carver()
        wio = cv.get([128, 8, 1792], BF16)
        qa = cv.get([128, 4, T], BF16)
        gmT = cv.get([128, 4, T], BF16)
        kT = cv.get([128, 2, 2304], BF16)
        vaug = cv.get([128, 18, 2, 192], BF16)
        xt = [cv.get([128, D], F32) for _ in range(2)]
        sqj = cv.get([128, D], BF16)
        xn = cv.get([128, D], BF16)
        hTt = cv.get([128, 8, 128], BF16)
        qk = cv.get([128, 10, 64], F32)
        qk2 = cv.get([128, 10, 64], F32)
        qkr = cv.get([128, 10, 64], BF16)
        kd = cv.get([128, 4, 64], BF16)
        rp = cv.get([128, 4, 320], F32)
        u_sb = cv.get([128, 512], F32)
        vv_sb = cv.get([128, 512], F32)
        vvn = cv.get([128, 512], BF16)
        gtmp = cv.get([128, 512], F32)
        gmb = cv.get([128, 512], BF16)
        PT = [cv.get([128, 1024], BF16) for _ in range(4)]
        rd = cv.get([128, 512], F32)
        gt1bc = cv.get([128, D], F32)
        tmpo = cv.get([128, D], F32)
        s10 = st[:, 16:26]
        r10 = st[:, 32:42]

        for kk in range(4):
            load("pool", wio[:, 2 * kk:2 * kk + 2, :],
                 w_in[kk * 256:(kk + 1) * 256, :].rearrange("(k p) n -> p k n", p=128), "wio")
        for hh in range(2):
            P.op("pe", lambda e, hh=hh: e.matmul(bank(0), lhsT=sel3[:, b, :], rhs=msb[:, 0, hh * 512:(hh + 1) * 512],
                                                 start=True, stop=True), reads=["sel3", "msb"], writes=["ps:0"])
            P.op("act", lambda e, hh=hh: e.copy(out=gt1bc[:, hh * 512:(hh + 1) * 512], in_=bank(0)),
                 reads=["ps:0"], writes=["gt1bc"])
        P.op("pool!", lambda e: e.memset(vaug.rearrange("p a b c -> p (a b c)"), 1.0), writes=["vaug"])

        g3 = gain640[:].rearrange("p (a b) -> p a b", a=10)
        qkf = qk.rearrange("p a b -> p (a b)")
        qkrf = qkr.rearrange("p a b -> p (a b)")
        kdf = kd.rearrange("p a b -> p (a b)")
        kdv = kd.rearrange("p (a b) c -> p a b c", a=2)
        rpv = [rp[:, i, :].rearrange("p (a b) -> p a b", a=10) for i in range(4)]
        pA = bankb(0)
        pB = bankb(5)
        pC = bankb(7)

        def X1(t):
            lat = t < 16
            xb = xt[t % 2]
            xk = "xt%d" % (t % 2)
            src = x[b, t * 128:(t + 1) * 128, :] if lat else ctx[b, (t - 16) * 128:(t - 15) * 128, :]
            load("sync", xb, src, xk)
            P.op("act", lambda e: e.activation(out=sqj, in_=xb, func=AF.Square, accum_out=st[:, 0:1]),
                 reads=[xk], writes=["sqj", "st0"])
            rstd_chain(0, 1.0 / D)
            P.op("act", lambda e: e.activation(out=xn, in_=xb, func=AF.Copy, scale=st[:, 1:2]),
                 reads=[xk, "st1"], writes=["xn"])
            for k in range(8):
                P.op("pe", lambda e, k=k: e.transpose(out=pA[:, k * 128:(k + 1) * 128], in_=xn[:, k * 128:(k + 1) * 128],
                                                      identity=identb[:]), reads=["xn", "identb"], writes=["ps:0"])

        def X2(t):
            lat = t < 16
            j = b if lat else 2
            for k in range(8):
                eng_ = "dve"
                if eng_ == "act":
                    P.op("act", lambda e, k=k: e.activation(out=hTt[:, k, :], in_=pA[:, k * 128:(k + 1) * 128],
                                                            func=AF.Identity, scale=s1T[:, k, j:j + 1],
                                                            bias=mT[:, k, j:j + 1]),
                         reads=["ps:0", "s1T", "mT"], writes=["hTt%d" % k])
                else:
                    P.op("dve", lambda e, k=k: e.tensor_scalar(out=hTt[:, k, :], in0=pA[:, k * 128:(k + 1) * 128],
                                                              scalar1=s1T[:, k, j:j + 1], scalar2=mT[:, k, j:j + 1],
                                                              op0=ALU.mult, op1=ALU.add),
                         reads=["ps:0", "s1T", "mT"], writes=["hTt%d" % k])
            slices = [(1, 0, 512), (2, 512, 256), (3, 768, 512), (4, 1280, 512)] if lat else [(2, 512, 256)]
            for (bi, c0, n_) in slices:
                for k in range(8):
                    P.op("pe", lambda e, bi=bi, c0=c0, n_=n_, k=k: e.matmul(bank(bi)[:, 0:n_], lhsT=hTt[:, k, :],
                                                                           rhs=wio[:, k, c0:c0 + n_],
                                                                           start=(k == 0), stop=(k == 7)),
                         reads=["hTt%d" % k, "wio"], writes=["ps:%d" % bi])

        def Ya(t):
            lat = t < 16
            if lat:
                P.op("dve", lambda e: e.tensor_copy(out=qkf[:, 0:512], in_=bank(1)), reads=["ps:1"], writes=["qk"])
            P.op("dve", lambda e: e.tensor_copy(out=qkf[:, 512:640], in_=bank(2)[:, 0:128]), reads=["ps:2"], writes=["qk"])
            P.op("act", lambda e: e.copy(out=vaug[:, t, :, 64:128],
                                         in_=bank(2)[:, 128:256].rearrange("p (a b) -> p a b", a=2)),
                 reads=["ps:2"], writes=["vaug"])
            if lat:
                P.op("act", lambda e: e.activation(out=u_sb, in_=bank(3), func=AF.Gelu), reads=["ps:3"], writes=["u_sb"])
                P.op("act", lambda e: e.activation(out=vv_sb, in_=bank(4), func=AF.Gelu), reads=["ps:4"], writes=["vv_sb"])

        def Yb(t):
            lat = t < 16
            h0 = 0 if lat else 8
            nh = 10 - h0
            if lat:
                P.op("act", lambda e: e.activation(out=gtmp, in_=vv_sb, func=AF.Square, accum_out=st[:, 4:5]),
                     reads=["vv_sb"], writes=["gtmp", "st4"])
                P.op("act", lambda e: e.activation(out=st[:, 5:6], in_=st[:, 4:5], func=AF.Sqrt, scale=1.0 / 512,
                                                   bias=epsb[:, 0:1]), reads=["st4", "epsb"], writes=["st5"])
            P.op("dve", lambda e: e.tensor_tensor(out=qk2[:, h0:10, :], in0=qk[:, h0:10, :], in1=qk[:, h0:10, :],
                                                  op=ALU.mult), reads=["qk"], writes=["qk2"])
            P.op("dve", lambda e: e.tensor_reduce(out=s10[:, h0:10], in_=qk2[:, h0:10, :], axis=AX.X, op=ALU.add),
                 reads=["qk2"], writes=["s10"])
            P.op("act", lambda e: e.activation(out=r10[:, h0:10], in_=s10[:, h0:10], func=AF.Sqrt,
                                               scale=1.0 / 64, bias=epsb[:, 0:1]),
                 reads=["s10", "epsb"], writes=["r10"])
            if lat:
                P.op("dve", lambda e: e.reciprocal(out=st[:, 5:6], in_=st[:, 5:6]), reads=["st5"], writes=["st5"])
                P.op("dve", lambda e: e.scalar_tensor_tensor(out=vvn, in0=vv_sb, scalar=st[:, 5:6], in1=vgainbc[:],
                                                             op0=ALU.mult, op1=ALU.mult),
                     reads=["vv_sb", "st5", "bcst"], writes=["vvn"])
                for g in range(8):
                    P.op("pe", lambda e, g=g: e.matmul(bank(6)[:, g * 64:(g + 1) * 64], lhsT=wsT[:, g, :],
                                                       rhs=vvn[:, g * 64:(g + 1) * 64], start=True, stop=True),
                         reads=["wsT", "vvn"], writes=["ps:6"])
            P.op("dve", lambda e: e.reciprocal(out=r10[:, h0:10], in_=r10[:, h0:10]), reads=["r10"], writes=["r10"])
            P.op("dve", lambda e: e.tensor_tensor(out=qk2[:, h0:10, :], in0=qk[:, h0:10, :],
                                                  in1=r10[:, h0:10].unsqueeze(2).to_broadcast([128, nh, 64]),
                                                  op=ALU.mult), reads=["qk", "r10"], writes=["qk2"])
            P.op("dve", lambda e: e.tensor_tensor(out=qk2[:, h0:10, :], in0=qk2[:, h0:10, :], in1=g3[:, h0:10, :],
                                                  op=ALU.mult), reads=["qk2", "bcst"], writes=["qk2"])
            if lat:
                cosb = cs[:, t, 0:32].unsqueeze(1).to_broadcast([128, 10, 32])
                sinb = cs[:, t, 32:64].unsqueeze(1).to_broadcast([128, 10, 32])
                x1v = qk2[:, :, 0:32]
                x2v = qk2[:, :, 32:64]
                P.op("dve", lambda e: e.tensor_tensor(out=rpv[0], in0=x1v, in1=cosb, op=ALU.mult),
                     reads=["qk2", "cs"], writes=["rp0"])
                P.op("dve", lambda e: e.tensor_tensor(out=rpv[1], in0=x2v, in1=sinb, op=ALU.mult),
                     reads=["qk2", "cs"], writes=["rp1"])
                P.op("pool", lambda e: e.tensor_tensor(out=rpv[2], in0=x1v, in1=sinb, op=ALU.mult),
                     reads=["qk2", "cs"], writes=["rp2"])
                P.op("pool", lambda e: e.tensor_tensor(out=rpv[3], in0=x2v, in1=cosb, op=ALU.mult),
                     reads=["qk2", "cs"], writes=["rp3"])
                P.op("dve", lambda e: e.tensor_tensor(out=qkr[:, :, 0:32], in0=rpv[0], in1=rpv[1], op=ALU.subtract),
                     reads=["rp0", "rp1"], writes=["qkr"])
                P.op("pool", lambda e: e.tensor_tensor(out=qkr[:, :, 32:64], in0=rpv[2], in1=rpv[3], op=ALU.add),
                     reads=["rp2", "rp3"], writes=["qkr"])
            else:
                P.op("dve", lambda e: e.tensor_copy(out=qkr[:, 8:10, :], in_=qk2[:, 8:10, :]), reads=["qk2"], writes=["qkr"])
            P.op("pool", lambda e: e.tensor_copy(out=kdv, in_=qkr[:, 8:10, :].unsqueeze(2).to_broadcast([128, 2, 2, 64])),
                 reads=["qkr"], writes=["kd"])
            if lat:
                for c in range(4):
                    P.op("pe", lambda e, c=c: e.transpose(out=pB[:, c * 128:(c + 1) * 128], in_=qkrf[:, c * 128:(c + 1) * 128],
                                                          identity=identb[:]), reads=["qkr", "identb"], writes=["ps:5"])
            for kv in range(2):
                P.op("pe", lambda e, kv=kv: e.transpose(out=pB[:, 512 + kv * 128:512 + (kv + 1) * 128],
                                                        in_=kdf[:, kv * 128:(kv + 1) * 128], identity=identb[:]),
                     reads=["kd", "identb"], writes=["ps:5"])
            if lat:
                P.op("act", lambda e: e.copy(out=qa[:, :, t * 128:(t + 1) * 128],
                                             in_=pB[:, 0:512].rearrange("p (a b) -> p a b", a=4)),
                     reads=["ps:5"], writes=["qa"])
            P.op("act", lambda e: e.copy(out=kT[:, :, t * 128:(t + 1) * 128],
                                         in_=pB[:, 512:768].rearrange("p (a b) -> p a b", a=2)),
                 reads=["ps:5"], writes=["kT"])
            if not lat:
                return
            P.op("dve", lambda e: e.tensor_tensor(out=gtmp.rearrange("p (a b) -> p a b", a=8),
                                                  in0=bank(6).rearrange("p (a b) -> p a b", a=8),
                                                  in1=bsT[:].unsqueeze(2).to_broadcast([128, 8, 64]), op=ALU.add),
                 reads=["ps:6", "bsT"], writes=["gtmp"])
            P.op("pool", lambda e: e.tensor_tensor(out=gmb, in0=gtmp, in1=u_sb, op=ALU.mult),
                 reads=["gtmp", "u_sb"], writes=["gmb"])
            for c in range(4):
                P.op("pe", lambda e, c=c: e.transpose(out=pC[:, c * 128:(c + 1) * 128], in_=gmb[:, c * 128:(c + 1) * 128],
                                                      identity=identb[:]), reads=["gmb", "identb"], writes=["ps:7"])
            P.op("act", lambda e: e.copy(out=gmT[:, :, t * 128:(t + 1) * 128],
                                         in_=pC[:, 0:512].rearrange("p (a b) -> p a b", a=4)),
                 reads=["ps:7"], writes=["gmT"])

        NTA = 18
        X1(0)
        X2(0)
        X1(1)
        for t in range(NTA):
            Ya(t)
            if t + 1 < NTA:
                X2(t + 1)
            if t + 2 < NTA:
                X1(t + 2)
            Yb(t)

        P.barrier_all()
        steps = [(c, qg, sc_) for c in range(4) for qg in range(4) for sc_ in range(18)]
        Osb = cv.get([128, 1024], F32)

        def qk_step(i):
            c, qg, sc_ = steps[i]
            kv = c // 2
            sj = (0, 1, 3)[i % 3]
            S = pp[sj]
            for half in range(2):
                r0 = half * 64
                P.op("pe", lambda e, half=half, r0=r0: e.matmul(
                    S[:, half * 512:(half + 1) * 512], lhsT=kT[r0:r0 + 64, kv, sc_ * 128:(sc_ + 1) * 128],
                    rhs=qa[r0:r0 + 64, c, qg * 512:(qg + 1) * 512], start=True, stop=True),
                     reads=["kT", "qa%d_%d_%d" % (c, half, qg), "qa"], writes=BP(sj))
            P.op("act", lambda e: e.activation(out=PT[i % 4], in_=S[:], func=AF.Exp, scale=0.125),
                 reads=BP(sj), writes=["PT%d" % (i % 4)])

        def pv_step(i):
            c, qg, sc_ = steps[i]
            kv = c // 2
            for half in range(2):
                off = 64 if half == 0 else 0
                P.op("pe", lambda e, half=half, off=off: e.matmul(
                    bank(4 + half), lhsT=vaug[:, sc_, kv, off:off + 128],
                    rhs=PT[i % 4][:, half * 512:(half + 1) * 512], start=(sc_ == 0), stop=(sc_ == 17)),
                     reads=["vaug", "PT%d" % (i % 4)], writes=["ps:%d" % (4 + half)])
            if sc_ == 17:
                P.op("dve", lambda e: e.tensor_copy(out=Osb, in_=pp[2][:]), reads=BP(2), writes=["Osb"])
                for half in range(2):
                    nr = half * 64
                    dr = 64 - nr
                    P.op("dve", lambda e, half=half, nr=nr, dr=dr: e.reciprocal(
                        out=rd[nr:nr + 64, :], in_=Osb[dr:dr + 64, half * 512:(half + 1) * 512]),
                         reads=["Osb"], writes=["rd%d" % half])
                    P.op("dve", lambda e, half=half, nr=nr: e.tensor_tensor(
                        out=qa[nr:nr + 64, c, qg * 512:(qg + 1) * 512], in0=Osb[nr:nr + 64, half * 512:(half + 1) * 512],
                        in1=rd[nr:nr + 64, :], op=ALU.mult),
                         reads=["Osb", "rd%d" % half], writes=["qa%d_%d_%d" % (c, half, qg)])

        if stage >= 2:
            n = len(steps)
            qk_step(0)
            qk_step(1)
            for i in range(n):
                if i + 2 < n:
                    qk_step(i + 2)
                pv_step(i)

        for kk in range(4):
            load("pool", wio[:, 2 * kk:2 * kk + 2, 0:1024],
                 w_out[kk * 256:(kk + 1) * 256, :].rearrange("(k p) n -> p k n", p=128), "wio")
        tmpo2 = [tmpo, Osb]
        for t in range(16):
            xb = xt[t % 2]
            xk = "xt%d" % (t % 2)
            load("sync", xb, x[b, t * 128:(t + 1) * 128, :], xk)
            pj = 2 + t % 2
            pO = pp[pj]
            tb = tmpo2[t % 2]
            tk = ("tmpo", "Osb")[t % 2]
            for hh in range(2):
                for k in range(8):
                    src = qa[:, k, t * 128:(t + 1) * 128] if k < 4 else gmT[:, k - 4, t * 128:(t + 1) * 128]
                    rk = ["qa%d_%d_%d" % (k, hf, t // 4) for hf in range(2)] + ["qa"] if k < 4 else ["gmT"]
                    P.op("pe", lambda e, hh=hh, k=k, src=src, pO=pO: e.matmul(pO[:, hh * 512:(hh + 1) * 512], lhsT=src,
                                                                             rhs=wio[:, k, hh * 512:(hh + 1) * 512],
                                                                             start=(k == 0), stop=(k == 7)),
                         reads=rk + ["wio"], writes=BP(pj))
            P.op("dve", lambda e, pO=pO, tb=tb: e.tensor_tensor(out=tb, in0=pO[:], in1=gt1bc, op=ALU.mult),
                 reads=BP(pj) + ["gt1bc"], writes=[tk])
            P.op("pool", lambda e, xb=xb, tb=tb: e.tensor_tensor(out=tb, in0=xb, in1=tb, op=ALU.add),
                 reads=[tk, xk], writes=[tk])
            dst = x1s[b, t * 128:(t + 1) * 128, :] if stage >= 3 else y[b, t * 128:(t + 1) * 128, :]
            P.dma("sync", lambda e, tb=tb, dst=dst: e.dma_start(out=dst, in_=tb), d_outs[t % 2],
                  reads=[tk], writes=["x1s%d_%d" % (b, t)])

    for b in range(2):
        epoch_ac(b)
    P.barrier_all()


    def epoch_d():
        cv = Carver()
        h2 = cv.get([128, 2 * NT, D], BF16)
        x1t = [cv.get([128, D], F32) for _ in range(2)]
        sqj = cv.get([128, D], BF16)
        tmp = cv.get([128, D], F32)
        s2bc = cv.get([128, D], F32)
        sh2bc = cv.get([128, D], F32)
        h2Tt = cv.get([128, D], BF16)
        ex = cv.get([128, 16], F32)
        aff2 = cv.get([128, NT, 48], F32)
        affT = cv.get([48, T], F32, parts=48)
        work = cv.get([48, T], F32, parts=48)
        mx8 = cv.get([48, 8], F32, parts=48)
        gate2 = cv.get([128, NT, 48], F32)
        mask2 = cv.get([128, NT, 48], BF16)
        mask2f = cv.get([128, NT, 48], F32)
        totsb = cv.get([128, NT, 48], F32)
        base = cv.get([128, NT, 48], F32)
        pos = cv.get([128, NT, 48], F32)
        ghf = cv.get([128, NT, 48], F32)
        gh = cv.get([128, NT, 48], BF16)
        fl = lambda v: v.rearrange("p a b -> p (a b)")

        P.op("pool!", lambda e: e.memset(fl(aff2), 0.0), writes=["aff2"])
        d_h2s = [P.dsem("h2s0"), P.dsem("h2s1")]
        tokf = cv.get([128, NT * 48, 2], F32)
        load("sync", tokf.rearrange("p a b -> p (a b)"), k_tokc, "tokf")
        P.op("dve", lambda e: e.tensor_copy(out=ghl[:].rearrange("p a b c -> p (a b) c")[:, :, 2:4], in_=tokf),
             reads=["tokf"], writes=["ghl23"])
        for b in range(2):
            for (dst, ri, key) in ((s2bc, 1, "s2bc"), (sh2bc, 2, "sh2bc")):
                for hh in range(2):
                    P.op("pe", lambda e, ri=ri, hh=hh, b=b: e.matmul(bank(0), lhsT=sel3[:, b, :],
                                                               rhs=msb[:, ri, hh * 512:(hh + 1) * 512],
                                                               start=True, stop=True), reads=[], writes=["ps:0"])
                    P.op("act", lambda e, dst=dst, hh=hh: e.copy(out=dst[:, hh * 512:(hh + 1) * 512], in_=bank(0)),
                         reads=["ps:0"], writes=[key])
            def DX(t, b=b):
                xb = x1t[t % 2]
                xk = "x1t%d" % (t % 2)
                load("sync", xb, x1s[b, t * 128:(t + 1) * 128, :], xk)
                P.op("act", lambda e: e.activation(out=sqj, in_=xb, func=AF.Square, accum_out=st[:, 0:1]),
                     reads=[xk], writes=["sqj", "st0"])
                rstd_chain(0, 1.0 / D)
                P.op("dve", lambda e: e.scalar_tensor_tensor(out=tmp, in0=xb, scalar=st[:, 1:2], in1=s2bc,
                                                             op0=ALU.mult, op1=ALU.mult),
                     reads=[xk, "st1", "s2bc"], writes=["tmp"])
                h2t = h2[:, b * NT + t, :]
                P.op("pool", lambda e: e.tensor_tensor(out=h2t, in0=tmp, in1=sh2bc, op=ALU.add),
                     reads=["tmp", "sh2bc"], writes=["h2t"])
                P.dma("sync", lambda e: e.dma_start(out=h2s[b * T + t * 128:b * T + (t + 1) * 128, :], in_=h2t),
                      d_h2s[t % 2], reads=["h2t"])
                pA = bankb(1)
                for k in range(8):
                    P.op("pe", lambda e, k=k: e.transpose(out=pA[:, k * 128:(k + 1) * 128],
                                                          in_=h2t[:, k * 128:(k + 1) * 128], identity=identb[:]),
                         reads=["h2t"], writes=["ps:1"])
                P.op("act", lambda e: e.copy(out=h2Tt, in_=pA), reads=["ps:1"], writes=["h2Tt"])
                lg = bank(2)[:, t * 16:(t + 1) * 16]
                for k in range(8):
                    P.op("pe", lambda e, k=k: e.matmul(lg, lhsT=h2Tt[:, k * 128:(k + 1) * 128], rhs=wr[:, k, :],
                                                       start=(k == 0), stop=(k == 7)),
                         reads=["h2Tt"], writes=["ps:2"])

            def DY(t, b=b):
                lg = bank(2)[:, t * 16:(t + 1) * 16]
                P.op("dve", lambda e: e.tensor_reduce(out=st[:, 8:9], in_=lg, axis=AX.X, op=ALU.max),
                     reads=["ps:2"], writes=["st8"])
                P.op("dve", lambda e: e.tensor_scalar(out=st[:, 9:10], in0=st[:, 8:9], scalar1=-1.0, scalar2=None,
                                                      op0=ALU.mult), reads=["st8"], writes=["st9"])
                P.op("act", lambda e: e.activation(out=ex, in_=lg, func=AF.Exp, bias=st[:, 9:10],
                                                   accum_out=st[:, 10:11]),
                     reads=["ps:2", "st9"], writes=["ex", "st10"])
                P.op("dve", lambda e: e.reciprocal(out=st[:, 11:12], in_=st[:, 10:11]), reads=["st10"], writes=["st11"])
                P.op("dve", lambda e: e.tensor_scalar(out=aff2[:, t, 32 * b:32 * b + 16], in0=ex,
                                                      scalar1=st[:, 11:12], scalar2=None, op0=ALU.mult),
                     reads=["ex", "st11"], writes=["aff2"])

            DX(0)
            for t in range(NT):
                if t + 1 < NT:
                    DX(t + 1)
                DY(t)
        for t in range(NT):
            P.op("pe", lambda e, t=t: e.transpose(out=pp[2 + t // 8][0:48, (t % 8) * 128:(t % 8) * 128 + 128],
                                                  in_=aff2[:, t, :], identity=identf[:]),
                 reads=["aff2"], writes=BP(2 + t // 8))
        for hh in range(2):
            P.op("act", lambda e, hh=hh: e.copy(out=affT[:, hh * 1024:(hh + 1) * 1024], in_=pp[2 + hh][0:48, :]),
                 reads=BP(2 + hh), writes=["affT"])
            P.op("dve", lambda e, hh=hh: e.tensor_copy(out=work[:, hh * 1024:(hh + 1) * 1024], in_=pp[2 + hh][0:48, :]),
                 reads=BP(2 + hh), writes=["work"])
        for r in range(CAP // 8):
            P.op("dve", lambda e: e.max(out=mx8, in_=work), reads=["work"], writes=["mx8"])
            P.op("dve", lambda e: e.match_replace(out=work, in_to_replace=mx8, in_values=work, imm_value=0.0),
                 reads=["work", "mx8"], writes=["work"])
        P.op("dve", lambda e: e.tensor_tensor(out=work, in0=affT, in1=work, op=ALU.subtract),
             reads=["affT", "work"], writes=["work"])
        for t in range(NT):
            P.op("pe", lambda e, t=t: e.transpose(out=pp[t // 8][:, (t % 8) * 64:(t % 8) * 64 + 48],
                                                  in_=work[:, t * 128:(t + 1) * 128], identity=identf[0:48, 0:48]),
                 reads=["work"], writes=BP(t // 8))
        for hh in range(2):
            P.op("act", lambda e, hh=hh: e.copy(out=gate2[:, hh * 8:(hh + 1) * 8, :],
                                                in_=pp[hh][:, 0:512].rearrange("p (a b) -> p a b", a=8)[:, :, 0:48]),
                 reads=BP(hh), writes=["gate2"])
        P.op("dve", lambda e: e.tensor_scalar(out=fl(mask2), in0=fl(gate2), scalar1=0.0, scalar2=None, op0=ALU.is_gt),
             reads=["gate2"], writes=["mask2"])
        P.op("dve", lambda e: e.tensor_scalar(out=fl(mask2f), in0=fl(gate2), scalar1=0.0, scalar2=None, op0=ALU.is_gt),
             reads=["gate2"], writes=["mask2f"])
        for hh in range(2):
            P.op("pe", lambda e, hh=hh: e.matmul(bank(4 + hh)[:, 0:384], lhsT=trib[:], rhs=fl(mask2)[:, hh * 384:(hh + 1) * 384],
                                                 start=True, stop=True), reads=["mask2"], writes=["ps:%d" % (4 + hh)])
            P.op("pe", lambda e, hh=hh: e.matmul(bank(6 + hh)[:, 0:384], lhsT=onesb[:], rhs=fl(mask2)[:, hh * 384:(hh + 1) * 384],
                                                 start=True, stop=True), reads=["mask2"], writes=["ps:%d" % (6 + hh)])
            P.op("act", lambda e, hh=hh: e.copy(out=fl(totsb)[:, hh * 384:(hh + 1) * 384], in_=bank(6 + hh)[:, 0:384]),
                 reads=["ps:%d" % (6 + hh)], writes=["totsb"])
        P.op("dve", lambda e: e.memset(base[:, 0, :], 0.0), writes=["base"])
        for t in range(1, NT):
            P.op("dve", lambda e, t=t: e.tensor_tensor(out=base[:, t, :], in0=base[:, t - 1, :], in1=totsb[:, t - 1, :],
                                                      op=ALU.add), reads=["base", "totsb"], writes=["base"])
        for hh in range(2):
            P.op("dve", lambda e, hh=hh: e.tensor_tensor(out=fl(pos)[:, hh * 384:(hh + 1) * 384], in0=bank(4 + hh)[:, 0:384],
                                                        in1=fl(base)[:, hh * 384:(hh + 1) * 384], op=ALU.add),
                 reads=["ps:%d" % (4 + hh), "base"], writes=["pos"])
        P.op("dve", lambda e: e.scalar_tensor_tensor(out=fl(pos), in0=fl(pos), scalar=1.0, in1=fl(mask2f),
                                                     op0=ALU.add, op1=ALU.mult), reads=["pos", "mask2f"], writes=["pos"])
        P.op("dve", lambda e: e.tensor_scalar(out=fl(posm[:]), in0=fl(pos), scalar1=-1.0, scalar2=None, op0=ALU.add),
             reads=["pos"], writes=["posm"])
        for t in range(NT):
            P.op("pe", lambda e, t=t: e.transpose(out=pp[2 + t // 8][0:48, (t % 8) * 128:(t % 8) * 128 + 128],
                                                  in_=posm[:, t, :], identity=identf[:]),
                 reads=["posm"], writes=BP(2 + t // 8))
        for hh in range(2):
            P.op("act", lambda e, hh=hh: e.copy(out=posmT[:, hh * 1024:(hh + 1) * 1024], in_=pp[2 + hh][0:48, :]),
                 reads=BP(2 + hh), writes=["posmT"])
        P.op("dve", lambda e: e.tensor_copy(out=fl(gh), in_=fl(gate2)), reads=["gate2"], writes=["gh"])
        P.op("dve", lambda e: e.tensor_copy(out=fl(ghf), in_=fl(gh)), reads=["gh"], writes=["ghf"])
        P.op("dve", lambda e: e.tensor_tensor(out=ghl[:, :, :, 1], in0=gate2, in1=ghf, op=ALU.subtract),
             reads=["gate2", "ghf"], writes=["ghl1"])
        P.op("dve", lambda e: e.tensor_copy(out=ghl[:, :, :, 0], in_=gh), reads=["gh"], writes=["ghl0"])

    def epoch_e():
        cv = Carver()
        ring = [cv.get([128, 8, 512], BF16) for _ in range(8)]
        S = [cv.get([128, NT, 256], BF16) for _ in range(2)]
        xeT = cv.get([128, 8, 512], BF16)
        hidT = cv.get([128, 16, 512], BF16)
        sil = [cv.get([128, 512], F32) for _ in range(2)]
        ye_sb = cv.get([128, 4, D], F32)
        gt2bc = cv.get([128, 2, D], F32)
        pgs = cv.get([128, 2, 16], F32)
        idxf = cv.get([128, 2, 4], F32)
        gsb = cv.get([128, 2, 4], F32)
        xetok = [cv.get([128, 4, D], BF16) for _ in range(2)]
        d_sc = [P.dsem("scat%d" % gi) for gi in range(4)]
        d_g = [[P.dsem("gath%d_%d" % (pp_, gi)) for gi in range(4)] for pp_ in range(2)]
        x1flat = x1s.rearrange("b t d -> (b t) d")
        for b in range(2):
            for hh in range(2):
                P.op("pe", lambda e, b=b, hh=hh: e.matmul(bank(6), lhsT=sel3[:, b, :], rhs=msb[:, 3, hh * 512:(hh + 1) * 512],
                                                         start=True, stop=True), reads=[], writes=["ps:6"])
                P.op("act", lambda e, b=b, hh=hh: e.copy(out=gt2bc[:, b, hh * 512:(hh + 1) * 512], in_=bank(6)),
                     reads=["ps:6"], writes=["gt2bc"])
        NRING = 8
        uspec = []
        for e2 in range(NE):
            for g in range(4):
                uspec.append(("a", w1[e2][:, g * 512:(g + 1) * 512].rearrange("(k p) f -> p k f", p=128)))
                uspec.append(("a", w3[e2][:, g * 512:(g + 1) * 512].rearrange("(k p) f -> p k f", p=128)))
            for dq in range(4):
                uspec.append(("b", [w2[e2][hf * 1024:(hf + 1) * 1024, dq * 256:(dq + 1) * 256]
                                    .rearrange("(c p) d -> p c d", p=128) for hf in range(2)]))
        issued = [0]

        def uview(u):
            s_ = u % NRING
            if uspec[u][0] == "a":
                return ring[s_], "ring%d" % s_
            return ring[s_].rearrange("p a b -> p (a b)").rearrange("p (c d) -> p c d", c=16), "ring%d" % s_

        def ensure_issued(upto):
            upto = min(upto, len(uspec) - 1)
            while issued[0] <= upto:
                u = issued[0]
                v, key = uview(u)
                if uspec[u][0] == "a":
                    load("pool", v, uspec[u][1], key)
                else:
                    for hf in range(2):
                        load("pool", v[:, hf * 8:(hf + 1) * 8, :], uspec[u][1][hf], key)
                issued[0] += 1

        def prep(e_):
            par = e_ % 2
            for b in range(2):
                col = 32 * b + e_
                for t in range(NT):
                    P.op("dve", lambda e, b=b, t=t, col=col: e.tensor_scalar(out=S[b][:, t, :], in0=iota256[:],
                                                                           scalar1=posm[:, t, col:col + 1], scalar2=None,
                                                                           op0=ALU.is_equal),
                         reads=[], writes=["S%d" % b])
            pg = bank(5)[:, 256:272]
            for b in range(2):
                col = 32 * b + e_
                for half in range(2):
                    gi = b * 2 + half
                    for t in range(NT):
                        P.op("pe", lambda e, b=b, t=t, half=half, gi=gi, col=col: e.matmul(
                            pg[:, gi * 4:gi * 4 + 4], lhsT=S[b][:, t, half * 128:(half + 1) * 128],
                            rhs=ghl[:, t, col, :], start=(t == 0), stop=(t == NT - 1)),
                             reads=["S%d" % b], writes=["ps:5"])
            P.op("dve", lambda e: e.tensor_copy(out=pgs[:, par, :], in_=pg), reads=["ps:5"], writes=["pgs%d" % par])
            pv4 = pgs[:, par, :].rearrange("p (a b) -> p a b", a=4)
            P.op("dve", lambda e: e.tensor_tensor(out=gsb[:, par, :], in0=pv4[:, :, 0], in1=pv4[:, :, 1], op=ALU.add),
                 reads=["pgs%d" % par], writes=["gs%d" % par, "x1gen"])
            P.op("dve", lambda e: e.scalar_tensor_tensor(out=idxf[:, par, :], in0=pv4[:, :, 2], scalar=256.0,
                                                         in1=pv4[:, :, 3], op0=ALU.mult, op1=ALU.add),
                 reads=["pgs%d" % par], writes=["idxf%d" % par])
            P.op("dve", lambda e: e.tensor_copy(out=idxi[:, par * 4:par * 4 + 4], in_=idxf[:, par, :]),
                 reads=["idxf%d" % par], writes=["idxi%d" % par])
            for gi in range(4):
                P.dma("pool", lambda e, gi=gi: e.indirect_dma_start(
                    out=xetok[par][:, gi, :], out_offset=None, in_=h2s[:, :],
                    in_offset=bass.IndirectOffsetOnAxis(ap=idxi[:, par * 4 + gi:par * 4 + gi + 1], axis=0)),
                      d_g[par][gi], reads=["idxi%d" % par], writes=["xetok%d_%d" % (par, gi)])

        import os
        ne_dbg = int(os.environ.get("K_NE", NE))
        prep(0)
        for e_ in range(ne_dbg):
            par = e_ % 2
            gs = gsb[:, par, :]
            for gi in range(4):
                pT = bankb(6 + gi % 2)
                for k in range(8):
                    P.op("pe", lambda e, gi=gi, k=k, pT=pT, par=par: e.transpose(out=pT[:, k * 128:(k + 1) * 128],
                                                                        in_=xetok[par][:, gi, k * 128:(k + 1) * 128],
                                                                        identity=identb[:]),
                         reads=["xetok%d_%d" % (par, gi)], writes=["ps:%d" % (6 + gi % 2)])
                P.op("act", lambda e, gi=gi, pT=pT: e.copy(out=xeT[:, :, gi * 128:(gi + 1) * 128],
                                                           in_=pT.rearrange("p (a b) -> p a b", a=8)),
                     reads=["ps:%d" % (6 + gi % 2)], writes=["xeT"])
            for g in range(4):
                ua = 12 * e_ + 2 * g
                ensure_issued(ua + 1)
                wa, ka = uview(ua)
                wb, kb = uview(ua + 1)
                for mm in range(4):
                    m = g * 4 + mm
                    ph1 = bank(m % 2)
                    ph3 = bank(2 + m % 2)
                    for k in range(8):
                        P.op("pe", lambda e, k=k, mm=mm, wa=wa, ph1=ph1: e.matmul(
                            ph1, lhsT=wa[:, k, mm * 128:(mm + 1) * 128], rhs=xeT[:, k, :], start=(k == 0), stop=(k == 7)),
                             reads=[ka, "xeT"], writes=["ps:%d" % (m % 2)])
                    for k in range(8):
                        P.op("pe", lambda e, k=k, mm=mm, wb=wb, ph3=ph3: e.matmul(
                            ph3, lhsT=wb[:, k, mm * 128:(mm + 1) * 128], rhs=xeT[:, k, :], start=(k == 0), stop=(k == 7)),
                             reads=[kb, "xeT"], writes=["ps:%d" % (2 + m % 2)])
                    P.op("act", lambda e, m=m, ph1=ph1: e.activation(out=sil[m % 2], in_=ph1, func=AF.Silu),
                         reads=["ps:%d" % (m % 2)], writes=["sil%d" % (m % 2)])
                    P.op("dve", lambda e, m=m, ph3=ph3: e.tensor_tensor(out=hidT[:, m, :], in0=sil[m % 2], in1=ph3,
                                                                        op=ALU.mult),
                         reads=["sil%d" % (m % 2), "ps:%d" % (2 + m % 2)], writes=["hidT"])
                ensure_issued(min(ua + 1 + NRING, 12 * e_ + 11))
            ensure_issued(12 * e_ + 11)
            if e_ + 1 < ne_dbg:
                prep(e_ + 1)
            for dq in range(4):
                uw = 12 * e_ + 8 + dq
                wv, kw = uview(uw)
                for sc in range(4):
                    pye = bank(4 + sc % 2)[:, 0:256]
                    for m in range(16):
                        P.op("pe", lambda e, m=m, sc=sc, wv=wv, pye=pye: e.matmul(
                            pye, lhsT=hidT[:, m, sc * 128:(sc + 1) * 128], rhs=wv[:, m, :], start=(m == 0), stop=(m == 15)),
                             reads=[kw, "hidT"], writes=["ps:%d" % (4 + sc % 2)])
                    P.op("dve", lambda e, sc=sc, dq=dq, pye=pye, gs=gs: e.scalar_tensor_tensor(
                        out=ye_sb[:, sc, dq * 256:(dq + 1) * 256], in0=pye, scalar=gs[:, sc:sc + 1],
                        in1=gt2bc[:, sc // 2, dq * 256:(dq + 1) * 256], op0=ALU.mult, op1=ALU.mult),
                         reads=["ps:%d" % (4 + sc % 2), "gs%d" % par, "gt2bc"], writes=["ye_sb"])
                ensure_issued(uw + NRING)
            for gi in range(4):
                P.dma("pool", lambda e, gi=gi, par=par: e.indirect_dma_start(
                    out=x1flat[:, :], out_offset=bass.IndirectOffsetOnAxis(ap=idxi[:, par * 4 + gi:par * 4 + gi + 1], axis=0),
                    in_=ye_sb[:, gi, :], in_offset=None, compute_op=ALU.add),
                      d_sc[gi], reads=["ye_sb", "idxi%d" % par, "x1gen"])

    def epoch_f(b):
        cv = Carver()
        x1t = [cv.get([128, D], F32) for _ in range(3)]
        outt = [cv.get([128, D], F32) for _ in range(2)]
        sqj = cv.get([128, D], BF16)
        for t in range(NT):
            xb = x1t[t % 3]
            xk = "x1t%d" % (t % 3)
            ob = outt[t % 2]
            okk = "outt%d" % (t % 2)
            c0 = 2 * (t % 2)
            load("sync", xb, x1s[b, t * 128:(t + 1) * 128, :], xk)
            P.op("act", lambda e, xb=xb, c0=c0: e.activation(out=sqj, in_=xb, func=AF.Square, accum_out=st[:, c0:c0 + 1]),
                 reads=[xk], writes=["sqj", "st%d" % c0])
            rstd_chain(c0, 1.0 / D)
            P.op("dve", lambda e, xb=xb, ob=ob, c0=c0: e.scalar_tensor_tensor(out=ob, in0=xb, scalar=st[:, c0 + 1:c0 + 2],
                                                                             in1=gfinbc[:], op0=ALU.mult, op1=ALU.mult),
                 reads=[xk, "st%d" % (c0 + 1)], writes=[okk])
            P.dma("sync", lambda e, ob=ob, t=t: e.dma_start(out=y[b, t * 128:(t + 1) * 128, :], in_=ob),
                  d_outs[t % 2], reads=[okk])

    if stage >= 3:
        epoch_d()
        P.barrier_all()
        if stage == 25:
            P.final_wait("sync", d_outs)
            P.emit()
            return nc
        epoch_e()
        P.barrier_all()
        if stage == 26:
            P.final_wait("sync", d_outs)
            P.emit()
            return nc
        for b in range(2):
            epoch_f(b)

    P.final_wait("sync", d_outs)
    P.emit()
    return nc


def build_moe(nc, P, env):
    raise NotImplementedError


_STAGE = 3


def kernel(x, c, ctx, c_ctx, w_mod, b_mod, g_mix, g_ffn, w_in, q_gain, k_gain, v_gain,
           w_s, b_s, w_out, w_router, w1, w3, w2, g_final):
    f = lambda a: np.ascontiguousarray(np.asarray(a, dtype=np.float32))
    x, c, ctx, c_ctx = f(x), f(c), f(ctx), f(c_ctx)
    consts = _consts()
    rows = np.concatenate([f(g_ffn)[0], f(g_final), np.tile(f(q_gain)[0], 8), np.tile(f(k_gain)[0], 2),
                           f(v_gain)[0]])
    assert rows.shape[0] == 3200
    shared = {
        "w_mod": f(w_mod)[0], "bmod3": np.tile(f(b_mod)[0][None, :], (3, 1)),
        "rows3": np.tile(rows[None, :], (3, 1)),
        "g2": np.concatenate([f(g_mix)[0].reshape(8, 128), f(g_ffn)[0].reshape(8, 128)], axis=0),
        "w_in": f(w_in)[0], "w_s": f(w_s)[0], "b_s": f(b_s)[0], "w_out": f(w_out)[0],
        "w_router": f(w_router)[0], "w1": f(w1)[0], "w3": f(w3)[0], "w2": f(w2)[0],
    }
    shared.update(consts)
    in_maps = []
    for i in range(NCORES):
        m = dict(shared)
        m["x"] = x[2 * i:2 * i + 2]
        m["ctx"] = ctx[2 * i:2 * i + 2]
        m["c3"] = np.concatenate([c[2 * i:2 * i + 2], c_ctx[None, :]], axis=0)
        in_maps.append(m)
    nc = build_nc(_STAGE)
    res = run_bass_kernel_spmd(nc, in_maps, core_ids=list(range(NCORES)))
    return np.concatenate([r["y"] for r in res.results], axis=0)
```
